# Optimizing a Trainium2 kernel written in Bass

```python
import math
import jax, jax.numpy as jnp
from jax import lax
import numpy as np

D_MODEL = 1024
BATCH = 16
SEQ = 2048
DEPTH = 2

D_MIX = D_MODEL
D_ATTN = D_MIX // 2
D_POOL = D_MIX - D_ATTN
N_HEADS = 8
HEAD_DIM = D_ATTN // N_HEADS
ROT_DIM = HEAD_DIM // 4
ROPE_THETA = 500000.0
MOBA_BLOCK = 256
MOBA_TOPK = 3
Q_CHUNK = 32
POOL_WINDOWS = (2, 4, 8, 16)
N_POOL_GROUPS = len(POOL_WINDOWS)
POOL_GROUP = D_POOL // N_POOL_GROUPS
D_IN = 3 * D_ATTN + D_POOL
N_EXPERTS = 16
N_EXPERT_GROUPS = 4
EXPERTS_PER_GROUP = N_EXPERTS // N_EXPERT_GROUPS
TOP_K = 2
D_EXPERT = 512
DEEPNORM_ALPHA = (2 * DEPTH) ** 0.25
DEEPNORM_BETA = (8 * DEPTH) ** -0.25
N_MOD = 6
LN_EPS = 1e-5
NEG_INF = -1e30

kernel_name = "hymba_moba_pool_grouped_moe_deepnorm"


def layer_norm_plain(x):
    xf = x.astype(jnp.float32)
    mu = xf.mean(-1, keepdims=True)
    var = jnp.square(xf - mu).mean(-1, keepdims=True)
    return ((xf - mu) * lax.rsqrt(var + LN_EPS)).astype(x.dtype)


def layer_norm_affine(x, g, b):
    xf = x.astype(jnp.float32)
    mu = xf.mean(-1, keepdims=True)
    var = jnp.square(xf - mu).mean(-1, keepdims=True)
    y = (xf - mu) * lax.rsqrt(var + LN_EPS) * g.astype(jnp.float32) + b.astype(jnp.float32)
    return y.astype(x.dtype)


def split_heads(t):
    B, S, _ = t.shape
    return t.reshape(B, S, N_HEADS, HEAD_DIM).transpose(0, 2, 1, 3)


def partial_rope(x, pos):
    half = ROT_DIM // 2
    inv_freq = ROPE_THETA ** (-(jnp.arange(half, dtype=jnp.float32) * 2.0 / ROT_DIM))
    ang = pos.astype(jnp.float32)[:, None] * inv_freq[None, :]
    cos, sin = jnp.cos(ang), jnp.sin(ang)
    xr = x[..., :ROT_DIM].astype(jnp.float32)
    x1, x2 = xr[..., :half], xr[..., half:]
    rot = jnp.concatenate([x1 * cos - x2 * sin, x2 * cos + x1 * sin], axis=-1).astype(x.dtype)
    return jnp.concatenate([rot, x[..., ROT_DIM:]], axis=-1)


def moba_attention(q, k, v):
    B, H, S, Dh = q.shape
    n_blk = -(-S // MOBA_BLOCK)
    S_pad = n_blk * MOBA_BLOCK
    pad = ((0, 0), (0, 0), (0, S_pad - S), (0, 0))
    q, k, v = jnp.pad(q, pad), jnp.pad(k, pad), jnp.pad(v, pad)
    n_sel = min(MOBA_TOPK, n_blk - 1)
    scale = HEAD_DIM ** -0.5
    kb = k.reshape(B, H, n_blk, MOBA_BLOCK, Dh)
    vb = v.reshape(B, H, n_blk, MOBA_BLOCK, Dh)
    qblk = jnp.arange(S_pad) // MOBA_BLOCK
    if n_sel > 0:
        kbar = kb.astype(jnp.float32).mean(axis=3)
        gate = jnp.einsum('bhsd,bhnd->bhsn', q.astype(jnp.float32), kbar)
        past = jnp.arange(n_blk)[None, :] < qblk[:, None]
        gate = jnp.where(past, gate, NEG_INF)
        _, sel = lax.top_k(gate, n_sel)
        sel_ok = jnp.arange(n_sel)[None, :] < qblk[:, None]
    else:
        sel = jnp.zeros((B, H, S_pad, 0), jnp.int32)
        sel_ok = jnp.zeros((S_pad, 0), bool)
    n_chunk = S_pad // Q_CHUNK
    q_all = q.reshape(B, H, n_chunk, Q_CHUNK, Dh).transpose(2, 0, 1, 3, 4)
    sel_all = sel.reshape(B, H, n_chunk, Q_CHUNK, n_sel).transpose(2, 0, 1, 3, 4)
    ok_all = sel_ok.reshape(n_chunk, Q_CHUNK, n_sel)
    starts = jnp.arange(n_chunk, dtype=jnp.int32) * Q_CHUNK
    b_ix = jnp.arange(B)[:, None, None]
    h_ix = jnp.arange(H)[None, :, None]

    def chunk(args):
        qc, selc, okc, start = args
        blk_start = (start // MOBA_BLOCK) * MOBA_BLOCK
        k_own = lax.dynamic_slice_in_dim(k, blk_start, MOBA_BLOCK, axis=2)
        v_own = lax.dynamic_slice_in_dim(v, blk_start, MOBA_BLOCK, axis=2)
        qp = start + jnp.arange(Q_CHUNK)
        kp = blk_start + jnp.arange(MOBA_BLOCK)
        own = jnp.einsum('bhqd,bhkd->bhqk', qc, k_own, preferred_element_type=jnp.float32) * scale
        logits = [jnp.where(kp[None, :] <= qp[:, None], own, NEG_INF)]
        for r in range(n_sel):
            kg = kb[b_ix, h_ix, selc[..., r]]
            lr = jnp.einsum('bhqd,bhqkd->bhqk', qc, kg, preferred_element_type=jnp.float32) * scale
            logits.append(jnp.where(okc[:, r][:, None], lr, NEG_INF))
        p = jax.nn.softmax(jnp.concatenate(logits, axis=-1), axis=-1)
        ps = jnp.split(p, n_sel + 1, axis=-1)
        out = jnp.einsum('bhqk,bhkd->bhqd', ps[0].astype(v.dtype), v_own, preferred_element_type=jnp.float32)
        for r in range(n_sel):
            vg = vb[b_ix, h_ix, selc[..., r]]
            out = out + jnp.einsum('bhqk,bhqkd->bhqd', ps[r + 1].astype(v.dtype), vg, preferred_element_type=jnp.float32)
        return out.astype(v.dtype)

    out = lax.map(chunk, (q_all, sel_all, ok_all, starts))
    return out.transpose(1, 2, 0, 3, 4).reshape(B, H, S_pad, Dh)[:, :, :S]


def pool_mixer(p, w_pool, pool_scale):
    B, S, _ = p.shape
    pf = p.astype(jnp.float32).reshape(B, S, N_POOL_GROUPS, POOL_GROUP)
    cs = jnp.concatenate([jnp.zeros_like(pf[:, :1]), jnp.cumsum(pf, axis=1)], axis=1)
    t = jnp.arange(1, S + 1, dtype=jnp.float32)
    outs = []
    for g, w in enumerate(POOL_WINDOWS):
        hi = cs[:, 1:, g]
        lo = jnp.concatenate([jnp.zeros_like(cs[:, :w - 1, g]), cs[:, :S - w + 1, g]], axis=1)
        cnt = jnp.minimum(t, float(w))[None, :, None]
        outs.append((hi - lo) / cnt - pf[:, :, g])
    d = jnp.stack(outs, axis=2).astype(p.dtype)
    y = jnp.einsum('bsgc,gce->bsge', d, w_pool).reshape(B, S, D_POOL)
    return y * pool_scale


def grouped_moe(h, w_router, router_bias, w_gate, w_up, w_down):
    B, S, D = h.shape
    hf = h.reshape(B * S, D)
    scores = jax.nn.softmax((hf @ w_router).astype(jnp.float32), axis=-1)
    sel = scores + router_bias.astype(jnp.float32)
    grp = sel.reshape(-1, N_EXPERT_GROUPS, EXPERTS_PER_GROUP)
    grp_score = lax.top_k(grp, TOP_K)[0].sum(-1)
    best = jnp.argmax(grp_score, axis=-1)
    in_grp = (jnp.arange(N_EXPERTS) // EXPERTS_PER_GROUP)[None, :] == best[:, None]
    _, idx = lax.top_k(jnp.where(in_grp, sel, NEG_INF), TOP_K)
    wts = jnp.take_along_axis(scores, idx, axis=-1)
    wts = wts / wts.sum(-1, keepdims=True)
    gates = jnp.sum(jax.nn.one_hot(idx, N_EXPERTS, dtype=jnp.float32) * wts[..., None], axis=1)
    y = jnp.zeros((B * S, D), jnp.float32)
    for e in range(N_EXPERTS):
        a = jax.nn.silu(hf @ w_gate[e]) * (hf @ w_up[e])
        y = y + gates[:, e:e + 1] * (a @ w_down[e]).astype(jnp.float32)
    return y.astype(h.dtype).reshape(B, S, D)


def setup_inputs(seed: int = 0) -> dict:
    key = jax.random.key(seed)
    ks = jax.random.split(key, 20)
    f32 = jnp.float32
    nrm = lambda k, shape, s: jax.random.normal(k, shape, f32) * s
    x = jax.random.normal(ks[0], (BATCH, SEQ, D_MODEL), f32)
    c = jax.random.normal(ks[1], (BATCH, D_MODEL), f32)
    w_mod = nrm(ks[2], (DEPTH, D_MODEL, N_MOD * D_MODEL), 0.5 * D_MODEL ** -0.5)
    b_mod = nrm(ks[3], (DEPTH, N_MOD * D_MODEL), 0.02)
    w_in = nrm(ks[4], (DEPTH, D_MODEL, D_IN), D_MODEL ** -0.5)
    col_scale = jnp.concatenate([jnp.ones((2 * D_ATTN,), f32),
                                 jnp.full((D_ATTN,), DEEPNORM_BETA, f32),
                                 jnp.ones((D_POOL,), f32)])
    w_in = w_in * col_scale
    w_pool = nrm(ks[5], (DEPTH, N_POOL_GROUPS, POOL_GROUP, POOL_GROUP), POOL_GROUP ** -0.5 * DEEPNORM_BETA)
    pool_scale = 1.0 + nrm(ks[6], (DEPTH, D_POOL), 0.1)
    w_out = nrm(ks[7], (DEPTH, D_MIX, D_MODEL), D_MIX ** -0.5 * DEEPNORM_BETA)
    ln1_g = 1.0 + nrm(ks[8], (DEPTH, D_MODEL), 0.05)
    ln1_b = nrm(ks[9], (DEPTH, D_MODEL), 0.02)
    w_router = nrm(ks[10], (D_MODEL, N_EXPERTS), D_MODEL ** -0.5)
    router_bias = nrm(ks[11], (N_EXPERTS,), 0.01)
    w_gate = nrm(ks[12], (DEPTH, N_EXPERTS, D_MODEL, D_EXPERT), D_MODEL ** -0.5)
    w_up = nrm(ks[13], (DEPTH, N_EXPERTS, D_MODEL, D_EXPERT), D_MODEL ** -0.5)
    w_down = nrm(ks[14], (DEPTH, N_EXPERTS, D_EXPERT, D_MODEL), D_EXPERT ** -0.5 * DEEPNORM_BETA)
    ln2_g = 1.0 + nrm(ks[15], (DEPTH, D_MODEL), 0.05)
    ln2_b = nrm(ks[16], (DEPTH, D_MODEL), 0.02)
    return {"x": x, "c": c, "w_mod": w_mod, "b_mod": b_mod, "w_in": w_in,
            "w_pool": w_pool, "pool_scale": pool_scale, "w_out": w_out,
            "ln1_g": ln1_g, "ln1_b": ln1_b, "w_router": w_router,
            "router_bias": router_bias, "w_gate": w_gate, "w_up": w_up,
            "w_down": w_down, "ln2_g": ln2_g, "ln2_b": ln2_b}


def reference(x, c, w_mod, b_mod, w_in, w_pool, pool_scale, w_out, ln1_g, ln1_b,
              w_router, router_bias, w_gate, w_up, w_down, ln2_g, ln2_b):
    B, S, D = x.shape
    pos = jnp.arange(S)
    cond = jax.nn.silu(c)
    for l in range(DEPTH):
        mod = cond @ w_mod[l] + b_mod[l]
        sh1, sc1, g1, sh2, sc2, g2 = [m[:, None, :] for m in jnp.split(mod, N_MOD, axis=-1)]
        h = layer_norm_plain(x) * (1 + sc1) + sh1
        proj = h @ w_in[l]
        q, k, v, p = jnp.split(proj, [D_ATTN, 2 * D_ATTN, 3 * D_ATTN], axis=-1)
        q = partial_rope(split_heads(q), pos)
        k = partial_rope(split_heads(k), pos)
        a = moba_attention(q, k, split_heads(v))
        a = a.transpose(0, 2, 1, 3).reshape(B, S, D_ATTN)
        m = pool_mixer(p, w_pool[l], pool_scale[l])
        y = jnp.concatenate([a, m], axis=-1) @ w_out[l]
        x = layer_norm_affine(DEEPNORM_ALPHA * x + g1 * y, ln1_g[l], ln1_b[l])
        h = layer_norm_plain(x) * (1 + sc2) + sh2
        y = grouped_moe(h, w_router, router_bias, w_gate[l], w_up[l], w_down[l])
        x = layer_norm_affine(DEEPNORM_ALPHA * x + g2 * y, ln2_g[l], ln2_b[l])
    return x
```

```python
import contextlib
import math
import numpy as np
import concourse.bass as bass
import concourse.mybir as mybir
from concourse.bass_utils import run_bass_kernel_spmd

F32 = mybir.dt.float32
BF16 = mybir.dt.bfloat16
I32 = mybir.dt.int32
AF = mybir.ActivationFunctionType
ALU = mybir.AluOpType
AX = mybir.AxisListType

ENGS = ["pe", "act", "dve", "pool", "sp"]
_EMBED_WAIT = True
DMA_RING = {"sp": 8, "act": 4, "pool": 6}


class Op:
    __slots__ = ("idx", "eng", "fn", "reads", "writes", "dma", "deps", "signal",
                 "sig", "ring_wait", "extra", "is_bar")

    def __init__(self, idx, eng, fn, reads, writes, dma):
        self.idx = idx
        self.eng = eng
        self.fn = fn
        self.reads = tuple(reads)
        self.writes = tuple(writes)
        self.dma = dma
        self.deps = ()
        self.signal = False
        self.sig = None
        self.ring_wait = None
        self.extra = ()
        self.is_bar = False


class Sched:
    def __init__(self):
        self.ops = []

    def add(self, eng, fn, reads=(), writes=(), dma=False):
        op = Op(len(self.ops), eng, fn, reads, writes, dma)
        self.ops.append(op)
        return op

    def pe(self, fn, reads=(), writes=()):
        return self.add("pe", fn, reads, writes)

    def act(self, fn, reads=(), writes=()):
        return self.add("act", fn, reads, writes)

    def dve(self, fn, reads=(), writes=()):
        return self.add("dve", fn, reads, writes)

    def pool(self, fn, reads=(), writes=()):
        return self.add("pool", fn, reads, writes)

    def dma(self, q, fn, reads=(), writes=()):
        return self.add(q, fn, reads, writes, dma=True)

    def barrier(self):
        last = {}
        dmas = {q: [] for q in DMA_RING}
        for op in self.ops:
            if op.is_bar:
                continue
            if op.dma:
                dmas[op.eng].append(op.idx)
            elif op.fn is not None:
                last[op.eng] = op.idx
        extra = list(last.values())
        for q, lst in dmas.items():
            extra.extend(lst[-DMA_RING[q]:])
        for e in ENGS:
            op = self.add(e, None)
            op.extra = tuple(extra)
            op.is_bar = True

    def analyze(self):
        last_w = {}
        readers = {}
        for op in self.ops:
            if op.is_bar:
                op.deps = tuple(sorted(d for d in op.extra
                                       if not (self.ops[d].eng == "pe" and op.eng == "pe")))
                continue
            deps = set()
            for k in op.reads:
                w = last_w.get(k)
                if w is not None:
                    deps.add(w)
            for k in op.writes:
                w = last_w.get(k)
                if w is not None:
                    deps.add(w)
                deps.update(readers.get(k, {}).values())
            deps.discard(op.idx)
            pruned = []
            for d in deps:
                p = self.ops[d]
                if p.eng == "pe" and op.eng == "pe" and not p.dma and not op.dma:
                    continue
                pruned.append(d)
            op.deps = tuple(sorted(pruned))
            for k in op.reads:
                rk = (op.eng, op.idx) if op.dma else op.eng
                readers.setdefault(k, {})[rk] = op.idx
            for k in op.writes:
                last_w[k] = op.idx
                readers[k] = {}
        for op in self.ops:
            for d in op.deps:
                self.ops[d].signal = True
        cnt = {e: 0 for e in ENGS}
        dcnt = {}
        dnum = {q: 0 for q in DMA_RING}
        for op in self.ops:
            if op.dma:
                q = op.eng
                i = dnum[q]
                dnum[q] += 1
                key = ("d", q, i % DMA_RING[q])
                prev = dcnt.get(key, 0)
                if prev > 0:
                    op.ring_wait = (key, prev)
                dcnt[key] = prev + 16
                op.sig = (key, prev + 16)
                op.signal = True
            elif op.signal:
                cnt[op.eng] += 1
                op.sig = (("c", op.eng), cnt[op.eng])

    def emit(self, nc):
        self.analyze()
        streams = {e: [o for o in self.ops if o.eng == e] for e in ENGS}
        with contextlib.ExitStack() as st:
            sems = {}
            for e in ENGS:
                sems[("c", e)] = st.enter_context(nc.semaphore("c_" + e))
            for q, n in DMA_RING.items():
                for i in range(n):
                    sems[("d", q, i)] = st.enter_context(nc.semaphore("d_%s%d" % (q, i)))
            block = st.enter_context(nc.Block())
            ops = self.ops

            def run(ename, eng):
                known = {}
                for op in streams[ename]:
                    waits = []
                    if op.ring_wait is not None:
                        waits.append(op.ring_wait)
                    for d in op.deps:
                        waits.append(ops[d].sig)
                    best = {}
                    for k, v in waits:
                        if v > best.get(k, 0):
                            best[k] = v
                    pend = []
                    for k, v in best.items():
                        if known.get(k, 0) >= v:
                            continue
                        pend.append((k, v))
                        known[k] = v
                    attach = None
                    if _EMBED_WAIT and pend and op.fn is not None and not op.dma and ename == 'pe':
                        attach = pend.pop()
                    for k, v in pend:
                        eng.wait_ge(sems[k], v)
                    if op.fn is None:
                        continue
                    ins = op.fn(eng)
                    if attach is not None:
                        ins._wait_ge(sems[attach[0]], attach[1])
                    if op.signal:
                        assert ins is not None
                        ins.then_inc(sems[op.sig[0]], 16 if op.dma else 1)

            @block.tensor
            def _(e):
                run("pe", e)

            @block.scalar
            def _(e):
                run("act", e)

            @block.vector
            def _(e):
                run("dve", e)

            @block.gpsimd
            def _(e):
                run("pool", e)

            @block.sync
            def _(e):
                run("sp", e)


D = 1024
SEQ = 2048
NT = 16
NL = 2
NE = 16
ALPHA = float(4.0 ** 0.25)
EPS = 1e-5
NEG = -1.0e30
BIGB = 30000.0
INVF = [float(np.float32(500000.0) ** np.float32(-(i * 2.0 / 16.0))) for i in range(8)]
TWO_PI = float(2 * np.pi)
PI = float(np.pi)


def build(nseq=2, nlayers=NL, dbg=None):
    nc = bass.Bass("TRN2", target_bir_lowering=False)

    def din(name, shape):
        return nc.dram_tensor(name, shape, F32, kind="ExternalInput").ap()

    x_d = din("x", [2, SEQ, D])
    c_d = din("c", [2, D])
    wmod_d = din("w_mod", [NL, D, 6 * D])
    bmod_d = din("b_mod", [NL, 6 * D])
    win_d = din("w_in", [NL, D, 2048])
    wpool_d = din("w_pool", [NL, 4, 128, 128])
    pscale_d = din("pool_scale", [NL, 512])
    wout_d = din("w_out", [NL, D, D])
    ln1g_d = din("ln1_g", [NL, D])
    ln1b_d = din("ln1_b", [NL, D])
    wr_d = din("w_router", [D, NE])
    rb_d = din("router_bias", [1, NE])
    wg_d = din("w_gate", [NL, NE, D, 512])
    wu_d = din("w_up", [NL, NE, D, 512])
    wd_d = din("w_down", [NL, NE, 512, D])
    ln2g_d = din("ln2_g", [NL, D])
    ln2b_d = din("ln2_b", [NL, D])
    out_d = nc.dram_tensor("out", [2, SEQ, D], F32, kind="ExternalOutput").ap()
    xs_d = nc.dram_tensor("xs_scr", [2, SEQ, D], F32, kind="Internal").ap()
    mod_d = nc.dram_tensor("mod_scr", [NL, 2, 6 * D], F32, kind="Internal").ap()
    dbg_d = None
    if dbg is not None:
        dbg_d = nc.dram_tensor("dbg", [128, dbg[1]], F32, kind="ExternalOutput").ap()

    S = Sched()

    B0 = 16640
    LIMIT = 229376

    def at(name, shape, dt, off):
        assert off % 32 == 0, (name, off)
        nb = int(np.prod(shape[1:])) * (4 if dt in (F32, I32) else 2)
        assert off + nb <= LIMIT, (name, off, nb)
        return nc.alloc_sbuf_tensor_at(name, shape, dt, offset=off)

    hT = at("hT", [128, 8, SEQ], BF16, B0)
    R = B0 + 32768
    qkT = at("qkT", [128, 8, SEQ], BF16, R)
    wbf = [at("wbf%d" % i, [128, 8, 512], BF16, R + 32768 + 8192 * i) for i in range(2)]
    stg = [at("stg%d" % i, [128, 2048], F32, R + 49152 + 8192 * i) for i in range(2)]
    wo = at("wo", [128, 8, D], BF16, R + 32768)
    vaug = at("vaug", [128, NT, 8, 65], BF16, R + 65536)
    dT = at("dT", [128, 4, SEQ], BF16, R + 82176)
    pbuf = at("pbuf", [128, 16 + SEQ], F32, R + 98560)
    tmpA = at("tmpA", [128, 16 + SEQ], F32, R + 106816)
    tmpB = at("tmpB", [128, 16 + SEQ], F32, R + 115072)
    probs = [at("probs%d" % i, [128, 512], BF16, R + 98560 + 1024 * i) for i in range(3)]
    biasT = at("biasT", [128, 1024], BF16, R + 101632)
    apair = [at("apair%d" % i, [128, 4, 128], BF16, R + 103680 + 1024 * i) for i in range(2)]
    qktok = [at("qktok%d" % i, [128, 512], BF16, R + 123328 + 1024 * i) for i in range(2)]
    esel = at("esel", [128, 64, 128], BF16, R + 106816)
    wpst = at("wpst", [128, 4, 128], F32, R + 125376)
    wpbf = at("wpbf", [128, 4, 128], BF16, R + 127424)
    wgu = [at("wgu%d" % i, [128, 8, 2, 512], BF16, R + 16384 * i) for i in range(2)]
    wdb = [at("wdb%d" % i, [128, 4, D], BF16, R + 32768 + 8192 * i) for i in range(2)]
    X = at("X", [128, NT, D], F32, R + 65536)
    Pp = R + 131072
    lnA = [at("lnA%d" % i, [128, D], F32, Pp + 4096 * i) for i in range(2)]
    htok = [at("htok%d" % i, [128, D], BF16, Pp + 8192 + 2048 * i) for i in range(2)]
    bc = [at("bc%d" % i, [128, D], F32, Pp + 12288 + 4096 * i) for i in range(4)]
    aT = [at("aT%d" % i, [128, 4, 512], BF16, Pp + 28672 + 4096 * i) for i in range(2)]
    sg = [at("sg%d" % i, [128, 512], F32, Pp + 36864 + 2048 * i) for i in range(2)]
    cpos = [Pp + 40960]

    def small(name, shape, dt):
        nb = int(np.prod(shape[1:])) * (4 if dt in (F32, I32) else 2)
        nb = (nb + 31) // 32 * 32
        t = at(name, shape, dt, cpos[0])
        cpos[0] += nb
        return t

    ident = small("ident", [128, 128], BF16)
    tri = small("tri", [128, 128], BF16)
    cos_t = small("cos_t", [128, NT, 8], F32)
    sin_t = small("sin_t", [128, NT, 8], F32)
    pastm = small("pastm", [128, 4, 64], F32)
    corr = small("corr", [128, 4, 16], F32)
    rb4 = small("rb4", [128, 4, NE], F32)
    wrb = small("wrb", [128, 8, NE], BF16)
    wrst = small("wrst", [128, 8, NE], F32)
    pscol = small("pscol", [128, NL, 4], F32)
    condT = small("condT", [128, 8, 2], F32)
    st6 = [small("st6_%d" % i, [128, 12], F32) for i in range(2)]
    mv = [small("mv%d" % i, [128, 2], F32) for i in range(2)]
    rstd = [small("rstd%d" % i, [128, 1], F32) for i in range(2)]
    nmr = [small("nmr%d" % i, [128, 1], F32) for i in range(2)]
    rcp = small("rcp", [128, 4], F32)
    gates = small("gates", [128, NT, NE], F32)
    rtb = Pp + 28672
    rt = [at("rt%d" % i, [128, 64], F32, rtb + 256 * i) for i in range(14)]
    rope_t = [at("rope_t%d" % i, [128, 8, 8], F32, rtb + 4096 + 256 * i) for i in range(4)]
    KB32 = at("KB32", [128, 4, 64], F32, rtb + 5120)
    KB = at("KB", [128, 4, 64], BF16, rtb + 6144)
    gm = at("gm", [128, 64], F32, rtb + 6656)
    srt = at("srt", [128, 64], F32, rtb + 6912)
    selt = at("selt", [128, 64], F32, rtb + 7168)
    btok = at("btok", [128, 128], BF16, rtb + 7424)
    modrow = at("modrow", [2, 512], F32, Pp + 36864)

    stA = small("stA", [128, NT, 12], F32)
    mvA = small("mvA", [128, NT, 2], F32)
    rsA = small("rsA", [128, NT], F32)
    nmA = small("nmA", [128, NT], F32)
    PF = [nc.alloc_psum_tensor("pf%d" % i, [128, 512], F32) for i in range(7)]
    PB = nc.alloc_psum_tensor("pb", [128, 1024], BF16)

    def MM(out, lhsT, rhs, st, sp):
        return lambda e: e.matmul(out, lhsT=lhsT, rhs=rhs, start=st, stop=sp)

    def TR(out, in_):
        idn = ident[:]
        return lambda e: e.transpose(out=out, in_=in_, identity=idn)

    def TT(out, in0, in1, op):
        return lambda e: e.tensor_tensor(out=out, in0=in0, in1=in1, op=op)

    def TS(out, in0, s1, s2, op0, op1=None):
        if op1 is None:
            return lambda e: e.tensor_scalar(out=out, in0=in0, scalar1=s1, scalar2=None, op0=op0)
        return lambda e: e.tensor_scalar(out=out, in0=in0, scalar1=s1, scalar2=s2, op0=op0, op1=op1)

    def STT(out, in0, scalar, in1, op0, op1):
        return lambda e: e.scalar_tensor_tensor(out=out, in0=in0, scalar=scalar, in1=in1, op0=op0, op1=op1)

    def CP(out, in_):
        return lambda e: e.tensor_copy(out=out, in_=in_)

    def ACP(out, in_):
        return lambda e: e.copy(out=out, in_=in_)

    def AV(out, in_, func, bias=None, scale=None):
        kw = {}
        if bias is not None:
            kw["bias"] = bias
        if scale is not None:
            kw["scale"] = scale
        return lambda e: e.activation(out=out, in_=in_, func=func, **kw)

    def DM(out, in_, **kw):
        return lambda e: e.dma_start(out=out, in_=in_, **kw)

    def MS(ap, val):
        return lambda e: e.memset(ap, val)

    def TRD(out, in_, op):
        return lambda e: e.tensor_reduce(out=out, in_=in_, axis=AX.X, op=op)

    def RCP(out, in_):
        return lambda e: e.reciprocal(out=out, in_=in_)

    def MAX8(out, in_):
        return lambda e: e.max(out=out, in_=in_)

    def range_reduce(a, t_i, t_f, t_m):
        S.dve(TS(t_f, a, float(1.0 / TWO_PI), None, ALU.mult), ["ang"], ["rr_f"])
        S.dve(CP(t_i, t_f), ["rr_f"], ["rr_i"])
        S.dve(CP(t_f, t_i), ["rr_i"], ["rr_f"])
        S.dve(STT(a, t_f, -TWO_PI, a, ALU.mult, ALU.add), ["rr_f", "ang"], ["ang"])
        S.dve(TS(t_m, a, PI, None, ALU.is_gt), ["ang"], ["rr_m"])
        S.dve(STT(a, t_m, -TWO_PI, a, ALU.mult, ALU.add), ["rr_m", "ang"], ["ang"])
        S.dve(TS(t_m, a, -PI, None, ALU.is_lt), ["ang"], ["rr_m"])
        S.dve(STT(a, t_m, TWO_PI, a, ALU.mult, ALU.add), ["rr_m", "ang"], ["ang"])

    S.pool(MS(ident[:], 1.0), [], ["ident"])
    S.pool(lambda e: e.affine_select(out=ident[:], in_=ident[:], pattern=[[-1, 128]], compare_op=ALU.is_equal,
                                     fill=0.0, base=0, channel_multiplier=1), ["ident"], ["ident"])
    S.pool(MS(tri[:], 1.0), [], ["tri"])
    S.pool(lambda e: e.affine_select(out=tri[:], in_=tri[:], pattern=[[1, 128]], compare_op=ALU.is_ge,
                                     fill=0.0, base=0, channel_multiplier=-1), ["tri"], ["tri"])
    posi = at("posi", [128, NT], I32, Pp)
    posf = at("posf", [128, NT], F32, Pp + 64)
    angc = at("angc", [128, NT * 8], F32, Pp + 128)
    angs = at("angs", [128, NT * 8], F32, Pp + 128 + 512)
    rr_i = at("rr_i", [128, NT * 8], I32, Pp + 128 + 1024)
    rr_f = at("rr_f", [128, NT * 8], F32, Pp + 128 + 1536)
    rr_m = at("rr_m", [128, NT * 8], F32, Pp + 128 + 2048)
    S.pool(lambda e: e.iota(posi[:], pattern=[[128, NT]], base=0, channel_multiplier=1), [], ["posi"])
    S.dve(CP(posf[:], posi[:]), ["posi"], ["posf"])
    angs3 = angs[:].rearrange("p (i f) -> p i f", f=8)
    for f in range(8):
        S.dve(TS(angs3[:, :, f], posf[:], INVF[f], None, ALU.mult), ["posf"], ["ang"])
    S.dve(TS(angc[:], angs[:], float(PI / 2), None, ALU.add), ["ang"], ["angc"])
    range_reduce(angs[:], rr_i[:], rr_f[:], rr_m[:])
    S.act(AV(sin_t[:].rearrange("p i f -> p (i f)"), angs[:], AF.Sin), ["ang"], ["sin_t"])
    S.dve(CP(angs[:], angc[:]), ["angc", "sin_t"], ["ang"])
    range_reduce(angs[:], rr_i[:], rr_f[:], rr_m[:])
    S.act(AV(cos_t[:].rearrange("p i f -> p (i f)"), angs[:], AF.Sin), ["ang"], ["cos_t"])
    S.pool(MS(pastm[:], 0.0), [], ["pastm"])
    for jj in range(4):
        pv = pastm[:, jj, :].rearrange("p (h n) -> p h n", n=8)
        S.pool(MS(pv[:, :, 4 + jj:8], NEG), ["pastm"], ["pastm"])
    S.pool(MS(corr[:], 1.0), [], ["corr"])
    for g in range(4):
        w = 2 ** (g + 1)
        for t in range(w - 1):
            S.pool(MS(corr[:, g, t:t + 1], float(w) / float(t + 1)), ["corr"], ["corr"])
    S.dma("sp", DM(wrst[:], wr_d.rearrange("(k p) n -> p k n", p=128)), [], ["wrst"])
    S.dve(CP(wrb[:], wrst[:]), ["wrst"], ["wrb"])
    for i in range(4):
        S.dma("sp", DM(rb4[:, i, :], rb_d.partition_broadcast(128)), [], [("rb4", i)])
    for l in range(NL):
        S.dma("sp", DM(pscol[:, l, :], pscale_d[l].rearrange("(g p) -> p g", p=128), allow_slow_non_contiguous=True),
              [], [("pscol", l)])
    for k in range(8):
        S.dma("sp", DM(condT[:, k, :], c_d[:, k * 128:(k + 1) * 128].rearrange("b p -> p b"),
                       allow_slow_non_contiguous=True), [], [("condT", k)])
    S.act(AV(condT[:], condT[:], AF.Silu), [("condT", k) for k in range(8)], ["condT"])
    NWM = 6
    wm = [at("wm%d" % i, [128, 8, 512], F32, R + 16384 * i) for i in range(NWM)]
    gi = 0
    for l in range(nlayers):
        for cg in range(12):
            sl = gi % NWM
            for h in range(2):
                S.dma("sp" if h == 0 else "act", DM(wm[sl][:, 4 * h:4 * h + 4, :],
                               wmod_d[l, 512 * h:512 * h + 512, cg * 512:(cg + 1) * 512].rearrange("(k p) n -> p k n", p=128)),
                      [], [("wm", sl, h)])
            for k in range(8):
                S.pe(MM(PF[0][0:2, :], condT[:, k, :], wm[sl][:, k, :], k == 0, k == 7),
                     [("wm", sl, k // 4), "condT"], ["pf0"])
            S.act(ACP(modrow[:], PF[0][0:2, :]), ["pf0"], ["modrow"])
            S.dma("sp", DM(mod_d[l, :, cg * 512:(cg + 1) * 512], modrow[:]), ["modrow"], [("mod_d", l, cg)])
            gi += 1
    mod_keys = [("mod_d", l, cg) for l in range(nlayers) for cg in range(12)]

    def load_bc(slot, src_row, add_row=None, plus1=False):
        key = ("bc", slot)
        S.dma("sp", DM(bc[slot][:], src_row.partition_broadcast(128)), [], [key])
        if add_row is not None:
            S.dma("sp", DM(lnA[1][:], add_row.partition_broadcast(128)), [], ["lnA1"])
            if plus1:
                S.dve(STT(bc[slot][:], bc[slot][:], 1.0, lnA[1][:], ALU.add, ALU.add), [key, "lnA1"], [key])
            else:
                S.dve(TT(bc[slot][:], bc[slot][:], lnA[1][:], ALU.add), [key, "lnA1"], [key])

    def ln_tile(src, src_keys, work, work_keys, gain, gain_keys, bias, bias_keys, out, out_keys, sl, add_eng="pool"):
        kst, kmv, krs, knm = "st6_%d" % sl, "mv%d" % sl, "rstd%d" % sl, "nmr%d" % sl
        st_, mv_, rs_, nm_ = st6[sl], mv[sl], rstd[sl], nmr[sl]
        S.dve(lambda e: e.bn_stats(out=st_[:, 0:6], in_=src[:, 0:512]), src_keys, [kst + "a"])
        S.dve(lambda e: e.bn_stats(out=st_[:, 6:12], in_=src[:, 512:1024]), src_keys, [kst + "b"])
        S.dve(lambda e: e.bn_aggr(out=mv_[:], in_=st_[:]), [kst + "a", kst + "b"], [kmv])
        S.dve(TS(rs_[:], mv_[:, 1:2], EPS, None, ALU.add), [kmv], [krs])
        S.act(lambda e: e.sqrt(out=rs_[:], in_=rs_[:]), [krs], [krs])
        S.dve(RCP(rs_[:], rs_[:]), [krs], [krs])
        S.dve(STT(nm_[:], mv_[:, 0:1], -1.0, rs_[:], ALU.mult, ALU.mult), [kmv, krs], [knm])
        S.act(AV(work, src, AF.Identity, bias=nm_[:], scale=rs_[:]), list(src_keys) + [krs, knm], work_keys)
        S.dve(TT(work, work, gain, ALU.mult), list(work_keys) + list(gain_keys), work_keys)
        S.add(add_eng, TT(out, work, bias, ALU.add), list(work_keys) + list(bias_keys), out_keys)

    def ln_stats(i, src, src_keys):
        S.dve(lambda e: e.bn_stats(out=stA[:, i, 0:6], in_=src[:, 0:512]), src_keys, [("stA", i, 0)])
        S.dve(lambda e: e.bn_stats(out=stA[:, i, 6:12], in_=src[:, 512:1024]), src_keys, [("stA", i, 1)])
        S.dve(lambda e: e.bn_aggr(out=mvA[:, i, :], in_=stA[:, i, :]), [("stA", i, 0), ("stA", i, 1)], [("mvA", i)])

    def ln_finish(eps=EPS):
        mvk = [("mvA", i_) for i_ in range(NT)]
        S.dve(TS(rsA[:], mvA[:, :, 1], eps, None, ALU.add), mvk, ["rsA"])
        S.act(lambda e: e.sqrt(out=rsA[:], in_=rsA[:]), ["rsA"], ["rsA"])
        S.dve(RCP(rsA[:], rsA[:]), ["rsA"], ["rsA"])
        S.dve(STT(nmA[:], mvA[:, :, 0], -1.0, rsA[:], ALU.mult, ALU.mult), mvk + ["rsA"], ["nmA"])

    def ln_apply(i, src, src_keys, work, work_keys, gain, gain_keys, bias, bias_keys, out, out_keys):
        S.act(AV(work, src, AF.Identity, bias=nmA[:, i:i + 1], scale=rsA[:, i:i + 1]),
              list(src_keys) + ["rsA", "nmA"], work_keys)
        S.dve(TT(work, work, gain, ALU.mult), list(work_keys) + list(gain_keys), work_keys)
        S.dve(TT(out, work, bias, ALU.add), list(work_keys) + list(bias_keys), out_keys)

    def transpose_to_hT(i, sl):
        for k in range(8):
            S.pe(TR(PB[:, k * 128:(k + 1) * 128], htok[sl][:, k * 128:(k + 1) * 128]), [("htok", sl), "ident"], ["pb"])
        S.act(ACP(hT[:, :, i * 128:(i + 1) * 128], PB[:, :].rearrange("p (k t) -> p k t", t=128)), ["pb"], [("hT", i)])

    def dump2(ap0, ap1=None):
        S.barrier()
        S.dma("sp", DM(dbg_d[:, 0:1024], ap0), [], ["dbg0"])
        if ap1 is not None:
            S.dma("sp", DM(dbg_d[:, 1024:2048], ap1), [], ["dbg1"])

    def dbg_is(name):
        return dbg is not None and dbg[0] == name

    stop = [False]

    for s in range(nseq):
        for l in range(nlayers):
            if stop[0]:
                break
            xin = x_d if l == 0 else xs_d
            xout = out_d if l == nlayers - 1 else xs_d
            S.barrier()
            load_bc(0, mod_d[l, s:s + 1, 1 * D:2 * D], bmod_d[l:l + 1, 1 * D:2 * D], plus1=True)
            load_bc(1, mod_d[l, s:s + 1, 0:D], bmod_d[l:l + 1, 0:D])
            for gpre in range(2):
                S.dma("pool", DM(wbf[gpre][:], win_d[l, :, gpre * 512:(gpre + 1) * 512].rearrange("(k p) n -> p k n", p=128)),
                      [], [("wbf", gpre, 0), ("wbf", gpre, 1)])
            for i in range(NT):
                S.dma("sp", DM(X[:, i, :], xin[s, i * 128:(i + 1) * 128, :]),
                      [("xs", s, i)] if l > 0 else [], [("X", i)])
                ln_stats(i, X[:, i, :], [("X", i)])
            ln_finish()
            for i in range(NT):
                sl = i % 2
                ln_apply(i, X[:, i, :], [("X", i)], lnA[sl][:], ["lnA%d" % sl], bc[0][:], [("bc", 0)],
                         bc[1][:], [("bc", 1)], htok[sl][:], [("htok", sl)])
                transpose_to_hT(i, sl)
            S.barrier()
            if dbg_is("hT"):
                S.barrier()
                S.dve(CP(lnA[0][:], hT[:, 0, 0:1024]), [], ["Xd"])
                dump2(lnA[0][:])
                stop[0] = True
                break
            S.pool(MS(vaug[:, :, :, 64:65], 1.0), [], ["vones"])

            def load_win(gidx, sl):
                S.dma("pool", DM(wbf[sl][:], win_d[l, :, gidx * 512:(gidx + 1) * 512].rearrange("(k p) n -> p k n", p=128)),
                      [], [("wbf", sl, 0), ("wbf", sl, 1)])

            for gidx in range(4):
                sl = gidx % 2
                if gidx >= 2:
                    load_win(gidx, sl)
                wkeys = [("wbf", sl, 0), ("wbf", sl, 1)]
                if gidx < 3:
                    def proj_mm(i):
                        pf = PF[i % 2]
                        pk = "pf%d" % (i % 2)
                        for k in range(8):
                            S.pe(MM(pf[:, :], hT[:, k, i * 128:(i + 1) * 128], wbf[sl][:, k, :], k == 0, k == 7),
                                 [("hT", i)] + wkeys, [pk])

                    def proj_post(i):
                        pf = PF[i % 2]
                        pk = "pf%d" % (i % 2)
                        ps3 = pf[:, :].rearrange("p (h d) -> p h d", d=64)
                        if gidx == 2:
                            S.act(ACP(vaug[:, i, :, 0:64], ps3), [pk], [("v", i)])
                            return
                        qs = i % 2
                        o3 = qktok[qs][:].rearrange("p (h d) -> p h d", d=64)
                        cb = cos_t[:, i:i + 1, :].broadcast_to([128, 8, 8])
                        sb_ = sin_t[:, i:i + 1, :].broadcast_to([128, 8, 8])
                        x1 = ps3[:, :, 0:8]
                        x2 = ps3[:, :, 8:16]
                        qk_key = ("qktok", qs)
                        S.dve(TT(rope_t[0][:], x1, cb, ALU.mult), [pk, "cos_t"], ["rt0"])
                        S.dve(TT(rope_t[1][:], x2, sb_, ALU.mult), [pk, "sin_t"], ["rt1"])
                        S.dve(TT(rope_t[2][:], x2, cb, ALU.mult), [pk, "cos_t"], ["rt2"])
                        S.dve(TT(rope_t[3][:], x1, sb_, ALU.mult), [pk, "sin_t"], ["rt3"])
                        S.pool(TT(o3[:, :, 0:8], rope_t[0][:], rope_t[1][:], ALU.subtract), ["rt0", "rt1"], [qk_key])
                        S.pool(TT(o3[:, :, 8:16], rope_t[2][:], rope_t[3][:], ALU.add), ["rt2", "rt3"], [qk_key])
                        S.act(ACP(o3[:, :, 16:64], ps3[:, :, 16:64]), [pk], [qk_key])
                        for cc in range(4):
                            S.pe(TR(PB[:, cc * 128:(cc + 1) * 128], qktok[qs][:, cc * 128:(cc + 1) * 128]),
                                 [qk_key, "ident"], ["pb"])
                        base = 4 * gidx
                        S.dve(CP(qkT[:, base:base + 4, i * 128:(i + 1) * 128],
                                 PB[:, 0:512].rearrange("p (k t) -> p k t", t=128)),
                              ["pb"], [("qk", base + cc_, i) for cc_ in range(4)])

                    proj_mm(0)
                    for i in range(1, NT):
                        proj_mm(i)
                        proj_post(i - 1)
                    proj_post(NT - 1)
                else:
                    for g in range(4):
                        w = 2 ** (g + 1)
                        S.pool(MS(pbuf[:, 0:16], 0.0), [], ["pbuf_h"])
                        S.pool(MS(tmpA[:, 0:16], 0.0), [], ["tmpA_h"])
                        S.pool(MS(tmpB[:, 0:16], 0.0), [], ["tmpB_h"])
                        for tc in range(4):
                            pf = PF[tc % 2]
                            pk = "pf%d" % (tc % 2)
                            for k in range(8):
                                S.pe(MM(pf[:, :], wbf[sl][:, k, g * 128:(g + 1) * 128], hT[:, k, tc * 512:(tc + 1) * 512],
                                        k == 0, k == 7), [("hT", 4 * tc + j) for j in range(4)] + wkeys, [pk])
                            S.act(ACP(pbuf[:, 16 + tc * 512:16 + (tc + 1) * 512], pf[:, :]), [pk], [("pbuf", tc)])
                        pkeys = ["pbuf_h"] + [("pbuf", tc) for tc in range(4)]
                        cur, curk = pbuf, pkeys
                        nxts = [(tmpA, ["tmpA_h", "tmpA"]), (tmpB, ["tmpB_h", "tmpB"])]
                        for step in range(g + 1):
                            sh = 2 ** step
                            nxt, nk = nxts[step % 2]
                            eng = "dve"
                            S.add(eng, TT(nxt[:, 16:16 + SEQ], cur[:, 16:16 + SEQ], cur[:, 16 - sh:16 - sh + SEQ], ALU.add),
                                  curk, [nk[1]])
                            cur, curk = nxt, nk
                        S.dve(TT(cur[:, 16:16 + w - 1], cur[:, 16:16 + w - 1], corr[:, g, 0:w - 1], ALU.mult),
                              curk + ["corr"], [curk[1]])
                        S.dve(STT(dT[:, g, :], cur[:, 16:16 + SEQ], float(1.0 / w), pbuf[:, 16:16 + SEQ], ALU.mult, ALU.subtract),
                              curk + pkeys, [("dT", g)])
                if dbg_is("qearly") and gidx == dbg[3]:
                    break
            S.dma("sp", DM(wpst[:], wpool_d[l].rearrange("g c e -> c g e")), [], ["wpst"])
            S.dve(CP(wpbf[:], wpst[:]), ["wpst"], ["wpbf"])
            if dbg_is("qkT") or dbg_is("qearly"):
                S.barrier()
                cidx = dbg[2]
                S.dve(CP(lnA[0][:], qkT[:, cidx, 0:1024]), [], ["Xd"])
                S.dve(CP(lnA[1][:], qkT[:, cidx, 1024:2048]), [], ["Xd"])
                dump2(lnA[0][:], lnA[1][:])
                stop[0] = True
                break
            S.barrier()
            S.pool(MS(esel[:, :, :], 1.0), [], ["esel"])
            for hb in range(2):
                ev = esel[hb * 64:(hb + 1) * 64, :, :]
                S.pool((lambda ev=ev: lambda e: e.affine_select(out=ev, in_=ev, pattern=[[-1, 64], [0, 128]],
                                                                compare_op=ALU.is_equal, fill=0.0, base=0,
                                                                channel_multiplier=1))(), ["esel"], ["esel"])
            S.dve(MS(KB32[:], 0.0), [], ["KB32"])
            for c in range(4):
                for hh in range(2):
                    h = 2 * c + hh
                    S.dve(TRD(KB32[hh * 64:(hh + 1) * 64, c, h * 8:(h + 1) * 8],
                              qkT[hh * 64:(hh + 1) * 64, 4 + c, :].rearrange("p (n k) -> p n k", k=256), ALU.add),
                          [], ["KB32"])
            S.dve(CP(KB[:], KB32[:]), ["KB32"], ["KB"])
            for i in range(8, NT):
                j = i // 2
                jj = j - 4
                for c in range(4):
                    S.pe(MM(PF[6][:, 0:64], qkT[:, c, i * 128:(i + 1) * 128], KB[:, c, :], c == 0, c == 3), ["KB"], ["pf6"])
                S.dve(TT(gm[:], PF[6][:, 0:64], pastm[:, jj, :], ALU.add), ["pf6", "pastm"], ["gm"])
                for h in range(8):
                    S.dve(MAX8(srt[:, h * 8:(h + 1) * 8], gm[:, h * 8:(h + 1) * 8]), ["gm"], ["srt"])
                gm3 = gm[:].rearrange("p (h n) -> p h n", n=8)
                srt3 = srt[:].rearrange("p (h n) -> p h n", n=8)
                sel3 = selt[:].rearrange("p (h n) -> p h n", n=8)
                bt3 = btok[:, 0:64].rearrange("p (h n) -> p h n", n=8)
                S.dve(TT(sel3, gm3, srt3[:, :, 2:3].broadcast_to([128, 8, 8]), ALU.is_ge), ["gm", "srt"], ["selt"])
                S.dve(TS(btok[:, 0:64], selt[:], -1.0, BIGB, ALU.add, ALU.mult), ["selt"], ["btok"])
                S.dve(MS(bt3[:, :, j:j + 1], 0.0), ["btok"], ["btok"])
                S.dve(CP(btok[:, 64:128], btok[:, 0:64]), ["btok"], ["btok"])
                S.pe(TR(PB[:, 0:128], btok[:, :]), ["btok", "ident"], ["pb"])
                S.act(ACP(biasT[:, (i - 8) * 128:(i - 7) * 128], PB[:, 0:128]), ["pb"], [("biasT", i)])
            load_bc(3, mod_d[l, s:s + 1, 2 * D:3 * D], bmod_d[l:l + 1, 2 * D:3 * D])
            for pc in range(4):
                S.dma("sp", DM(stg[pc % 2][:].rearrange("p (k n) -> p k n", n=D),
                               wout_d[l, 256 * pc:256 * pc + 256, :].rearrange("(k p) n -> p k n", p=128)),
                      [], [("stg", pc % 2)])
                for kk in range(2):
                    S.dve(TT(wo[:, 2 * pc + kk, :], stg[pc % 2][:, kk * D:(kk + 1) * D], bc[3][:], ALU.mult),
                          [("stg", pc % 2), ("bc", 3)], [("wo", 2 * pc + kk)])
            wokeys = [("wo", k) for k in range(8)]
            load_bc(1, ln1g_d[l:l + 1, :])
            load_bc(2, ln1b_d[l:l + 1, :])
            load_bc(0, mod_d[l, s:s + 1, 4 * D:5 * D], bmod_d[l:l + 1, 4 * D:5 * D], plus1=True)
            load_bc(3, mod_d[l, s:s + 1, 3 * D:4 * D], bmod_d[l:l + 1, 3 * D:4 * D])
            if dbg_is("a3"):
                S.barrier()
                S.dve(CP(lnA[0][0:64, :], biasT[0:64, :]), [], ["Xd"])
                dump2(lnA[0][:])
                stop[0] = True
                break
            items = []
            for c in range(4):
                for qc in range(4):
                    for hh in range(2):
                        for kt in range(4 * qc + 4):
                            items.append((c, qc, hh, kt))

            def emit_qk(idx):
                c, qc, hh, kt = items[idx]
                h = 2 * c + hh
                r0 = hh * 64
                n = kt // 2
                q0 = qc * 512 if kt < 4 * qc else kt * 128
                q1 = (qc + 1) * 512
                nq = q1 - q0
                qtiles = list(range(q0 // 128, q1 // 128))
                sc = PF[idx % 2]
                sck = "pf%d" % (idx % 2)
                pr = probs[idx % 3]
                prk = ("probs", idx % 3)
                need_bias = qc >= 2
                S.pe(MM(sc[:, 0:nq], qkT[r0:r0 + 64, 4 + c, kt * 128:(kt + 1) * 128],
                        qkT[r0:r0 + 64, c, q0:q1], True, not need_bias),
                     [("qk", 4 + c, kt)] + [("qk", c, t) for t in qtiles], [sck])
                if need_bias:
                    S.pe(MM(sc[:, 0:nq], esel[r0:r0 + 64, h * 8 + n, :],
                            biasT[r0:r0 + 64, q0 - 1024:q1 - 1024], False, True),
                         [("biasT", t) for t in qtiles] + ["esel"], [sck])
                S.act(AV(pr[:, 0:nq], sc[:, 0:nq], AF.Exp, scale=0.125), [sck], [prk])
                if kt >= 4 * qc:
                    S.dve(TT(pr[:, 0:128], pr[:, 0:128], tri[:], ALU.mult), [prk, "tri"], [prk])

            def emit_pv(idx):
                c, qc, hh, kt = items[idx]
                h = 2 * c + hh
                q0 = qc * 512 if kt < 4 * qc else kt * 128
                q1 = (qc + 1) * 512
                qt0 = q0 // 128
                pr = probs[idx % 3]
                prk = ("probs", idx % 3)
                ap_sl = (c * 4 + qc) % 2
                gpar = ((c * 4 + qc) * 2 + hh) % 2
                accb = PF[2 + gpar]
                for qt in range(qt0, q1 // 128):
                    qq = qt - 4 * qc
                    acck = ("acc", gpar, qq)
                    S.pe(MM(accb[:, qq * 128:qq * 128 + 65], pr[:, (qt - qt0) * 128:(qt - qt0 + 1) * 128], vaug[:, kt, h, :],
                            kt == 0 and qt == qt0, kt == 4 * qc + 3), [prk, ("v", kt), "vones"], [acck])
                if kt == 4 * qc + 3:
                    for qq in range(4):
                        accks = [("acc", gpar, q_) for q_ in range(4)]
                        S.dve(RCP(rcp[:, qq:qq + 1], accb[:, qq * 128 + 64:qq * 128 + 65]), accks, [("rcp", qq)])
                        S.dve(TS(apair[ap_sl][:, qq, hh * 64:(hh + 1) * 64], accb[:, qq * 128:qq * 128 + 64], rcp[:, qq:qq + 1],
                                 None, ALU.mult), accks + [("rcp", qq)], [("apair", ap_sl, qq)])
                    if hh == 1:
                        for qq in range(4):
                            S.pe(TR(PB[:, qq * 128:(qq + 1) * 128], apair[ap_sl][:, qq, :]),
                                 [("apair", ap_sl, qq), "ident"], ["pb"])
                        S.act(ACP(qkT[:, c, qc * 512:(qc + 1) * 512], PB[:, 0:512]), ["pb"],
                              [("qk", c, 4 * qc + t) for t in range(4)])

            emit_qk(0)
            for idx in range(1, len(items)):
                emit_qk(idx)
                emit_pv(idx - 1)
            emit_pv(len(items) - 1)
            if dbg_is("a4"):
                S.barrier()
                S.dve(CP(lnA[0][:], qkT[:, dbg[2], 0:1024]), [], ["Xd"])
                S.dve(CP(lnA[1][:], qkT[:, dbg[2], 1024:2048]), [], ["Xd"])
                dump2(lnA[0][:], lnA[1][:])
                stop[0] = True
                break
            for g in range(4):
                for tc in range(4):
                    pf = PF[tc % 2]
                    pk = "pf%d" % (tc % 2)
                    S.pe(MM(pf[:, :], wpbf[:, g, :], dT[:, g, tc * 512:(tc + 1) * 512], True, True), ["wpbf", ("dT", g)], [pk])
                    S.act(AV(qkT[:, 4 + g, tc * 512:(tc + 1) * 512], pf[:, :], AF.Identity, scale=pscol[:, l, g:g + 1]),
                          [pk, ("pscol", l)], [("qk", 4 + g, 4 * tc + t) for t in range(4)])
            if dbg_is("amT"):
                S.barrier()
                S.dve(CP(lnA[0][:], qkT[:, dbg[2], 0:1024]), [], ["Xd"])
                S.dve(CP(lnA[1][:], qkT[:, dbg[2], 1024:2048]), [], ["Xd"])
                dump2(lnA[0][:], lnA[1][:])
                stop[0] = True
                break
            S.barrier()
            for i in range(NT):
                S.dma("sp", DM(X[:, i, :], xin[s, i * 128:(i + 1) * 128, :]),
                      [("xs", s, i)] if l > 0 else [], [("X", i)])
            for i in range(NT):
                for half in range(2):
                    pf = PF[4 + half]
                    pk = "pf%d" % (4 + half)
                    for k in range(8):
                        S.pe(MM(pf[:, :], qkT[:, k, i * 128:(i + 1) * 128], wo[:, k, half * 512:(half + 1) * 512], k == 0, k == 7),
                             wokeys, [pk])
                    S.dve(STT(X[:, i, half * 512:(half + 1) * 512], X[:, i, half * 512:(half + 1) * 512], ALPHA,
                              pf[:, :], ALU.mult, ALU.add), [pk, ("X", i)], [("X", i)])
                ln_stats(i, X[:, i, :], [("X", i)])
            ln_finish()
            for i in range(NT):
                sl = i % 2
                ln_apply(i, X[:, i, :], [("X", i)], lnA[sl][:], ["lnA%d" % sl], bc[1][:], [("bc", 1)],
                         bc[2][:], [("bc", 2)], X[:, i, :], [("X", i)])
                ln_stats(i, X[:, i, :], [("X", i)])
            ln_finish()
            for i in range(NT):
                sl = i % 2
                ln_apply(i, X[:, i, :], [("X", i)], lnA[sl][:], ["lnA%d" % sl], bc[0][:], [("bc", 0)],
                         bc[3][:], [("bc", 3)], htok[sl][:], [("htok", sl)])
                transpose_to_hT(i, sl)
            if dbg_is("X1"):
                S.barrier()
                dump2(X[:, dbg[2], :])
                stop[0] = True
                break
            for tg in range(4):
                lg = PF[6]
                for tt in range(4):
                    i = 4 * tg + tt
                    for k in range(8):
                        S.pe(MM(lg[:, tt * 16:(tt + 1) * 16], hT[:, k, i * 128:(i + 1) * 128], wrb[:, k, :], k == 0, k == 7),
                             [("hT", i), "wrb"], ["pf6"])

                def v3(t, a):
                    return t[:].rearrange("p (a b) -> p a b", a=a)

                def bcl(t, a):
                    return t[:, 0:a].rearrange("p (a o) -> p a o", o=1).broadcast_to([128, a, 64 // a])

                R_ = lambda k: ("rt", k)
                rb_keys = [("rb4", i_) for i_ in range(4)]
                lg3 = lg[:, 0:64].rearrange("p (a b) -> p a b", a=4)
                S.dve(TRD(rt[0][:, 0:4], lg3, ALU.max), ["pf6"], [R_(0)])
                S.dve(TT(v3(rt[1], 4), lg3, bcl(rt[0], 4), ALU.subtract), ["pf6", R_(0)], [R_(1)])
                S.act(AV(rt[1][:], rt[1][:], AF.Exp), [R_(1)], [R_(1)])
                S.dve(TRD(rt[0][:, 0:4], v3(rt[1], 4), ALU.add), [R_(1)], [R_(0)])
                S.dve(RCP(rt[0][:, 0:4], rt[0][:, 0:4]), [R_(0)], [R_(0)])
                S.dve(TT(v3(rt[2], 4), v3(rt[1], 4), bcl(rt[0], 4), ALU.mult), [R_(1), R_(0)], [R_(2)])
                S.dve(TT(rt[3][:], rt[2][:], rb4[:].rearrange("p a b -> p (a b)"), ALU.add), [R_(2)] + rb_keys, [R_(3)])
                S.dve(TRD(rt[4][:, 0:16], v3(rt[3], 16), ALU.max), [R_(3)], [R_(4)])
                S.dve(TT(v3(rt[5], 16), v3(rt[3], 16), bcl(rt[4], 16), ALU.is_equal), [R_(3), R_(4)], [R_(5)])
                S.dve(STT(rt[5][:], rt[5][:], NEG, rt[3][:], ALU.mult, ALU.add), [R_(5), R_(3)], [R_(5)])
                S.dve(TRD(rt[6][:, 0:16], v3(rt[5], 16), ALU.max), [R_(5)], [R_(6)])
                S.dve(TT(rt[4][:, 0:16], rt[4][:, 0:16], rt[6][:, 0:16], ALU.add), [R_(4), R_(6)], [R_(4)])
                gs3 = rt[4][:, 0:16].rearrange("p (a b) -> p a b", a=4)
                S.dve(TRD(rt[6][:, 0:4], gs3, ALU.max), [R_(4)], [R_(6)])
                S.dve(TT(rt[7][:, 0:16].rearrange("p (a b) -> p a b", a=4), gs3,
                         rt[6][:, 0:4].rearrange("p (a o) -> p a o", o=1).broadcast_to([128, 4, 4]), ALU.is_equal),
                      [R_(4), R_(6)], [R_(7)])
                S.dve(TS(rt[7][:, 0:16], rt[7][:, 0:16], -1.0, -NEG, ALU.add, ALU.mult), [R_(7)], [R_(7)])
                S.dve(TT(v3(rt[8], 16), v3(rt[3], 16), bcl(rt[7], 16), ALU.add), [R_(3), R_(7)], [R_(8)])
                S.dve(TRD(rt[6][:, 0:4], v3(rt[8], 4), ALU.max), [R_(8)], [R_(6)])
                S.dve(TT(v3(rt[9], 4), v3(rt[8], 4), bcl(rt[6], 4), ALU.is_equal), [R_(8), R_(6)], [R_(9)])
                S.dve(STT(rt[8][:], rt[9][:], NEG, rt[8][:], ALU.mult, ALU.add), [R_(9), R_(8)], [R_(8)])
                S.dve(TRD(rt[6][:, 0:4], v3(rt[8], 4), ALU.max), [R_(8)], [R_(6)])
                S.dve(TT(v3(rt[10], 4), v3(rt[8], 4), bcl(rt[6], 4), ALU.is_equal), [R_(8), R_(6)], [R_(10)])
                S.dve(TT(rt[9][:], rt[9][:], rt[10][:], ALU.add), [R_(9), R_(10)], [R_(9)])
                S.dve(TT(rt[9][:], rt[9][:], rt[2][:], ALU.mult), [R_(9), R_(2)], [R_(9)])
                S.dve(TRD(rt[6][:, 0:4], v3(rt[9], 4), ALU.add), [R_(9)], [R_(6)])
                S.dve(RCP(rt[6][:, 0:4], rt[6][:, 0:4]), [R_(6)], [R_(6)])
                S.dve(TS(rt[6][:, 0:4], rt[6][:, 0:4], float(1.0 / ALPHA), None, ALU.mult), [R_(6)], [R_(6)])
                S.dve(TT(gates[:, 4 * tg:4 * tg + 4, :], v3(rt[9], 4), bcl(rt[6], 4), ALU.mult), [R_(9), R_(6)], [("gates", tg)])
            if dbg_is("gates"):
                S.barrier()
                S.dve(CP(lnA[0][:, 0:256], gates[:].rearrange("p a b -> p (a b)")), [], ["Xd"])
                dump2(lnA[0][:])
                stop[0] = True
                break
            S.barrier()
            load_bc(0, mod_d[l, s:s + 1, 5 * D:6 * D], bmod_d[l:l + 1, 5 * D:6 * D])
            load_bc(1, ln2g_d[l:l + 1, :])
            load_bc(2, ln2b_d[l:l + 1, :])
            nexp = 1 if dbg_is("moe1") else NE

            def load_expert_dma(e_):
                sl = e_ % 2
                for gu, wsrc in ((0, wg_d), (1, wu_d)):
                    S.dma("pool", DM(wgu[sl][:, :, gu, :], wsrc[l, e_].rearrange("(k p) n -> p k n", p=128)),
                          [], [("wgu", sl, gu, 0), ("wgu", sl, gu, 1)])
                for h in range(2):
                    S.dma("sp", DM(stg[h][:].rearrange("p (k n) -> p k n", n=D),
                                   wd_d[l, e_, 256 * h:256 * h + 256, :].rearrange("(k p) n -> p k n", p=128)),
                          [], [("stg", h)])

            def load_expert_cast(e_):
                sl = e_ % 2
                for h in range(2):
                    for kk in range(2):
                        S.dve(TT(wdb[sl][:, 2 * h + kk, :], stg[h][:, kk * D:(kk + 1) * D], bc[0][:], ALU.mult),
                              [("stg", h), ("bc", 0)], [("wd", sl, 2 * h + kk)])

            stages = [(e_, tc) for e_ in range(nexp) for tc in range(4)]

            def emit_gu(si):
                e_, tc = stages[si]
                sl = e_ % 2
                asl = si % 2
                for f in range(4):
                    Gp, Gk = PF[f % 2], "pf%d" % (f % 2)
                    Up, Uk = PF[2 + f % 2], "pf%d" % (2 + f % 2)
                    for gu, pp, pk in ((0, Gp, Gk), (1, Up, Uk)):
                        for k in range(8):
                            S.pe(MM(pp[:, :], wgu[sl][:, k, gu, f * 128:(f + 1) * 128], hT[:, k, tc * 512:(tc + 1) * 512],
                                    k == 0, k == 7),
                                 [("wgu", sl, gu, 0), ("wgu", sl, gu, 1)] + [("hT", 4 * tc + j) for j in range(4)], [pk])
                    S.act(AV(sg[f % 2][:], Gp[:, :], AF.Silu), [Gk], [("sg", f % 2)])
                    S.dve(TT(aT[asl][:, f, :], Up[:, :], sg[f % 2][:], ALU.mult), [Uk, ("sg", f % 2)], [("aT", asl, f)])

            def emit_y(si):
                e_, tc = stages[si]
                sl = e_ % 2
                asl = si % 2
                for tt in range(4):
                    i = 4 * tc + tt
                    for half in range(2):
                        Yp, Yk = PF[4 + half], "pf%d" % (4 + half)
                        for f in range(4):
                            S.pe(MM(Yp[:, :], aT[asl][:, f, tt * 128:(tt + 1) * 128], wdb[sl][:, f, half * 512:(half + 1) * 512],
                                    f == 0, f == 3), [("aT", asl, f), ("wd", sl, f)], [Yk])
                        S.dve(STT(X[:, i, half * 512:(half + 1) * 512], Yp[:, :], gates[:, i, e_:e_ + 1],
                                  X[:, i, half * 512:(half + 1) * 512], ALU.mult, ALU.add),
                              [Yk, ("gates", i // 4), ("X", i)], [("X", i)])

            load_expert_dma(0)
            load_expert_cast(0)
            for si in range(len(stages)):
                e_, tc = stages[si]
                emit_gu(si)
                if si > 0:
                    emit_y(si - 1)
                if tc == 0 and e_ + 1 < nexp:
                    load_expert_dma(e_ + 1)
                if tc == 2 and e_ + 1 < nexp:
                    load_expert_cast(e_ + 1)
            emit_y(len(stages) - 1)
            if dbg_is("moe1") or dbg_is("X2"):
                S.barrier()
                dump2(X[:, dbg[2], :])
                stop[0] = True
                break
            for i in range(NT):
                ln_stats(i, X[:, i, :], [("X", i)])
            ln_finish(float(EPS / (ALPHA * ALPHA)))
            for i in range(NT):
                sl = i % 2
                ln_apply(i, X[:, i, :], [("X", i)], lnA[sl][:], ["lnA%d" % sl], bc[1][:], [("bc", 1)],
                         bc[2][:], [("bc", 2)], lnA[sl][:], ["lnA%d" % sl])
                S.dma("pool", DM(xout[s, i * 128:(i + 1) * 128, :], lnA[sl][:]),
                      ["lnA%d" % sl], [("xs", s, i)] if xout is xs_d else [("out", s, i)])
        if stop[0]:
            break
    S.barrier()
    S.emit(nc)
    return nc


_NC_CACHE = {}


def kernel(**inputs):
    if "nc" not in _NC_CACHE:
        _NC_CACHE["nc"] = build()
    nc = _NC_CACHE["nc"]
    f = lambda a: np.ascontiguousarray(np.asarray(a, dtype=np.float32))
    x = f(inputs["x"])
    c = f(inputs["c"])
    shared = {k: f(inputs[k]) for k in ("w_mod", "b_mod", "w_in", "w_pool", "pool_scale", "w_out", "ln1_g", "ln1_b",
                                        "w_router", "w_gate", "w_up", "w_down", "ln2_g", "ln2_b")}
    shared["router_bias"] = f(inputs["router_bias"]).reshape(1, NE)
    in_maps = []
    for core in range(8):
        m = dict(shared)
        m["x"] = x[2 * core:2 * core + 2]
        m["c"] = c[2 * core:2 * core + 2]
        in_maps.append(m)
    res = run_bass_kernel_spmd(nc, in_maps, core_ids=list(range(8)))
    return np.concatenate([r["out"] for r in res.results], axis=0)
```

```python
import contextlib
import math
import numpy as np
import concourse.bass as bass
import concourse.mybir as mybir
from concourse.bass_utils import run_bass_kernel_spmd

F32 = mybir.dt.float32
BF16 = mybir.dt.bfloat16
I32 = mybir.dt.int32
AF = mybir.ActivationFunctionType
ALU = mybir.AluOpType
AX = mybir.AxisListType

ENGS = ["pe", "act", "dve", "pool", "sp"]
_EMBED_WAIT = False
DMA_RING = {"sp": 8, "act": 4, "pool": 6}


class Op:
    __slots__ = ("idx", "eng", "fn", "reads", "writes", "dma", "deps", "signal",
                 "sig", "ring_wait", "extra", "is_bar")

    def __init__(self, idx, eng, fn, reads, writes, dma):
        self.idx = idx
        self.eng = eng
        self.fn = fn
        self.reads = tuple(reads)
        self.writes = tuple(writes)
        self.dma = dma
        self.deps = ()
        self.signal = False
        self.sig = None
        self.ring_wait = None
        self.extra = ()
        self.is_bar = False


class Sched:
    def __init__(self):
        self.ops = []

    def add(self, eng, fn, reads=(), writes=(), dma=False):
        op = Op(len(self.ops), eng, fn, reads, writes, dma)
        self.ops.append(op)
        return op

    def pe(self, fn, reads=(), writes=()):
        return self.add("pe", fn, reads, writes)

    def act(self, fn, reads=(), writes=()):
        return self.add("act", fn, reads, writes)

    def dve(self, fn, reads=(), writes=()):
        return self.add("dve", fn, reads, writes)

    def pool(self, fn, reads=(), writes=()):
        return self.add("pool", fn, reads, writes)

    def dma(self, q, fn, reads=(), writes=()):
        return self.add(q, fn, reads, writes, dma=True)

    def barrier(self):
        last = {}
        dmas = {q: [] for q in DMA_RING}
        for op in self.ops:
            if op.is_bar:
                continue
            if op.dma:
                dmas[op.eng].append(op.idx)
            elif op.fn is not None:
                last[op.eng] = op.idx
        extra = list(last.values())
        for q, lst in dmas.items():
            extra.extend(lst[-DMA_RING[q]:])
        for e in ENGS:
            op = self.add(e, None)
            op.extra = tuple(extra)
            op.is_bar = True

    def analyze(self):
        last_w = {}
        readers = {}
        for op in self.ops:
            if op.is_bar:
                op.deps = tuple(sorted(d for d in op.extra
                                       if not (self.ops[d].eng == "pe" and op.eng == "pe")))
                continue
            deps = set()
            for k in op.reads:
                w = last_w.get(k)
                if w is not None:
                    deps.add(w)
            for k in op.writes:
                w = last_w.get(k)
                if w is not None:
                    deps.add(w)
                deps.update(readers.get(k, {}).values())
            deps.discard(op.idx)
            pruned = []
            for d in deps:
                p = self.ops[d]
                if p.eng == "pe" and op.eng == "pe" and not p.dma and not op.dma:
                    continue
                pruned.append(d)
            op.deps = tuple(sorted(pruned))
            for k in op.reads:
                rk = (op.eng, op.idx) if op.dma else op.eng
                readers.setdefault(k, {})[rk] = op.idx
            for k in op.writes:
                last_w[k] = op.idx
                readers[k] = {}
        for op in self.ops:
            for d in op.deps:
                self.ops[d].signal = True
        cnt = {e: 0 for e in ENGS}
        dcnt = {}
        dnum = {q: 0 for q in DMA_RING}
        for op in self.ops:
            if op.dma:
                q = op.eng
                i = dnum[q]
                dnum[q] += 1
                key = ("d", q, i % DMA_RING[q])
                prev = dcnt.get(key, 0)
                if prev > 0:
                    op.ring_wait = (key, prev)
                dcnt[key] = prev + 16
                op.sig = (key, prev + 16)
                op.signal = True
            elif op.signal:
                cnt[op.eng] += 1
                op.sig = (("c", op.eng), cnt[op.eng])

    def emit(self, nc):
        self.analyze()
        streams = {e: [o for o in self.ops if o.eng == e] for e in ENGS}
        with contextlib.ExitStack() as st:
            sems = {}
            for e in ENGS:
                sems[("c", e)] = st.enter_context(nc.semaphore("c_" + e))
            for q, n in DMA_RING.items():
                for i in range(n):
                    sems[("d", q, i)] = st.enter_context(nc.semaphore("d_%s%d" % (q, i)))
            block = st.enter_context(nc.Block())
            ops = self.ops

            def run(ename, eng):
                known = {}
                for op in streams[ename]:
                    waits = []
                    if op.ring_wait is not None:
                        waits.append(op.ring_wait)
                    for d in op.deps:
                        waits.append(ops[d].sig)
                    best = {}
                    for k, v in waits:
                        if v > best.get(k, 0):
                            best[k] = v
                    pend = []
                    for k, v in best.items():
                        if known.get(k, 0) >= v:
                            continue
                        pend.append((k, v))
                        known[k] = v
                    attach = None
                    if _EMBED_WAIT and pend and op.fn is not None and not op.dma:
                        attach = pend.pop()
                    for k, v in pend:
                        eng.wait_ge(sems[k], v)
                    if op.fn is None:
                        continue
                    ins = op.fn(eng)
                    if attach is not None:
                        ins._wait_ge(sems[attach[0]], attach[1])
                    if op.signal:
                        assert ins is not None
                        ins.then_inc(sems[op.sig[0]], 16 if op.dma else 1)

            @block.tensor
            def _(e):
                run("pe", e)

            @block.scalar
            def _(e):
                run("act", e)

            @block.vector
            def _(e):
                run("dve", e)

            @block.gpsimd
            def _(e):
                run("pool", e)

            @block.sync
            def _(e):
                run("sp", e)


D = 1024
SEQ = 2048
NT = 16
NL = 2
NE = 16
ALPHA = float(4.0 ** 0.25)
EPS = 1e-5
NEG = -1.0e30
BIGB = 30000.0
INVF = [float(np.float32(500000.0) ** np.float32(-(i * 2.0 / 16.0))) for i in range(8)]
TWO_PI = float(2 * np.pi)
PI = float(np.pi)


def build(nseq=2, nlayers=NL, dbg=None):
    nc = bass.Bass("TRN2", target_bir_lowering=False)

    def din(name, shape):
        return nc.dram_tensor(name, shape, F32, kind="ExternalInput").ap()

    x_d = din("x", [2, SEQ, D])
    c_d = din("c", [2, D])
    wmod_d = din("w_mod", [NL, D, 6 * D])
    bmod_d = din("b_mod", [NL, 6 * D])
    win_d = din("w_in", [NL, D, 2048])
    wpool_d = din("w_pool", [NL, 4, 128, 128])
    pscale_d = din("pool_scale", [NL, 512])
    wout_d = din("w_out", [NL, D, D])
    ln1g_d = din("ln1_g", [NL, D])
    ln1b_d = din("ln1_b", [NL, D])
    wr_d = din("w_router", [D, NE])
    rb_d = din("router_bias", [1, NE])
    wg_d = din("w_gate", [NL, NE, D, 512])
    wu_d = din("w_up", [NL, NE, D, 512])
    wd_d = din("w_down", [NL, NE, 512, D])
    ln2g_d = din("ln2_g", [NL, D])
    ln2b_d = din("ln2_b", [NL, D])
    out_d = nc.dram_tensor("out", [2, SEQ, D], F32, kind="ExternalOutput").ap()
    xs_d = nc.dram_tensor("xs_scr", [2, SEQ, D], F32, kind="Internal").ap()
    mod_d = nc.dram_tensor("mod_scr", [NL, 2, 6 * D], F32, kind="Internal").ap()
    dbg_d = None
    if dbg is not None:
        dbg_d = nc.dram_tensor("dbg", [128, dbg[1]], F32, kind="ExternalOutput").ap()

    S = Sched()

    B0 = 16640
    LIMIT = 229376

    def at(name, shape, dt, off):
        assert off % 32 == 0, (name, off)
        nb = int(np.prod(shape[1:])) * (4 if dt in (F32, I32) else 2)
        assert off + nb <= LIMIT, (name, off, nb)
        return nc.alloc_sbuf_tensor_at(name, shape, dt, offset=off)

    hT = at("hT", [128, 8, SEQ], BF16, B0)
    R = B0 + 32768
    qkT = at("qkT", [128, 8, SEQ], BF16, R)
    wbf = [at("wbf%d" % i, [128, 8, 512], BF16, R + 32768 + 8192 * i) for i in range(2)]
    stg = [at("stg%d" % i, [128, 2048], F32, R + 49152 + 8192 * i) for i in range(2)]
    wo = at("wo", [128, 8, D], BF16, R + 32768)
    vaug = at("vaug", [128, NT, 8, 65], BF16, R + 65536)
    dT = at("dT", [128, 4, SEQ], BF16, R + 82176)
    pbuf = at("pbuf", [128, 16 + SEQ], F32, R + 98560)
    tmpA = at("tmpA", [128, 16 + SEQ], F32, R + 106816)
    tmpB = at("tmpB", [128, 16 + SEQ], F32, R + 115072)
    probs = [at("probs%d" % i, [128, 512], BF16, R + 98560 + 1024 * i) for i in range(3)]
    biasT = at("biasT", [128, 1024], BF16, R + 101632)
    apair = [at("apair%d" % i, [128, 4, 128], BF16, R + 103680 + 1024 * i) for i in range(2)]
    qktok = [at("qktok%d" % i, [128, 512], BF16, R + 123328 + 1024 * i) for i in range(2)]
    esel = at("esel", [128, 64, 128], BF16, R + 106816)
    wpst = at("wpst", [128, 4, 128], F32, R + 125376)
    wpbf = at("wpbf", [128, 4, 128], BF16, R + 127424)
    wgu = [at("wgu%d" % i, [128, 8, 2, 512], BF16, R + 16384 * i) for i in range(2)]
    wdb = [at("wdb%d" % i, [128, 4, D], BF16, R + 32768 + 8192 * i) for i in range(2)]
    X = at("X", [128, NT, D], F32, R + 65536)
    Pp = R + 131072
    lnA = [at("lnA%d" % i, [128, D], F32, Pp + 4096 * i) for i in range(2)]
    htok = [at("htok%d" % i, [128, D], BF16, Pp + 8192 + 2048 * i) for i in range(2)]
    bc = [at("bc%d" % i, [128, D], F32, Pp + 12288 + 4096 * i) for i in range(4)]
    aT = [at("aT%d" % i, [128, 4, 512], BF16, Pp + 28672 + 4096 * i) for i in range(2)]
    sg = [at("sg%d" % i, [128, 512], F32, Pp + 36864 + 2048 * i) for i in range(2)]
    cpos = [Pp + 40960]

    def small(name, shape, dt):
        nb = int(np.prod(shape[1:])) * (4 if dt in (F32, I32) else 2)
        nb = (nb + 31) // 32 * 32
        t = at(name, shape, dt, cpos[0])
        cpos[0] += nb
        return t

    ident = small("ident", [128, 128], BF16)
    tri = small("tri", [128, 128], BF16)
    cos_t = small("cos_t", [128, NT, 8], F32)
    sin_t = small("sin_t", [128, NT, 8], F32)
    pastm = small("pastm", [128, 4, 64], F32)
    corr = small("corr", [128, 4, 16], F32)
    rb4 = small("rb4", [128, 4, NE], F32)
    wrb = small("wrb", [128, 8, NE], BF16)
    wrst = small("wrst", [128, 8, NE], F32)
    pscol = small("pscol", [128, NL, 4], F32)
    condT = small("condT", [128, 8, 2], F32)
    st6 = [small("st6_%d" % i, [128, 12], F32) for i in range(2)]
    mv = [small("mv%d" % i, [128, 2], F32) for i in range(2)]
    rstd = [small("rstd%d" % i, [128, 1], F32) for i in range(2)]
    nmr = [small("nmr%d" % i, [128, 1], F32) for i in range(2)]
    rcp = small("rcp", [128, 4], F32)
    gates = small("gates", [128, NT, NE], F32)
    rtb = Pp + 28672
    rt = [at("rt%d" % i, [128, 64], F32, rtb + 256 * i) for i in range(14)]
    rope_t = [at("rope_t%d" % i, [128, 8, 8], F32, rtb + 4096 + 256 * i) for i in range(4)]
    KB32 = at("KB32", [128, 4, 64], F32, rtb + 5120)
    KB = at("KB", [128, 4, 64], BF16, rtb + 6144)
    gm = at("gm", [128, 64], F32, rtb + 6656)
    srt = at("srt", [128, 64], F32, rtb + 6912)
    selt = at("selt", [128, 64], F32, rtb + 7168)
    btok = at("btok", [128, 128], BF16, rtb + 7424)
    modrow = at("modrow", [2, 512], F32, Pp + 36864)

    stA = small("stA", [128, NT, 12], F32)
    mvA = small("mvA", [128, NT, 2], F32)
    rsA = small("rsA", [128, NT], F32)
    nmA = small("nmA", [128, NT], F32)
    PF = [nc.alloc_psum_tensor("pf%d" % i, [128, 512], F32) for i in range(7)]
    PB = nc.alloc_psum_tensor("pb", [128, 1024], BF16)

    def MM(out, lhsT, rhs, st, sp):
        return lambda e: e.matmul(out, lhsT=lhsT, rhs=rhs, start=st, stop=sp)

    def TR(out, in_):
        idn = ident[:]
        return lambda e: e.transpose(out=out, in_=in_, identity=idn)

    def TT(out, in0, in1, op):
        return lambda e: e.tensor_tensor(out=out, in0=in0, in1=in1, op=op)

    def TS(out, in0, s1, s2, op0, op1=None):
        if op1 is None:
            return lambda e: e.tensor_scalar(out=out, in0=in0, scalar1=s1, scalar2=None, op0=op0)
        return lambda e: e.tensor_scalar(out=out, in0=in0, scalar1=s1, scalar2=s2, op0=op0, op1=op1)

    def STT(out, in0, scalar, in1, op0, op1):
        return lambda e: e.scalar_tensor_tensor(out=out, in0=in0, scalar=scalar, in1=in1, op0=op0, op1=op1)

    def CP(out, in_):
        return lambda e: e.tensor_copy(out=out, in_=in_)

    def ACP(out, in_):
        return lambda e: e.copy(out=out, in_=in_)

    def AV(out, in_, func, bias=None, scale=None):
        kw = {}
        if bias is not None:
            kw["bias"] = bias
        if scale is not None:
            kw["scale"] = scale
        return lambda e: e.activation(out=out, in_=in_, func=func, **kw)

    def DM(out, in_, **kw):
        return lambda e: e.dma_start(out=out, in_=in_, **kw)

    def MS(ap, val):
        return lambda e: e.memset(ap, val)

    def TRD(out, in_, op):
        return lambda e: e.tensor_reduce(out=out, in_=in_, axis=AX.X, op=op)

    def RCP(out, in_):
        return lambda e: e.reciprocal(out=out, in_=in_)

    def MAX8(out, in_):
        return lambda e: e.max(out=out, in_=in_)

    def range_reduce(a, t_i, t_f, t_m):
        S.dve(TS(t_f, a, float(1.0 / TWO_PI), None, ALU.mult), ["ang"], ["rr_f"])
        S.dve(CP(t_i, t_f), ["rr_f"], ["rr_i"])
        S.dve(CP(t_f, t_i), ["rr_i"], ["rr_f"])
        S.dve(STT(a, t_f, -TWO_PI, a, ALU.mult, ALU.add), ["rr_f", "ang"], ["ang"])
        S.dve(TS(t_m, a, PI, None, ALU.is_gt), ["ang"], ["rr_m"])
        S.dve(STT(a, t_m, -TWO_PI, a, ALU.mult, ALU.add), ["rr_m", "ang"], ["ang"])
        S.dve(TS(t_m, a, -PI, None, ALU.is_lt), ["ang"], ["rr_m"])
        S.dve(STT(a, t_m, TWO_PI, a, ALU.mult, ALU.add), ["rr_m", "ang"], ["ang"])

    S.pool(MS(ident[:], 1.0), [], ["ident"])
    S.pool(lambda e: e.affine_select(out=ident[:], in_=ident[:], pattern=[[-1, 128]], compare_op=ALU.is_equal,
                                     fill=0.0, base=0, channel_multiplier=1), ["ident"], ["ident"])
    S.pool(MS(tri[:], 1.0), [], ["tri"])
    S.pool(lambda e: e.affine_select(out=tri[:], in_=tri[:], pattern=[[1, 128]], compare_op=ALU.is_ge,
                                     fill=0.0, base=0, channel_multiplier=-1), ["tri"], ["tri"])
    posi = at("posi", [128, NT], I32, Pp)
    posf = at("posf", [128, NT], F32, Pp + 64)
    angc = at("angc", [128, NT * 8], F32, Pp + 128)
    angs = at("angs", [128, NT * 8], F32, Pp + 128 + 512)
    rr_i = at("rr_i", [128, NT * 8], I32, Pp + 128 + 1024)
    rr_f = at("rr_f", [128, NT * 8], F32, Pp + 128 + 1536)
    rr_m = at("rr_m", [128, NT * 8], F32, Pp + 128 + 2048)
    S.pool(lambda e: e.iota(posi[:], pattern=[[128, NT]], base=0, channel_multiplier=1), [], ["posi"])
    S.dve(CP(posf[:], posi[:]), ["posi"], ["posf"])
    angs3 = angs[:].rearrange("p (i f) -> p i f", f=8)
    for f in range(8):
        S.dve(TS(angs3[:, :, f], posf[:], INVF[f], None, ALU.mult), ["posf"], ["ang"])
    S.dve(TS(angc[:], angs[:], float(PI / 2), None, ALU.add), ["ang"], ["angc"])
    range_reduce(angs[:], rr_i[:], rr_f[:], rr_m[:])
    S.act(AV(sin_t[:].rearrange("p i f -> p (i f)"), angs[:], AF.Sin), ["ang"], ["sin_t"])
    S.dve(CP(angs[:], angc[:]), ["angc", "sin_t"], ["ang"])
    range_reduce(angs[:], rr_i[:], rr_f[:], rr_m[:])
    S.act(AV(cos_t[:].rearrange("p i f -> p (i f)"), angs[:], AF.Sin), ["ang"], ["cos_t"])
    S.pool(MS(pastm[:], 0.0), [], ["pastm"])
    for jj in range(4):
        pv = pastm[:, jj, :].rearrange("p (h n) -> p h n", n=8)
        S.pool(MS(pv[:, :, 4 + jj:8], NEG), ["pastm"], ["pastm"])
    S.pool(MS(corr[:], 1.0), [], ["corr"])
    for g in range(4):
        w = 2 ** (g + 1)
        for t in range(w - 1):
            S.pool(MS(corr[:, g, t:t + 1], float(w) / float(t + 1)), ["corr"], ["corr"])
    S.dma("sp", DM(wrst[:], wr_d.rearrange("(k p) n -> p k n", p=128)), [], ["wrst"])
    S.dve(CP(wrb[:], wrst[:]), ["wrst"], ["wrb"])
    for i in range(4):
        S.dma("sp", DM(rb4[:, i, :], rb_d.partition_broadcast(128)), [], [("rb4", i)])
    for l in range(NL):
        S.dma("sp", DM(pscol[:, l, :], pscale_d[l].rearrange("(g p) -> p g", p=128), allow_slow_non_contiguous=True),
              [], [("pscol", l)])
    for k in range(8):
        S.dma("sp", DM(condT[:, k, :], c_d[:, k * 128:(k + 1) * 128].rearrange("b p -> p b"),
                       allow_slow_non_contiguous=True), [], [("condT", k)])
    S.act(AV(condT[:], condT[:], AF.Silu), [("condT", k) for k in range(8)], ["condT"])
    NWM = 6
    wm = [at("wm%d" % i, [128, 8, 512], BF16, R + 8192 * i) for i in range(NWM)]
    condTb = small("condTb", [128, 8, 2], BF16)
    S.dve(CP(condTb[:], condT[:]), ["condT"], ["condTb"])
    gi = 0
    for l in range(nlayers):
        for cg in range(12):
            sl = gi % NWM
            S.dma("pool", DM(wm[sl][:], wmod_d[l, :, cg * 512:(cg + 1) * 512].rearrange("(k p) n -> p k n", p=128)),
                  [], [("wm", sl)])
            for k in range(8):
                S.pe(MM(PF[0][0:2, :], condTb[:, k, :], wm[sl][:, k, :], k == 0, k == 7),
                     [("wm", sl), "condTb"], ["pf0"])
            S.act(ACP(modrow[:], PF[0][0:2, :]), ["pf0"], ["modrow"])
            S.dma("sp", DM(mod_d[l, :, cg * 512:(cg + 1) * 512], modrow[:]), ["modrow"], [("mod_d", l, cg)])
            gi += 1
    mod_keys = [("mod_d", l, cg) for l in range(nlayers) for cg in range(12)]

    def load_bc(slot, src_row, add_row=None, plus1=False):
        key = ("bc", slot)
        S.dma("sp", DM(bc[slot][:], src_row.partition_broadcast(128)), [], [key])
        if add_row is not None:
            S.dma("sp", DM(lnA[1][:], add_row.partition_broadcast(128)), [], ["lnA1"])
            if plus1:
                S.dve(STT(bc[slot][:], bc[slot][:], 1.0, lnA[1][:], ALU.add, ALU.add), [key, "lnA1"], [key])
            else:
                S.dve(TT(bc[slot][:], bc[slot][:], lnA[1][:], ALU.add), [key, "lnA1"], [key])

    def ln_tile(src, src_keys, work, work_keys, gain, gain_keys, bias, bias_keys, out, out_keys, sl, add_eng="pool"):
        kst, kmv, krs, knm = "st6_%d" % sl, "mv%d" % sl, "rstd%d" % sl, "nmr%d" % sl
        st_, mv_, rs_, nm_ = st6[sl], mv[sl], rstd[sl], nmr[sl]
        S.dve(lambda e: e.bn_stats(out=st_[:, 0:6], in_=src[:, 0:512]), src_keys, [kst + "a"])
        S.dve(lambda e: e.bn_stats(out=st_[:, 6:12], in_=src[:, 512:1024]), src_keys, [kst + "b"])
        S.dve(lambda e: e.bn_aggr(out=mv_[:], in_=st_[:]), [kst + "a", kst + "b"], [kmv])
        S.dve(TS(rs_[:], mv_[:, 1:2], EPS, None, ALU.add), [kmv], [krs])
        S.act(lambda e: e.sqrt(out=rs_[:], in_=rs_[:]), [krs], [krs])
        S.dve(RCP(rs_[:], rs_[:]), [krs], [krs])
        S.dve(STT(nm_[:], mv_[:, 0:1], -1.0, rs_[:], ALU.mult, ALU.mult), [kmv, krs], [knm])
        S.act(AV(work, src, AF.Identity, bias=nm_[:], scale=rs_[:]), list(src_keys) + [krs, knm], work_keys)
        S.dve(TT(work, work, gain, ALU.mult), list(work_keys) + list(gain_keys), work_keys)
        S.add(add_eng, TT(out, work, bias, ALU.add), list(work_keys) + list(bias_keys), out_keys)

    def ln_stats(i, src, src_keys):
        S.dve(lambda e: e.bn_stats(out=stA[:, i, 0:6], in_=src[:, 0:512]), src_keys, [("stA", i, 0)])
        S.dve(lambda e: e.bn_stats(out=stA[:, i, 6:12], in_=src[:, 512:1024]), src_keys, [("stA", i, 1)])
        S.dve(lambda e: e.bn_aggr(out=mvA[:, i, :], in_=stA[:, i, :]), [("stA", i, 0), ("stA", i, 1)], [("mvA", i)])

    def ln_finish(eps=EPS):
        mvk = [("mvA", i_) for i_ in range(NT)]
        S.dve(TS(rsA[:], mvA[:, :, 1], eps, None, ALU.add), mvk, ["rsA"])
        S.act(lambda e: e.sqrt(out=rsA[:], in_=rsA[:]), ["rsA"], ["rsA"])
        S.dve(RCP(rsA[:], rsA[:]), ["rsA"], ["rsA"])
        S.dve(STT(nmA[:], mvA[:, :, 0], -1.0, rsA[:], ALU.mult, ALU.mult), mvk + ["rsA"], ["nmA"])

    def ln_apply(i, src, src_keys, work, work_keys, gain, gain_keys, bias, bias_keys, out, out_keys):
        S.act(AV(work, src, AF.Identity, bias=nmA[:, i:i + 1], scale=rsA[:, i:i + 1]),
              list(src_keys) + ["rsA", "nmA"], work_keys)
        S.dve(TT(work, work, gain, ALU.mult), list(work_keys) + list(gain_keys), work_keys)
        S.dve(TT(out, work, bias, ALU.add), list(work_keys) + list(bias_keys), out_keys)

    def transpose_to_hT(i, sl):
        for k in range(8):
            S.pe(TR(PB[:, k * 128:(k + 1) * 128], htok[sl][:, k * 128:(k + 1) * 128]), [("htok", sl), "ident"], ["pb"])
        S.act(ACP(hT[:, :, i * 128:(i + 1) * 128], PB[:, :].rearrange("p (k t) -> p k t", t=128)), ["pb"], [("hT", i)])

    def dump2(ap0, ap1=None):
        S.barrier()
        S.dma("sp", DM(dbg_d[:, 0:1024], ap0), [], ["dbg0"])
        if ap1 is not None:
            S.dma("sp", DM(dbg_d[:, 1024:2048], ap1), [], ["dbg1"])

    def dbg_is(name):
        return dbg is not None and dbg[0] == name

    stop = [False]

    for s in range(nseq):
        for l in range(nlayers):
            if stop[0]:
                break
            xin = x_d if l == 0 else xs_d
            xout = out_d if l == nlayers - 1 else xs_d
            S.barrier()
            load_bc(0, mod_d[l, s:s + 1, 1 * D:2 * D], bmod_d[l:l + 1, 1 * D:2 * D], plus1=True)
            load_bc(1, mod_d[l, s:s + 1, 0:D], bmod_d[l:l + 1, 0:D])
            for gpre in range(2):
                S.dma("pool", DM(wbf[gpre][:], win_d[l, :, gpre * 512:(gpre + 1) * 512].rearrange("(k p) n -> p k n", p=128)),
                      [], [("wbf", gpre, 0), ("wbf", gpre, 1)])
            for i in range(NT):
                S.dma("sp", DM(X[:, i, :], xin[s, i * 128:(i + 1) * 128, :]),
                      [("xs", s, i)] if l > 0 else [], [("X", i)])
                ln_stats(i, X[:, i, :], [("X", i)])
            ln_finish()
            for i in range(NT):
                sl = i % 2
                ln_apply(i, X[:, i, :], [("X", i)], lnA[sl][:], ["lnA%d" % sl], bc[0][:], [("bc", 0)],
                         bc[1][:], [("bc", 1)], htok[sl][:], [("htok", sl)])
                transpose_to_hT(i, sl)
            S.barrier()
            if dbg_is("hT"):
                S.barrier()
                S.dve(CP(lnA[0][:], hT[:, 0, 0:1024]), [], ["Xd"])
                dump2(lnA[0][:])
                stop[0] = True
                break
            S.pool(MS(vaug[:, :, :, 64:65], 1.0), [], ["vones"])

            def load_win(gidx, sl):
                S.dma("pool", DM(wbf[sl][:], win_d[l, :, gidx * 512:(gidx + 1) * 512].rearrange("(k p) n -> p k n", p=128)),
                      [], [("wbf", sl, 0), ("wbf", sl, 1)])

            for gidx in range(4):
                sl = gidx % 2
                if gidx >= 2:
                    load_win(gidx, sl)
                wkeys = [("wbf", sl, 0), ("wbf", sl, 1)]
                if gidx < 3:
                    def proj_mm(i):
                        pf = PF[i % 2]
                        pk = "pf%d" % (i % 2)
                        for k in range(8):
                            S.pe(MM(pf[:, :], hT[:, k, i * 128:(i + 1) * 128], wbf[sl][:, k, :], k == 0, k == 7),
                                 [("hT", i)] + wkeys, [pk])

                    def proj_post(i):
                        pf = PF[i % 2]
                        pk = "pf%d" % (i % 2)
                        ps3 = pf[:, :].rearrange("p (h d) -> p h d", d=64)
                        if gidx == 2:
                            S.act(ACP(vaug[:, i, :, 0:64], ps3), [pk], [("v", i)])
                            return
                        qs = i % 2
                        o3 = qktok[qs][:].rearrange("p (h d) -> p h d", d=64)
                        cb = cos_t[:, i:i + 1, :].broadcast_to([128, 8, 8])
                        sb_ = sin_t[:, i:i + 1, :].broadcast_to([128, 8, 8])
                        x1 = ps3[:, :, 0:8]
                        x2 = ps3[:, :, 8:16]
                        qk_key = ("qktok", qs)
                        S.dve(TT(rope_t[0][:], x1, cb, ALU.mult), [pk, "cos_t"], ["rt0"])
                        S.dve(TT(rope_t[1][:], x2, sb_, ALU.mult), [pk, "sin_t"], ["rt1"])
                        S.dve(TT(rope_t[2][:], x2, cb, ALU.mult), [pk, "cos_t"], ["rt2"])
                        S.dve(TT(rope_t[3][:], x1, sb_, ALU.mult), [pk, "sin_t"], ["rt3"])
                        S.pool(TT(o3[:, :, 0:8], rope_t[0][:], rope_t[1][:], ALU.subtract), ["rt0", "rt1"], [qk_key])
                        S.pool(TT(o3[:, :, 8:16], rope_t[2][:], rope_t[3][:], ALU.add), ["rt2", "rt3"], [qk_key])
                        S.act(ACP(o3[:, :, 16:64], ps3[:, :, 16:64]), [pk], [qk_key])
                        for cc in range(4):
                            S.pe(TR(PB[:, cc * 128:(cc + 1) * 128], qktok[qs][:, cc * 128:(cc + 1) * 128]),
                                 [qk_key, "ident"], ["pb"])
                        base = 4 * gidx
                        S.dve(CP(qkT[:, base:base + 4, i * 128:(i + 1) * 128],
                                 PB[:, 0:512].rearrange("p (k t) -> p k t", t=128)),
                              ["pb"], [("qk", base + cc_, i) for cc_ in range(4)])

                    proj_mm(0)
                    for i in range(1, NT):
                        proj_mm(i)
                        proj_post(i - 1)
                    proj_post(NT - 1)
                else:
                    for g in range(4):
                        w = 2 ** (g + 1)
                        S.pool(MS(pbuf[:, 0:16], 0.0), [], ["pbuf_h"])
                        S.pool(MS(tmpA[:, 0:16], 0.0), [], ["tmpA_h"])
                        S.pool(MS(tmpB[:, 0:16], 0.0), [], ["tmpB_h"])
                        for tc in range(4):
                            pf = PF[tc % 2]
                            pk = "pf%d" % (tc % 2)
                            for k in range(8):
                                S.pe(MM(pf[:, :], wbf[sl][:, k, g * 128:(g + 1) * 128], hT[:, k, tc * 512:(tc + 1) * 512],
                                        k == 0, k == 7), [("hT", 4 * tc + j) for j in range(4)] + wkeys, [pk])
                            S.act(ACP(pbuf[:, 16 + tc * 512:16 + (tc + 1) * 512], pf[:, :]), [pk], [("pbuf", tc)])
                        pkeys = ["pbuf_h"] + [("pbuf", tc) for tc in range(4)]
                        cur, curk = pbuf, pkeys
                        nxts = [(tmpA, ["tmpA_h", "tmpA"]), (tmpB, ["tmpB_h", "tmpB"])]
                        for step in range(g + 1):
                            sh = 2 ** step
                            nxt, nk = nxts[step % 2]
                            eng = "dve"
                            S.add(eng, TT(nxt[:, 16:16 + SEQ], cur[:, 16:16 + SEQ], cur[:, 16 - sh:16 - sh + SEQ], ALU.add),
                                  curk, [nk[1]])
                            cur, curk = nxt, nk
                        S.dve(TT(cur[:, 16:16 + w - 1], cur[:, 16:16 + w - 1], corr[:, g, 0:w - 1], ALU.mult),
                              curk + ["corr"], [curk[1]])
                        S.dve(STT(dT[:, g, :], cur[:, 16:16 + SEQ], float(1.0 / w), pbuf[:, 16:16 + SEQ], ALU.mult, ALU.subtract),
                              curk + pkeys, [("dT", g)])
                if dbg_is("qearly") and gidx == dbg[3]:
                    break
            S.dma("sp", DM(wpst[:], wpool_d[l].rearrange("g c e -> c g e")), [], ["wpst"])
            S.dve(CP(wpbf[:], wpst[:]), ["wpst"], ["wpbf"])
            if dbg_is("qkT") or dbg_is("qearly"):
                S.barrier()
                cidx = dbg[2]
                S.dve(CP(lnA[0][:], qkT[:, cidx, 0:1024]), [], ["Xd"])
                S.dve(CP(lnA[1][:], qkT[:, cidx, 1024:2048]), [], ["Xd"])
                dump2(lnA[0][:], lnA[1][:])
                stop[0] = True
                break
            S.barrier()
            S.pool(MS(esel[:, :, :], 1.0), [], ["esel"])
            for hb in range(2):
                ev = esel[hb * 64:(hb + 1) * 64, :, :]
                S.pool((lambda ev=ev: lambda e: e.affine_select(out=ev, in_=ev, pattern=[[-1, 64], [0, 128]],
                                                                compare_op=ALU.is_equal, fill=0.0, base=0,
                                                                channel_multiplier=1))(), ["esel"], ["esel"])
            S.dve(MS(KB32[:], 0.0), [], ["KB32"])
            for c in range(4):
                for hh in range(2):
                    h = 2 * c + hh
                    S.dve(TRD(KB32[hh * 64:(hh + 1) * 64, c, h * 8:(h + 1) * 8],
                              qkT[hh * 64:(hh + 1) * 64, 4 + c, :].rearrange("p (n k) -> p n k", k=256), ALU.add),
                          [], ["KB32"])
            S.dve(CP(KB[:], KB32[:]), ["KB32"], ["KB"])
            for i in range(8, NT):
                j = i // 2
                jj = j - 4
                for c in range(4):
                    S.pe(MM(PF[6][:, 0:64], qkT[:, c, i * 128:(i + 1) * 128], KB[:, c, :], c == 0, c == 3), ["KB"], ["pf6"])
                S.dve(TT(gm[:], PF[6][:, 0:64], pastm[:, jj, :], ALU.add), ["pf6", "pastm"], ["gm"])
                for h in range(8):
                    S.dve(MAX8(srt[:, h * 8:(h + 1) * 8], gm[:, h * 8:(h + 1) * 8]), ["gm"], ["srt"])
                gm3 = gm[:].rearrange("p (h n) -> p h n", n=8)
                srt3 = srt[:].rearrange("p (h n) -> p h n", n=8)
                sel3 = selt[:].rearrange("p (h n) -> p h n", n=8)
                bt3 = btok[:, 0:64].rearrange("p (h n) -> p h n", n=8)
                S.dve(TT(sel3, gm3, srt3[:, :, 2:3].broadcast_to([128, 8, 8]), ALU.is_ge), ["gm", "srt"], ["selt"])
                S.dve(TS(btok[:, 0:64], selt[:], -1.0, BIGB, ALU.add, ALU.mult), ["selt"], ["btok"])
                S.dve(MS(bt3[:, :, j:j + 1], 0.0), ["btok"], ["btok"])
                S.dve(CP(btok[:, 64:128], btok[:, 0:64]), ["btok"], ["btok"])
                S.pe(TR(PB[:, 0:128], btok[:, :]), ["btok", "ident"], ["pb"])
                S.act(ACP(biasT[:, (i - 8) * 128:(i - 7) * 128], PB[:, 0:128]), ["pb"], [("biasT", i)])
            load_bc(3, mod_d[l, s:s + 1, 2 * D:3 * D], bmod_d[l:l + 1, 2 * D:3 * D])
            for pc in range(4):
                S.dma("sp", DM(stg[pc % 2][:].rearrange("p (k n) -> p k n", n=D),
                               wout_d[l, 256 * pc:256 * pc + 256, :].rearrange("(k p) n -> p k n", p=128)),
                      [], [("stg", pc % 2)])
                for kk in range(2):
                    S.dve(TT(wo[:, 2 * pc + kk, :], stg[pc % 2][:, kk * D:(kk + 1) * D], bc[3][:], ALU.mult),
                          [("stg", pc % 2), ("bc", 3)], [("wo", 2 * pc + kk)])
            wokeys = [("wo", k) for k in range(8)]
            load_bc(1, ln1g_d[l:l + 1, :])
            load_bc(2, ln1b_d[l:l + 1, :])
            load_bc(0, mod_d[l, s:s + 1, 4 * D:5 * D], bmod_d[l:l + 1, 4 * D:5 * D], plus1=True)
            load_bc(3, mod_d[l, s:s + 1, 3 * D:4 * D], bmod_d[l:l + 1, 3 * D:4 * D])
            if dbg_is("a3"):
                S.barrier()
                S.dve(CP(lnA[0][0:64, :], biasT[0:64, :]), [], ["Xd"])
                dump2(lnA[0][:])
                stop[0] = True
                break
            items = []
            for c in range(4):
                for qc in range(4):
                    for hh in range(2):
                        for kt in range(4 * qc + 4):
                            items.append((c, qc, hh, kt))

            def emit_qk(idx):
                c, qc, hh, kt = items[idx]
                h = 2 * c + hh
                r0 = hh * 64
                n = kt // 2
                q0 = qc * 512 if kt < 4 * qc else kt * 128
                q1 = (qc + 1) * 512
                nq = q1 - q0
                qtiles = list(range(q0 // 128, q1 // 128))
                sc = PF[idx % 2]
                sck = "pf%d" % (idx % 2)
                pr = probs[idx % 3]
                prk = ("probs", idx % 3)
                need_bias = qc >= 2
                S.pe(MM(sc[:, 0:nq], qkT[r0:r0 + 64, 4 + c, kt * 128:(kt + 1) * 128],
                        qkT[r0:r0 + 64, c, q0:q1], True, not need_bias),
                     [("qk", 4 + c, kt)] + [("qk", c, t) for t in qtiles], [sck])
                if need_bias:
                    S.pe(MM(sc[:, 0:nq], esel[r0:r0 + 64, h * 8 + n, :],
                            biasT[r0:r0 + 64, q0 - 1024:q1 - 1024], False, True),
                         [("biasT", t) for t in qtiles] + ["esel"], [sck])
                S.act(AV(pr[:, 0:nq], sc[:, 0:nq], AF.Exp, scale=0.125), [sck], [prk])
                if kt >= 4 * qc:
                    S.dve(TT(pr[:, 0:128], pr[:, 0:128], tri[:], ALU.mult), [prk, "tri"], [prk])

            def emit_pv(idx):
                c, qc, hh, kt = items[idx]
                h = 2 * c + hh
                q0 = qc * 512 if kt < 4 * qc else kt * 128
                q1 = (qc + 1) * 512
                qt0 = q0 // 128
                pr = probs[idx % 3]
                prk = ("probs", idx % 3)
                ap_sl = (c * 4 + qc) % 2
                gpar = ((c * 4 + qc) * 2 + hh) % 2
                accb = PF[2 + gpar]
                for qt in range(qt0, q1 // 128):
                    qq = qt - 4 * qc
                    acck = ("acc", gpar, qq)
                    S.pe(MM(accb[:, qq * 128:qq * 128 + 65], pr[:, (qt - qt0) * 128:(qt - qt0 + 1) * 128], vaug[:, kt, h, :],
                            kt == 0 and qt == qt0, kt == 4 * qc + 3), [prk, ("v", kt), "vones"], [acck])
                if kt == 4 * qc + 3:
                    for qq in range(4):
                        accks = [("acc", gpar, q_) for q_ in range(4)]
                        S.dve(RCP(rcp[:, qq:qq + 1], accb[:, qq * 128 + 64:qq * 128 + 65]), accks, [("rcp", qq)])
                        S.dve(TS(apair[ap_sl][:, qq, hh * 64:(hh + 1) * 64], accb[:, qq * 128:qq * 128 + 64], rcp[:, qq:qq + 1],
                                 None, ALU.mult), accks + [("rcp", qq)], [("apair", ap_sl, qq)])
                    if hh == 1:
                        for qq in range(4):
                            S.pe(TR(PB[:, qq * 128:(qq + 1) * 128], apair[ap_sl][:, qq, :]),
                                 [("apair", ap_sl, qq), "ident"], ["pb"])
                        S.act(ACP(qkT[:, c, qc * 512:(qc + 1) * 512], PB[:, 0:512]), ["pb"],
                              [("qk", c, 4 * qc + t) for t in range(4)])

            emit_qk(0)
            for idx in range(1, len(items)):
                emit_qk(idx)
                emit_pv(idx - 1)
            emit_pv(len(items) - 1)
            if dbg_is("a4"):
                S.barrier()
                S.dve(CP(lnA[0][:], qkT[:, dbg[2], 0:1024]), [], ["Xd"])
                S.dve(CP(lnA[1][:], qkT[:, dbg[2], 1024:2048]), [], ["Xd"])
                dump2(lnA[0][:], lnA[1][:])
                stop[0] = True
                break
            for g in range(4):
                for tc in range(4):
                    pf = PF[tc % 2]
                    pk = "pf%d" % (tc % 2)
                    S.pe(MM(pf[:, :], wpbf[:, g, :], dT[:, g, tc * 512:(tc + 1) * 512], True, True), ["wpbf", ("dT", g)], [pk])
                    S.act(AV(qkT[:, 4 + g, tc * 512:(tc + 1) * 512], pf[:, :], AF.Identity, scale=pscol[:, l, g:g + 1]),
                          [pk, ("pscol", l)], [("qk", 4 + g, 4 * tc + t) for t in range(4)])
            if dbg_is("amT"):
                S.barrier()
                S.dve(CP(lnA[0][:], qkT[:, dbg[2], 0:1024]), [], ["Xd"])
                S.dve(CP(lnA[1][:], qkT[:, dbg[2], 1024:2048]), [], ["Xd"])
                dump2(lnA[0][:], lnA[1][:])
                stop[0] = True
                break
            S.barrier()
            for i in range(NT):
                S.dma("sp", DM(X[:, i, :], xin[s, i * 128:(i + 1) * 128, :]),
                      [("xs", s, i)] if l > 0 else [], [("X", i)])
            for i in range(NT):
                for half in range(2):
                    pf = PF[4 + half]
                    pk = "pf%d" % (4 + half)
                    for k in range(8):
                        S.pe(MM(pf[:, :], qkT[:, k, i * 128:(i + 1) * 128], wo[:, k, half * 512:(half + 1) * 512], k == 0, k == 7),
                             wokeys, [pk])
                    S.dve(STT(X[:, i, half * 512:(half + 1) * 512], X[:, i, half * 512:(half + 1) * 512], ALPHA,
                              pf[:, :], ALU.mult, ALU.add), [pk, ("X", i)], [("X", i)])
                ln_stats(i, X[:, i, :], [("X", i)])
            ln_finish()
            for i in range(NT):
                sl = i % 2
                ln_apply(i, X[:, i, :], [("X", i)], lnA[sl][:], ["lnA%d" % sl], bc[1][:], [("bc", 1)],
                         bc[2][:], [("bc", 2)], X[:, i, :], [("X", i)])
                ln_stats(i, X[:, i, :], [("X", i)])
            ln_finish()
            for i in range(NT):
                sl = i % 2
                ln_apply(i, X[:, i, :], [("X", i)], lnA[sl][:], ["lnA%d" % sl], bc[0][:], [("bc", 0)],
                         bc[3][:], [("bc", 3)], htok[sl][:], [("htok", sl)])
                transpose_to_hT(i, sl)
            if dbg_is("X1"):
                S.barrier()
                dump2(X[:, dbg[2], :])
                stop[0] = True
                break
            for tg in range(4):
                lg = PF[6]
                for tt in range(4):
                    i = 4 * tg + tt
                    for k in range(8):
                        S.pe(MM(lg[:, tt * 16:(tt + 1) * 16], hT[:, k, i * 128:(i + 1) * 128], wrb[:, k, :], k == 0, k == 7),
                             [("hT", i), "wrb"], ["pf6"])

                def v3(t, a):
                    return t[:].rearrange("p (a b) -> p a b", a=a)

                def bcl(t, a):
                    return t[:, 0:a].rearrange("p (a o) -> p a o", o=1).broadcast_to([128, a, 64 // a])

                R_ = lambda k: ("rt", k)
                rb_keys = [("rb4", i_) for i_ in range(4)]
                lg3 = lg[:, 0:64].rearrange("p (a b) -> p a b", a=4)
                S.dve(TRD(rt[0][:, 0:4], lg3, ALU.max), ["pf6"], [R_(0)])
                S.dve(TT(v3(rt[1], 4), lg3, bcl(rt[0], 4), ALU.subtract), ["pf6", R_(0)], [R_(1)])
                S.act(AV(rt[1][:], rt[1][:], AF.Exp), [R_(1)], [R_(1)])
                S.dve(TRD(rt[0][:, 0:4], v3(rt[1], 4), ALU.add), [R_(1)], [R_(0)])
                S.dve(RCP(rt[0][:, 0:4], rt[0][:, 0:4]), [R_(0)], [R_(0)])
                S.dve(TT(v3(rt[2], 4), v3(rt[1], 4), bcl(rt[0], 4), ALU.mult), [R_(1), R_(0)], [R_(2)])
                S.dve(TT(rt[3][:], rt[2][:], rb4[:].rearrange("p a b -> p (a b)"), ALU.add), [R_(2)] + rb_keys, [R_(3)])
                S.dve(TRD(rt[4][:, 0:16], v3(rt[3], 16), ALU.max), [R_(3)], [R_(4)])
                S.dve(TT(v3(rt[5], 16), v3(rt[3], 16), bcl(rt[4], 16), ALU.is_equal), [R_(3), R_(4)], [R_(5)])
                S.dve(STT(rt[5][:], rt[5][:], NEG, rt[3][:], ALU.mult, ALU.add), [R_(5), R_(3)], [R_(5)])
                S.dve(TRD(rt[6][:, 0:16], v3(rt[5], 16), ALU.max), [R_(5)], [R_(6)])
                S.dve(TT(rt[4][:, 0:16], rt[4][:, 0:16], rt[6][:, 0:16], ALU.add), [R_(4), R_(6)], [R_(4)])
                gs3 = rt[4][:, 0:16].rearrange("p (a b) -> p a b", a=4)
                S.dve(TRD(rt[6][:, 0:4], gs3, ALU.max), [R_(4)], [R_(6)])
                S.dve(TT(rt[7][:, 0:16].rearrange("p (a b) -> p a b", a=4), gs3,
                         rt[6][:, 0:4].rearrange("p (a o) -> p a o", o=1).broadcast_to([128, 4, 4]), ALU.is_equal),
                      [R_(4), R_(6)], [R_(7)])
                S.dve(TS(rt[7][:, 0:16], rt[7][:, 0:16], -1.0, -NEG, ALU.add, ALU.mult), [R_(7)], [R_(7)])
                S.dve(TT(v3(rt[8], 16), v3(rt[3], 16), bcl(rt[7], 16), ALU.add), [R_(3), R_(7)], [R_(8)])
                S.dve(TRD(rt[6][:, 0:4], v3(rt[8], 4), ALU.max), [R_(8)], [R_(6)])
                S.dve(TT(v3(rt[9], 4), v3(rt[8], 4), bcl(rt[6], 4), ALU.is_equal), [R_(8), R_(6)], [R_(9)])
                S.dve(STT(rt[8][:], rt[9][:], NEG, rt[8][:], ALU.mult, ALU.add), [R_(9), R_(8)], [R_(8)])
                S.dve(TRD(rt[6][:, 0:4], v3(rt[8], 4), ALU.max), [R_(8)], [R_(6)])
                S.dve(TT(v3(rt[10], 4), v3(rt[8], 4), bcl(rt[6], 4), ALU.is_equal), [R_(8), R_(6)], [R_(10)])
                S.dve(TT(rt[9][:], rt[9][:], rt[10][:], ALU.add), [R_(9), R_(10)], [R_(9)])
                S.dve(TT(rt[9][:], rt[9][:], rt[2][:], ALU.mult), [R_(9), R_(2)], [R_(9)])
                S.dve(TRD(rt[6][:, 0:4], v3(rt[9], 4), ALU.add), [R_(9)], [R_(6)])
                S.dve(RCP(rt[6][:, 0:4], rt[6][:, 0:4]), [R_(6)], [R_(6)])
                S.dve(TS(rt[6][:, 0:4], rt[6][:, 0:4], float(1.0 / ALPHA), None, ALU.mult), [R_(6)], [R_(6)])
                S.dve(TT(gates[:, 4 * tg:4 * tg + 4, :], v3(rt[9], 4), bcl(rt[6], 4), ALU.mult), [R_(9), R_(6)], [("gates", tg)])
            if dbg_is("gates"):
                S.barrier()
                S.dve(CP(lnA[0][:, 0:256], gates[:].rearrange("p a b -> p (a b)")), [], ["Xd"])
                dump2(lnA[0][:])
                stop[0] = True
                break
            S.barrier()
            load_bc(0, mod_d[l, s:s + 1, 5 * D:6 * D], bmod_d[l:l + 1, 5 * D:6 * D])
            load_bc(1, ln2g_d[l:l + 1, :])
            load_bc(2, ln2b_d[l:l + 1, :])
            nexp = 1 if dbg_is("moe1") else NE

            def load_expert_dma(e_):
                sl = e_ % 2
                for gu, wsrc in ((0, wg_d), (1, wu_d)):
                    S.dma("pool", DM(wgu[sl][:, :, gu, :], wsrc[l, e_].rearrange("(k p) n -> p k n", p=128)),
                          [], [("wgu", sl, gu, 0), ("wgu", sl, gu, 1)])
                for h in range(2):
                    S.dma("sp", DM(stg[h][:].rearrange("p (k n) -> p k n", n=D),
                                   wd_d[l, e_, 256 * h:256 * h + 256, :].rearrange("(k p) n -> p k n", p=128)),
                          [], [("stg", h)])

            def load_expert_cast(e_):
                sl = e_ % 2
                for h in range(2):
                    for kk in range(2):
                        S.dve(TT(wdb[sl][:, 2 * h + kk, :], stg[h][:, kk * D:(kk + 1) * D], bc[0][:], ALU.mult),
                              [("stg", h), ("bc", 0)], [("wd", sl, 2 * h + kk)])

            stages = [(e_, tc) for e_ in range(nexp) for tc in range(4)]

            def emit_gu(si):
                e_, tc = stages[si]
                sl = e_ % 2
                asl = si % 2
                for f in range(4):
                    Gp, Gk = PF[f % 2], "pf%d" % (f % 2)
                    Up, Uk = PF[2 + f % 2], "pf%d" % (2 + f % 2)
                    for gu, pp, pk in ((0, Gp, Gk), (1, Up, Uk)):
                        for k in range(8):
                            S.pe(MM(pp[:, :], wgu[sl][:, k, gu, f * 128:(f + 1) * 128], hT[:, k, tc * 512:(tc + 1) * 512],
                                    k == 0, k == 7),
                                 [("wgu", sl, gu, 0), ("wgu", sl, gu, 1)] + [("hT", 4 * tc + j) for j in range(4)], [pk])
                    S.act(AV(sg[f % 2][:], Gp[:, :], AF.Silu), [Gk], [("sg", f % 2)])
                    S.dve(TT(aT[asl][:, f, :], Up[:, :], sg[f % 2][:], ALU.mult), [Uk, ("sg", f % 2)], [("aT", asl, f)])

            def emit_y(si):
                e_, tc = stages[si]
                sl = e_ % 2
                asl = si % 2
                for tt in range(4):
                    i = 4 * tc + tt
                    for half in range(2):
                        Yp, Yk = PF[4 + half], "pf%d" % (4 + half)
                        for f in range(4):
                            S.pe(MM(Yp[:, :], aT[asl][:, f, tt * 128:(tt + 1) * 128], wdb[sl][:, f, half * 512:(half + 1) * 512],
                                    f == 0, f == 3), [("aT", asl, f), ("wd", sl, f)], [Yk])
                        S.dve(STT(X[:, i, half * 512:(half + 1) * 512], Yp[:, :], gates[:, i, e_:e_ + 1],
                                  X[:, i, half * 512:(half + 1) * 512], ALU.mult, ALU.add),
                              [Yk, ("gates", i // 4), ("X", i)], [("X", i)])

            load_expert_dma(0)
            load_expert_cast(0)
            for si in range(len(stages)):
                e_, tc = stages[si]
                emit_gu(si)
                if si > 0:
                    emit_y(si - 1)
                if tc == 0 and e_ + 1 < nexp:
                    load_expert_dma(e_ + 1)
                if tc == 2 and e_ + 1 < nexp:
                    load_expert_cast(e_ + 1)
            emit_y(len(stages) - 1)
            if dbg_is("moe1") or dbg_is("X2"):
                S.barrier()
                dump2(X[:, dbg[2], :])
                stop[0] = True
                break
            for i in range(NT):
                ln_stats(i, X[:, i, :], [("X", i)])
            ln_finish(float(EPS / (ALPHA * ALPHA)))
            for i in range(NT):
                sl = i % 2
                ln_apply(i, X[:, i, :], [("X", i)], lnA[sl][:], ["lnA%d" % sl], bc[1][:], [("bc", 1)],
                         bc[2][:], [("bc", 2)], lnA[sl][:], ["lnA%d" % sl])
                S.dma("pool", DM(xout[s, i * 128:(i + 1) * 128, :], lnA[sl][:]),
                      ["lnA%d" % sl], [("xs", s, i)] if xout is xs_d else [("out", s, i)])
        if stop[0]:
            break
    S.barrier()
    S.emit(nc)
    return nc


_NC_CACHE = {}


def kernel(**inputs):
    if "nc" not in _NC_CACHE:
        _NC_CACHE["nc"] = build()
    nc = _NC_CACHE["nc"]
    f = lambda a: np.ascontiguousarray(np.asarray(a, dtype=np.float32))
    x = f(inputs["x"])
    c = f(inputs["c"])
    shared = {k: f(inputs[k]) for k in ("w_mod", "b_mod", "w_in", "w_pool", "pool_scale", "w_out", "ln1_g", "ln1_b",
                                        "w_router", "w_gate", "w_up", "w_down", "ln2_g", "ln2_b")}
    shared["router_bias"] = f(inputs["router_bias"]).reshape(1, NE)
    in_maps = []
    for core in range(8):
        m = dict(shared)
        m["x"] = x[2 * core:2 * core + 2]
        m["c"] = c[2 * core:2 * core + 2]
        in_maps.append(m)
    res = run_bass_kernel_spmd(nc, in_maps, core_ids=list(range(8)))
    return np.concatenate([r["out"] for r in res.results], axis=0)
```

```python
import contextlib
import math
import numpy as np
import concourse.bass as bass
import concourse.mybir as mybir
from concourse.bass_utils import run_bass_kernel_spmd

F32 = mybir.dt.float32
BF16 = mybir.dt.bfloat16
I32 = mybir.dt.int32
AF = mybir.ActivationFunctionType
ALU = mybir.AluOpType
AX = mybir.AxisListType

ENGS = ["pe", "act", "dve", "pool", "sp"]
_EMBED_WAIT = False
DMA_RING = {"sp": 8, "act": 4, "pool": 6}


class Op:
    __slots__ = ("idx", "eng", "fn", "reads", "writes", "dma", "deps", "signal",
                 "sig", "ring_wait", "extra", "is_bar")

    def __init__(self, idx, eng, fn, reads, writes, dma):
        self.idx = idx
        self.eng = eng
        self.fn = fn
        self.reads = tuple(reads)
        self.writes = tuple(writes)
        self.dma = dma
        self.deps = ()
        self.signal = False
        self.sig = None
        self.ring_wait = None
        self.extra = ()
        self.is_bar = False


class Sched:
    def __init__(self):
        self.ops = []

    def add(self, eng, fn, reads=(), writes=(), dma=False):
        op = Op(len(self.ops), eng, fn, reads, writes, dma)
        self.ops.append(op)
        return op

    def pe(self, fn, reads=(), writes=()):
        return self.add("pe", fn, reads, writes)

    def act(self, fn, reads=(), writes=()):
        return self.add("act", fn, reads, writes)

    def dve(self, fn, reads=(), writes=()):
        return self.add("dve", fn, reads, writes)

    def pool(self, fn, reads=(), writes=()):
        return self.add("pool", fn, reads, writes)

    def dma(self, q, fn, reads=(), writes=()):
        return self.add(q, fn, reads, writes, dma=True)

    def barrier(self):
        last = {}
        dmas = {q: [] for q in DMA_RING}
        for op in self.ops:
            if op.is_bar:
                continue
            if op.dma:
                dmas[op.eng].append(op.idx)
            elif op.fn is not None:
                last[op.eng] = op.idx
        extra = list(last.values())
        for q, lst in dmas.items():
            extra.extend(lst[-DMA_RING[q]:])
        for e in ENGS:
            op = self.add(e, None)
            op.extra = tuple(extra)
            op.is_bar = True

    def analyze(self):
        last_w = {}
        readers = {}
        for op in self.ops:
            if op.is_bar:
                op.deps = tuple(sorted(d for d in op.extra
                                       if not (self.ops[d].eng == "pe" and op.eng == "pe")))
                continue
            deps = set()
            for k in op.reads:
                w = last_w.get(k)
                if w is not None:
                    deps.add(w)
            for k in op.writes:
                w = last_w.get(k)
                if w is not None:
                    deps.add(w)
                deps.update(readers.get(k, {}).values())
            deps.discard(op.idx)
            pruned = []
            for d in deps:
                p = self.ops[d]
                if p.eng == "pe" and op.eng == "pe" and not p.dma and not op.dma:
                    continue
                pruned.append(d)
            op.deps = tuple(sorted(pruned))
            for k in op.reads:
                rk = (op.eng, op.idx) if op.dma else op.eng
                readers.setdefault(k, {})[rk] = op.idx
            for k in op.writes:
                last_w[k] = op.idx
                readers[k] = {}
        for op in self.ops:
            for d in op.deps:
                self.ops[d].signal = True
        cnt = {e: 0 for e in ENGS}
        dcnt = {}
        dnum = {q: 0 for q in DMA_RING}
        for op in self.ops:
            if op.dma:
                q = op.eng
                i = dnum[q]
                dnum[q] += 1
                key = ("d", q, i % DMA_RING[q])
                prev = dcnt.get(key, 0)
                if prev > 0:
                    op.ring_wait = (key, prev)
                dcnt[key] = prev + 16
                op.sig = (key, prev + 16)
                op.signal = True
            elif op.signal:
                cnt[op.eng] += 1
                op.sig = (("c", op.eng), cnt[op.eng])

    def emit(self, nc):
        self.analyze()
        streams = {e: [o for o in self.ops if o.eng == e] for e in ENGS}
        with contextlib.ExitStack() as st:
            sems = {}
            for e in ENGS:
                sems[("c", e)] = st.enter_context(nc.semaphore("c_" + e))
            for q, n in DMA_RING.items():
                for i in range(n):
                    sems[("d", q, i)] = st.enter_context(nc.semaphore("d_%s%d" % (q, i)))
            block = st.enter_context(nc.Block())
            ops = self.ops

            def run(ename, eng):
                known = {}
                for op in streams[ename]:
                    waits = []
                    if op.ring_wait is not None:
                        waits.append(op.ring_wait)
                    for d in op.deps:
                        waits.append(ops[d].sig)
                    best = {}
                    for k, v in waits:
                        if v > best.get(k, 0):
                            best[k] = v
                    pend = []
                    for k, v in best.items():
                        if known.get(k, 0) >= v:
                            continue
                        pend.append((k, v))
                        known[k] = v
                    attach = None
                    if _EMBED_WAIT and pend and op.fn is not None and not op.dma:
                        attach = pend.pop()
                    for k, v in pend:
                        eng.wait_ge(sems[k], v)
                    if op.fn is None:
                        continue
                    ins = op.fn(eng)
                    if attach is not None:
                        ins._wait_ge(sems[attach[0]], attach[1])
                    if op.signal:
                        assert ins is not None
                        ins.then_inc(sems[op.sig[0]], 16 if op.dma else 1)

            @block.tensor
            def _(e):
                run("pe", e)

            @block.scalar
            def _(e):
                run("act", e)

            @block.vector
            def _(e):
                run("dve", e)

            @block.gpsimd
            def _(e):
                run("pool", e)

            @block.sync
            def _(e):
                run("sp", e)


D = 1024
SEQ = 2048
NT = 16
NL = 2
NE = 16
ALPHA = float(4.0 ** 0.25)
EPS = 1e-5
NEG = -1.0e30
BIGB = 30000.0
INVF = [float(np.float32(500000.0) ** np.float32(-(i * 2.0 / 16.0))) for i in range(8)]
TWO_PI = float(2 * np.pi)
PI = float(np.pi)


def build(nseq=2, nlayers=NL, dbg=None):
    nc = bass.Bass("TRN2", target_bir_lowering=False)

    def din(name, shape):
        return nc.dram_tensor(name, shape, F32, kind="ExternalInput").ap()

    x_d = din("x", [2, SEQ, D])
    c_d = din("c", [2, D])
    wmod_d = din("w_mod", [NL, D, 6 * D])
    bmod_d = din("b_mod", [NL, 6 * D])
    win_d = din("w_in", [NL, D, 2048])
    wpool_d = din("w_pool", [NL, 4, 128, 128])
    pscale_d = din("pool_scale", [NL, 512])
    wout_d = din("w_out", [NL, D, D])
    ln1g_d = din("ln1_g", [NL, D])
    ln1b_d = din("ln1_b", [NL, D])
    wr_d = din("w_router", [D, NE])
    rb_d = din("router_bias", [1, NE])
    wg_d = din("w_gate", [NL, NE, D, 512])
    wu_d = din("w_up", [NL, NE, D, 512])
    wd_d = din("w_down", [NL, NE, 512, D])
    ln2g_d = din("ln2_g", [NL, D])
    ln2b_d = din("ln2_b", [NL, D])
    out_d = nc.dram_tensor("out", [2, SEQ, D], F32, kind="ExternalOutput").ap()
    xs_d = nc.dram_tensor("xs_scr", [2, SEQ, D], F32, kind="Internal").ap()
    mod_d = nc.dram_tensor("mod_scr", [NL, 2, 6 * D], F32, kind="Internal").ap()
    dbg_d = None
    if dbg is not None:
        dbg_d = nc.dram_tensor("dbg", [128, dbg[1]], F32, kind="ExternalOutput").ap()

    S = Sched()

    B0 = 16640
    LIMIT = 229376

    def at(name, shape, dt, off):
        assert off % 32 == 0, (name, off)
        nb = int(np.prod(shape[1:])) * (4 if dt in (F32, I32) else 2)
        assert off + nb <= LIMIT, (name, off, nb)
        return nc.alloc_sbuf_tensor_at(name, shape, dt, offset=off)

    hT = at("hT", [128, 8, SEQ], BF16, B0)
    R = B0 + 32768
    qkT = at("qkT", [128, 8, SEQ], BF16, R)
    wbf = [at("wbf%d" % i, [128, 8, 512], BF16, R + 32768 + 8192 * i) for i in range(2)]
    stg = [at("stg%d" % i, [128, 2048], F32, R + 49152 + 8192 * i) for i in range(2)]
    wo = at("wo", [128, 8, D], BF16, R + 32768)
    vaug = at("vaug", [128, NT, 8, 65], BF16, R + 65536)
    dT = at("dT", [128, 4, SEQ], BF16, R + 82176)
    pbuf = at("pbuf", [128, 16 + SEQ], F32, R + 98560)
    tmpA = at("tmpA", [128, 16 + SEQ], F32, R + 106816)
    tmpB = at("tmpB", [128, 16 + SEQ], F32, R + 115072)
    probs = [at("probs%d" % i, [128, 512], BF16, R + 98560 + 1024 * i) for i in range(3)]
    biasT = at("biasT", [128, 1024], BF16, R + 101632)
    apair = [at("apair%d" % i, [128, 4, 128], BF16, R + 103680 + 1024 * i) for i in range(2)]
    qktok = [at("qktok%d" % i, [128, 512], BF16, R + 123328 + 1024 * i) for i in range(2)]
    esel = at("esel", [128, 64, 128], BF16, R + 106816)
    wpst = at("wpst", [128, 4, 128], F32, R + 125376)
    wpbf = at("wpbf", [128, 4, 128], BF16, R + 127424)
    wgu = [at("wgu%d" % i, [128, 8, 2, 512], BF16, R + 16384 * i) for i in range(2)]
    wdb = [at("wdb%d" % i, [128, 4, D], BF16, R + 32768 + 8192 * i) for i in range(2)]
    X = at("X", [128, NT, D], F32, R + 65536)
    Pp = R + 131072
    lnA = [at("lnA%d" % i, [128, D], F32, Pp + 4096 * i) for i in range(2)]
    htok = [at("htok%d" % i, [128, D], BF16, Pp + 8192 + 2048 * i) for i in range(2)]
    bc = [at("bc%d" % i, [128, D], F32, Pp + 12288 + 4096 * i) for i in range(4)]
    aT = [at("aT%d" % i, [128, 4, 512], BF16, Pp + 28672 + 4096 * i) for i in range(2)]
    sg = [at("sg%d" % i, [128, 512], F32, Pp + 36864 + 2048 * i) for i in range(2)]
    cpos = [Pp + 40960]

    def small(name, shape, dt):
        nb = int(np.prod(shape[1:])) * (4 if dt in (F32, I32) else 2)
        nb = (nb + 31) // 32 * 32
        t = at(name, shape, dt, cpos[0])
        cpos[0] += nb
        return t

    ident = small("ident", [128, 128], BF16)
    tri = small("tri", [128, 128], BF16)
    cos_t = small("cos_t", [128, NT, 8], F32)
    sin_t = small("sin_t", [128, NT, 8], F32)
    pastm = small("pastm", [128, 4, 64], F32)
    corr = small("corr", [128, 4, 16], F32)
    rb4 = small("rb4", [128, 4, NE], F32)
    wrb = small("wrb", [128, 8, NE], BF16)
    wrst = small("wrst", [128, 8, NE], F32)
    pscol = small("pscol", [128, NL, 4], F32)
    condT = small("condT", [128, 8, 2], F32)
    st6 = [small("st6_%d" % i, [128, 12], F32) for i in range(2)]
    mv = [small("mv%d" % i, [128, 2], F32) for i in range(2)]
    rstd = [small("rstd%d" % i, [128, 1], F32) for i in range(2)]
    nmr = [small("nmr%d" % i, [128, 1], F32) for i in range(2)]
    rcp = small("rcp", [128, 4], F32)
    gates = small("gates", [128, NT, NE], F32)
    rtb = Pp + 28672
    rt = [at("rt%d" % i, [128, 64], F32, rtb + 256 * i) for i in range(14)]
    rope_t = [at("rope_t%d" % i, [128, 8, 8], F32, rtb + 4096 + 256 * i) for i in range(4)]
    KB32 = at("KB32", [128, 4, 64], F32, rtb + 5120)
    KB = at("KB", [128, 4, 64], BF16, rtb + 6144)
    gm = at("gm", [128, 64], F32, rtb + 6656)
    srt = at("srt", [128, 64], F32, rtb + 6912)
    selt = at("selt", [128, 64], F32, rtb + 7168)
    btok = at("btok", [128, 128], BF16, rtb + 7424)
    modrow = at("modrow", [2, 512], F32, Pp + 36864)

    stA = small("stA", [128, NT, 12], F32)
    mvA = small("mvA", [128, NT, 2], F32)
    rsA = small("rsA", [128, NT], F32)
    nmA = small("nmA", [128, NT], F32)
    PF = [nc.alloc_psum_tensor("pf%d" % i, [128, 512], F32) for i in range(7)]
    PB = nc.alloc_psum_tensor("pb", [128, 1024], BF16)

    def MM(out, lhsT, rhs, st, sp):
        return lambda e: e.matmul(out, lhsT=lhsT, rhs=rhs, start=st, stop=sp)

    def TR(out, in_):
        idn = ident[:]
        return lambda e: e.transpose(out=out, in_=in_, identity=idn)

    def TT(out, in0, in1, op):
        return lambda e: e.tensor_tensor(out=out, in0=in0, in1=in1, op=op)

    def TS(out, in0, s1, s2, op0, op1=None):
        if op1 is None:
            return lambda e: e.tensor_scalar(out=out, in0=in0, scalar1=s1, scalar2=None, op0=op0)
        return lambda e: e.tensor_scalar(out=out, in0=in0, scalar1=s1, scalar2=s2, op0=op0, op1=op1)

    def STT(out, in0, scalar, in1, op0, op1):
        return lambda e: e.scalar_tensor_tensor(out=out, in0=in0, scalar=scalar, in1=in1, op0=op0, op1=op1)

    def CP(out, in_):
        return lambda e: e.tensor_copy(out=out, in_=in_)

    def ACP(out, in_):
        return lambda e: e.copy(out=out, in_=in_)

    def AV(out, in_, func, bias=None, scale=None):
        kw = {}
        if bias is not None:
            kw["bias"] = bias
        if scale is not None:
            kw["scale"] = scale
        return lambda e: e.activation(out=out, in_=in_, func=func, **kw)

    def DM(out, in_, **kw):
        return lambda e: e.dma_start(out=out, in_=in_, **kw)

    def MS(ap, val):
        return lambda e: e.memset(ap, val)

    def TRD(out, in_, op):
        return lambda e: e.tensor_reduce(out=out, in_=in_, axis=AX.X, op=op)

    def RCP(out, in_):
        return lambda e: e.reciprocal(out=out, in_=in_)

    def MAX8(out, in_):
        return lambda e: e.max(out=out, in_=in_)

    def range_reduce(a, t_i, t_f, t_m):
        S.dve(TS(t_f, a, float(1.0 / TWO_PI), None, ALU.mult), ["ang"], ["rr_f"])
        S.dve(CP(t_i, t_f), ["rr_f"], ["rr_i"])
        S.dve(CP(t_f, t_i), ["rr_i"], ["rr_f"])
        S.dve(STT(a, t_f, -TWO_PI, a, ALU.mult, ALU.add), ["rr_f", "ang"], ["ang"])
        S.dve(TS(t_m, a, PI, None, ALU.is_gt), ["ang"], ["rr_m"])
        S.dve(STT(a, t_m, -TWO_PI, a, ALU.mult, ALU.add), ["rr_m", "ang"], ["ang"])
        S.dve(TS(t_m, a, -PI, None, ALU.is_lt), ["ang"], ["rr_m"])
        S.dve(STT(a, t_m, TWO_PI, a, ALU.mult, ALU.add), ["rr_m", "ang"], ["ang"])

    S.pool(MS(ident[:], 1.0), [], ["ident"])
    S.pool(lambda e: e.affine_select(out=ident[:], in_=ident[:], pattern=[[-1, 128]], compare_op=ALU.is_equal,
                                     fill=0.0, base=0, channel_multiplier=1), ["ident"], ["ident"])
    S.pool(MS(tri[:], 1.0), [], ["tri"])
    S.pool(lambda e: e.affine_select(out=tri[:], in_=tri[:], pattern=[[1, 128]], compare_op=ALU.is_ge,
                                     fill=0.0, base=0, channel_multiplier=-1), ["tri"], ["tri"])
    posi = at("posi", [128, NT], I32, Pp)
    posf = at("posf", [128, NT], F32, Pp + 64)
    angc = at("angc", [128, NT * 8], F32, Pp + 128)
    angs = at("angs", [128, NT * 8], F32, Pp + 128 + 512)
    rr_i = at("rr_i", [128, NT * 8], I32, Pp + 128 + 1024)
    rr_f = at("rr_f", [128, NT * 8], F32, Pp + 128 + 1536)
    rr_m = at("rr_m", [128, NT * 8], F32, Pp + 128 + 2048)
    S.pool(lambda e: e.iota(posi[:], pattern=[[128, NT]], base=0, channel_multiplier=1), [], ["posi"])
    S.dve(CP(posf[:], posi[:]), ["posi"], ["posf"])
    angs3 = angs[:].rearrange("p (i f) -> p i f", f=8)
    for f in range(8):
        S.dve(TS(angs3[:, :, f], posf[:], INVF[f], None, ALU.mult), ["posf"], ["ang"])
    S.dve(TS(angc[:], angs[:], float(PI / 2), None, ALU.add), ["ang"], ["angc"])
    range_reduce(angs[:], rr_i[:], rr_f[:], rr_m[:])
    S.act(AV(sin_t[:].rearrange("p i f -> p (i f)"), angs[:], AF.Sin), ["ang"], ["sin_t"])
    S.dve(CP(angs[:], angc[:]), ["angc", "sin_t"], ["ang"])
    range_reduce(angs[:], rr_i[:], rr_f[:], rr_m[:])
    S.act(AV(cos_t[:].rearrange("p i f -> p (i f)"), angs[:], AF.Sin), ["ang"], ["cos_t"])
    S.pool(MS(pastm[:], 0.0), [], ["pastm"])
    for jj in range(4):
        pv = pastm[:, jj, :].rearrange("p (h n) -> p h n", n=8)
        S.pool(MS(pv[:, :, 4 + jj:8], NEG), ["pastm"], ["pastm"])
    S.pool(MS(corr[:], 1.0), [], ["corr"])
    for g in range(4):
        w = 2 ** (g + 1)
        for t in range(w - 1):
            S.pool(MS(corr[:, g, t:t + 1], float(w) / float(t + 1)), ["corr"], ["corr"])
    S.dma("sp", DM(wrst[:], wr_d.rearrange("(k p) n -> p k n", p=128)), [], ["wrst"])
    S.dve(CP(wrb[:], wrst[:]), ["wrst"], ["wrb"])
    for i in range(4):
        S.dma("sp", DM(rb4[:, i, :], rb_d.partition_broadcast(128)), [], [("rb4", i)])
    for l in range(NL):
        S.dma("sp", DM(pscol[:, l, :], pscale_d[l].rearrange("(g p) -> p g", p=128), allow_slow_non_contiguous=True),
              [], [("pscol", l)])
    for k in range(8):
        S.dma("sp", DM(condT[:, k, :], c_d[:, k * 128:(k + 1) * 128].rearrange("b p -> p b"),
                       allow_slow_non_contiguous=True), [], [("condT", k)])
    S.act(AV(condT[:], condT[:], AF.Silu), [("condT", k) for k in range(8)], ["condT"])
    NWM = 6
    wm = [at("wm%d" % i, [128, 8, 512], BF16, R + 8192 * i) for i in range(NWM)]
    condTb = small("condTb", [128, 8, 2], BF16)
    S.dve(CP(condTb[:], condT[:]), ["condT"], ["condTb"])
    modbig = at("modbig", [2, 6 * D], F32, R + 65536)
    gi = 0
    for l in range(nlayers):
        for cg in range(12):
            sl = gi % NWM
            pfm = PF[gi % 2]
            pfk = "pf%d" % (gi % 2)
            S.dma("pool", DM(wm[sl][:], wmod_d[l, :, cg * 512:(cg + 1) * 512].rearrange("(k p) n -> p k n", p=128)),
                  [], [("wm", sl)])
            for k in range(8):
                S.pe(MM(pfm[0:2, :], condTb[:, k, :], wm[sl][:, k, :], k == 0, k == 7),
                     [("wm", sl), "condTb"], [pfk])
            S.act(ACP(modbig[:, cg * 512:(cg + 1) * 512], pfm[0:2, :]), [pfk], [("modbig", cg)])
            gi += 1
        S.dma("sp", DM(mod_d[l, :, :], modbig[:]), [("modbig", cg_) for cg_ in range(12)],
              [("mod_d", l, cg_) for cg_ in range(12)])
    mod_keys = [("mod_d", l, cg) for l in range(nlayers) for cg in range(12)]

    def load_bc(slot, src_row, add_row=None, plus1=False):
        key = ("bc", slot)
        S.dma("sp", DM(bc[slot][:], src_row.partition_broadcast(128)), [], [key])
        if add_row is not None:
            S.dma("sp", DM(lnA[1][:], add_row.partition_broadcast(128)), [], ["lnA1"])
            if plus1:
                S.dve(STT(bc[slot][:], bc[slot][:], 1.0, lnA[1][:], ALU.add, ALU.add), [key, "lnA1"], [key])
            else:
                S.dve(TT(bc[slot][:], bc[slot][:], lnA[1][:], ALU.add), [key, "lnA1"], [key])

    def ln_tile(src, src_keys, work, work_keys, gain, gain_keys, bias, bias_keys, out, out_keys, sl, add_eng="pool"):
        kst, kmv, krs, knm = "st6_%d" % sl, "mv%d" % sl, "rstd%d" % sl, "nmr%d" % sl
        st_, mv_, rs_, nm_ = st6[sl], mv[sl], rstd[sl], nmr[sl]
        S.dve(lambda e: e.bn_stats(out=st_[:, 0:6], in_=src[:, 0:512]), src_keys, [kst + "a"])
        S.dve(lambda e: e.bn_stats(out=st_[:, 6:12], in_=src[:, 512:1024]), src_keys, [kst + "b"])
        S.dve(lambda e: e.bn_aggr(out=mv_[:], in_=st_[:]), [kst + "a", kst + "b"], [kmv])
        S.dve(TS(rs_[:], mv_[:, 1:2], EPS, None, ALU.add), [kmv], [krs])
        S.act(lambda e: e.sqrt(out=rs_[:], in_=rs_[:]), [krs], [krs])
        S.dve(RCP(rs_[:], rs_[:]), [krs], [krs])
        S.dve(STT(nm_[:], mv_[:, 0:1], -1.0, rs_[:], ALU.mult, ALU.mult), [kmv, krs], [knm])
        S.act(AV(work, src, AF.Identity, bias=nm_[:], scale=rs_[:]), list(src_keys) + [krs, knm], work_keys)
        S.dve(TT(work, work, gain, ALU.mult), list(work_keys) + list(gain_keys), work_keys)
        S.add(add_eng, TT(out, work, bias, ALU.add), list(work_keys) + list(bias_keys), out_keys)

    def ln_stats(i, src, src_keys):
        S.dve(lambda e: e.bn_stats(out=stA[:, i, 0:6], in_=src[:, 0:512]), src_keys, [("stA", i, 0)])
        S.dve(lambda e: e.bn_stats(out=stA[:, i, 6:12], in_=src[:, 512:1024]), src_keys, [("stA", i, 1)])
        S.dve(lambda e: e.bn_aggr(out=mvA[:, i, :], in_=stA[:, i, :]), [("stA", i, 0), ("stA", i, 1)], [("mvA", i)])

    def ln_finish(eps=EPS):
        mvk = [("mvA", i_) for i_ in range(NT)]
        S.dve(TS(rsA[:], mvA[:, :, 1], eps, None, ALU.add), mvk, ["rsA"])
        S.act(lambda e: e.sqrt(out=rsA[:], in_=rsA[:]), ["rsA"], ["rsA"])
        S.dve(RCP(rsA[:], rsA[:]), ["rsA"], ["rsA"])
        S.dve(STT(nmA[:], mvA[:, :, 0], -1.0, rsA[:], ALU.mult, ALU.mult), mvk + ["rsA"], ["nmA"])

    def ln_apply(i, src, src_keys, work, work_keys, gain, gain_keys, bias, bias_keys, out, out_keys):
        S.act(AV(work, src, AF.Identity, bias=nmA[:, i:i + 1], scale=rsA[:, i:i + 1]),
              list(src_keys) + ["rsA", "nmA"], work_keys)
        S.dve(TT(work, work, gain, ALU.mult), list(work_keys) + list(gain_keys), work_keys)
        S.dve(TT(out, work, bias, ALU.add), list(work_keys) + list(bias_keys), out_keys)

    def transpose_to_hT(i, sl):
        for k in range(8):
            S.pe(TR(PB[:, k * 128:(k + 1) * 128], htok[sl][:, k * 128:(k + 1) * 128]), [("htok", sl), "ident"], ["pb"])
        S.act(ACP(hT[:, :, i * 128:(i + 1) * 128], PB[:, :].rearrange("p (k t) -> p k t", t=128)), ["pb"], [("hT", i)])

    def dump2(ap0, ap1=None):
        S.barrier()
        S.dma("sp", DM(dbg_d[:, 0:1024], ap0), [], ["dbg0"])
        if ap1 is not None:
            S.dma("sp", DM(dbg_d[:, 1024:2048], ap1), [], ["dbg1"])

    def dbg_is(name):
        return dbg is not None and dbg[0] == name

    stop = [False]

    for s in range(nseq):
        for l in range(nlayers):
            if stop[0]:
                break
            xin = x_d if l == 0 else xs_d
            xout = out_d if l == nlayers - 1 else xs_d
            S.barrier()
            load_bc(0, mod_d[l, s:s + 1, 1 * D:2 * D], bmod_d[l:l + 1, 1 * D:2 * D], plus1=True)
            load_bc(1, mod_d[l, s:s + 1, 0:D], bmod_d[l:l + 1, 0:D])
            for gpre in range(2):
                S.dma("pool", DM(wbf[gpre][:], win_d[l, :, gpre * 512:(gpre + 1) * 512].rearrange("(k p) n -> p k n", p=128)),
                      [], [("wbf", gpre, 0), ("wbf", gpre, 1)])
            for i in range(NT):
                S.dma("sp", DM(X[:, i, :], xin[s, i * 128:(i + 1) * 128, :]),
                      [("xs", s, i)] if l > 0 else [], [("X", i)])
                ln_stats(i, X[:, i, :], [("X", i)])
            ln_finish()
            for i in range(NT):
                sl = i % 2
                ln_apply(i, X[:, i, :], [("X", i)], lnA[sl][:], ["lnA%d" % sl], bc[0][:], [("bc", 0)],
                         bc[1][:], [("bc", 1)], htok[sl][:], [("htok", sl)])
                transpose_to_hT(i, sl)
            S.barrier()
            if dbg_is("hT"):
                S.barrier()
                S.dve(CP(lnA[0][:], hT[:, 0, 0:1024]), [], ["Xd"])
                dump2(lnA[0][:])
                stop[0] = True
                break
            S.pool(MS(vaug[:, :, :, 64:65], 1.0), [], ["vones"])

            def load_win(gidx, sl):
                S.dma("pool", DM(wbf[sl][:], win_d[l, :, gidx * 512:(gidx + 1) * 512].rearrange("(k p) n -> p k n", p=128)),
                      [], [("wbf", sl, 0), ("wbf", sl, 1)])

            for gidx in range(4):
                sl = gidx % 2
                if gidx >= 2:
                    load_win(gidx, sl)
                wkeys = [("wbf", sl, 0), ("wbf", sl, 1)]
                if gidx < 3:
                    def proj_mm(i):
                        pf = PF[i % 2]
                        pk = "pf%d" % (i % 2)
                        for k in range(8):
                            S.pe(MM(pf[:, :], hT[:, k, i * 128:(i + 1) * 128], wbf[sl][:, k, :], k == 0, k == 7),
                                 [("hT", i)] + wkeys, [pk])

                    def proj_post(i):
                        pf = PF[i % 2]
                        pk = "pf%d" % (i % 2)
                        ps3 = pf[:, :].rearrange("p (h d) -> p h d", d=64)
                        if gidx == 2:
                            S.act(ACP(vaug[:, i, :, 0:64], ps3), [pk], [("v", i)])
                            return
                        qs = i % 2
                        o3 = qktok[qs][:].rearrange("p (h d) -> p h d", d=64)
                        cb = cos_t[:, i:i + 1, :].broadcast_to([128, 8, 8])
                        sb_ = sin_t[:, i:i + 1, :].broadcast_to([128, 8, 8])
                        x1 = ps3[:, :, 0:8]
                        x2 = ps3[:, :, 8:16]
                        qk_key = ("qktok", qs)
                        S.dve(TT(rope_t[0][:], x1, cb, ALU.mult), [pk, "cos_t"], ["rt0"])
                        S.dve(TT(rope_t[1][:], x2, sb_, ALU.mult), [pk, "sin_t"], ["rt1"])
                        S.dve(TT(rope_t[2][:], x2, cb, ALU.mult), [pk, "cos_t"], ["rt2"])
                        S.dve(TT(rope_t[3][:], x1, sb_, ALU.mult), [pk, "sin_t"], ["rt3"])
                        S.pool(TT(o3[:, :, 0:8], rope_t[0][:], rope_t[1][:], ALU.subtract), ["rt0", "rt1"], [qk_key])
                        S.pool(TT(o3[:, :, 8:16], rope_t[2][:], rope_t[3][:], ALU.add), ["rt2", "rt3"], [qk_key])
                        S.act(ACP(o3[:, :, 16:64], ps3[:, :, 16:64]), [pk], [qk_key])
                        for cc in range(4):
                            S.pe(TR(PB[:, cc * 128:(cc + 1) * 128], qktok[qs][:, cc * 128:(cc + 1) * 128]),
                                 [qk_key, "ident"], ["pb"])
                        base = 4 * gidx
                        S.dve(CP(qkT[:, base:base + 4, i * 128:(i + 1) * 128],
                                 PB[:, 0:512].rearrange("p (k t) -> p k t", t=128)),
                              ["pb"], [("qk", base + cc_, i) for cc_ in range(4)])

                    proj_mm(0)
                    for i in range(1, NT):
                        proj_mm(i)
                        proj_post(i - 1)
                    proj_post(NT - 1)
                else:
                    for g in range(4):
                        w = 2 ** (g + 1)
                        S.pool(MS(pbuf[:, 0:16], 0.0), [], ["pbuf_h"])
                        S.pool(MS(tmpA[:, 0:16], 0.0), [], ["tmpA_h"])
                        S.pool(MS(tmpB[:, 0:16], 0.0), [], ["tmpB_h"])
                        for tc in range(4):
                            pf = PF[tc % 2]
                            pk = "pf%d" % (tc % 2)
                            for k in range(8):
                                S.pe(MM(pf[:, :], wbf[sl][:, k, g * 128:(g + 1) * 128], hT[:, k, tc * 512:(tc + 1) * 512],
                                        k == 0, k == 7), [("hT", 4 * tc + j) for j in range(4)] + wkeys, [pk])
                            S.act(ACP(pbuf[:, 16 + tc * 512:16 + (tc + 1) * 512], pf[:, :]), [pk], [("pbuf", tc)])
                        pkeys = ["pbuf_h"] + [("pbuf", tc) for tc in range(4)]
                        cur, curk = pbuf, pkeys
                        nxts = [(tmpA, ["tmpA_h", "tmpA"]), (tmpB, ["tmpB_h", "tmpB"])]
                        for step in range(g + 1):
                            sh = 2 ** step
                            nxt, nk = nxts[step % 2]
                            eng = "dve"
                            S.add(eng, TT(nxt[:, 16:16 + SEQ], cur[:, 16:16 + SEQ], cur[:, 16 - sh:16 - sh + SEQ], ALU.add),
                                  curk, [nk[1]])
                            cur, curk = nxt, nk
                        S.dve(TT(cur[:, 16:16 + w - 1], cur[:, 16:16 + w - 1], corr[:, g, 0:w - 1], ALU.mult),
                              curk + ["corr"], [curk[1]])
                        S.dve(STT(dT[:, g, :], cur[:, 16:16 + SEQ], float(1.0 / w), pbuf[:, 16:16 + SEQ], ALU.mult, ALU.subtract),
                              curk + pkeys, [("dT", g)])
                if dbg_is("qearly") and gidx == dbg[3]:
                    break
            S.dma("sp", DM(wpst[:], wpool_d[l].rearrange("g c e -> c g e")), [], ["wpst"])
            S.dve(CP(wpbf[:], wpst[:]), ["wpst"], ["wpbf"])
            if dbg_is("qkT") or dbg_is("qearly"):
                S.barrier()
                cidx = dbg[2]
                S.dve(CP(lnA[0][:], qkT[:, cidx, 0:1024]), [], ["Xd"])
                S.dve(CP(lnA[1][:], qkT[:, cidx, 1024:2048]), [], ["Xd"])
                dump2(lnA[0][:], lnA[1][:])
                stop[0] = True
                break
            S.barrier()
            S.pool(MS(esel[:, :, :], 1.0), [], ["esel"])
            for hb in range(2):
                ev = esel[hb * 64:(hb + 1) * 64, :, :]
                S.pool((lambda ev=ev: lambda e: e.affine_select(out=ev, in_=ev, pattern=[[-1, 64], [0, 128]],
                                                                compare_op=ALU.is_equal, fill=0.0, base=0,
                                                                channel_multiplier=1))(), ["esel"], ["esel"])
            S.dve(MS(KB32[:], 0.0), [], ["KB32"])
            for c in range(4):
                for hh in range(2):
                    h = 2 * c + hh
                    S.dve(TRD(KB32[hh * 64:(hh + 1) * 64, c, h * 8:(h + 1) * 8],
                              qkT[hh * 64:(hh + 1) * 64, 4 + c, :].rearrange("p (n k) -> p n k", k=256), ALU.add),
                          [], ["KB32"])
            S.dve(CP(KB[:], KB32[:]), ["KB32"], ["KB"])
            for i in range(8, NT):
                j = i // 2
                jj = j - 4
                for c in range(4):
                    S.pe(MM(PF[6][:, 0:64], qkT[:, c, i * 128:(i + 1) * 128], KB[:, c, :], c == 0, c == 3), ["KB"], ["pf6"])
                S.dve(TT(gm[:], PF[6][:, 0:64], pastm[:, jj, :], ALU.add), ["pf6", "pastm"], ["gm"])
                for h in range(8):
                    S.dve(MAX8(srt[:, h * 8:(h + 1) * 8], gm[:, h * 8:(h + 1) * 8]), ["gm"], ["srt"])
                gm3 = gm[:].rearrange("p (h n) -> p h n", n=8)
                srt3 = srt[:].rearrange("p (h n) -> p h n", n=8)
                sel3 = selt[:].rearrange("p (h n) -> p h n", n=8)
                bt3 = btok[:, 0:64].rearrange("p (h n) -> p h n", n=8)
                S.dve(TT(sel3, gm3, srt3[:, :, 2:3].broadcast_to([128, 8, 8]), ALU.is_ge), ["gm", "srt"], ["selt"])
                S.dve(TS(btok[:, 0:64], selt[:], -1.0, BIGB, ALU.add, ALU.mult), ["selt"], ["btok"])
                S.dve(MS(bt3[:, :, j:j + 1], 0.0), ["btok"], ["btok"])
                S.dve(CP(btok[:, 64:128], btok[:, 0:64]), ["btok"], ["btok"])
                S.pe(TR(PB[:, 0:128], btok[:, :]), ["btok", "ident"], ["pb"])
                S.act(ACP(biasT[:, (i - 8) * 128:(i - 7) * 128], PB[:, 0:128]), ["pb"], [("biasT", i)])
            load_bc(3, mod_d[l, s:s + 1, 2 * D:3 * D], bmod_d[l:l + 1, 2 * D:3 * D])
            for pc in range(4):
                S.dma("sp", DM(stg[pc % 2][:].rearrange("p (k n) -> p k n", n=D),
                               wout_d[l, 256 * pc:256 * pc + 256, :].rearrange("(k p) n -> p k n", p=128)),
                      [], [("stg", pc % 2)])
                for kk in range(2):
                    S.dve(TT(wo[:, 2 * pc + kk, :], stg[pc % 2][:, kk * D:(kk + 1) * D], bc[3][:], ALU.mult),
                          [("stg", pc % 2), ("bc", 3)], [("wo", 2 * pc + kk)])
            wokeys = [("wo", k) for k in range(8)]
            load_bc(1, ln1g_d[l:l + 1, :])
            load_bc(2, ln1b_d[l:l + 1, :])
            load_bc(0, mod_d[l, s:s + 1, 4 * D:5 * D], bmod_d[l:l + 1, 4 * D:5 * D], plus1=True)
            load_bc(3, mod_d[l, s:s + 1, 3 * D:4 * D], bmod_d[l:l + 1, 3 * D:4 * D])
            if dbg_is("a3"):
                S.barrier()
                S.dve(CP(lnA[0][0:64, :], biasT[0:64, :]), [], ["Xd"])
                dump2(lnA[0][:])
                stop[0] = True
                break
            items = []
            for c in range(4):
                for qc in range(4):
                    for hh in range(2):
                        for kt in range(4 * qc + 4):
                            items.append((c, qc, hh, kt))

            def emit_qk(idx):
                c, qc, hh, kt = items[idx]
                h = 2 * c + hh
                r0 = hh * 64
                n = kt // 2
                q0 = qc * 512 if kt < 4 * qc else kt * 128
                q1 = (qc + 1) * 512
                nq = q1 - q0
                qtiles = list(range(q0 // 128, q1 // 128))
                sc = PF[idx % 2]
                sck = "pf%d" % (idx % 2)
                pr = probs[idx % 3]
                prk = ("probs", idx % 3)
                need_bias = qc >= 2
                S.pe(MM(sc[:, 0:nq], qkT[r0:r0 + 64, 4 + c, kt * 128:(kt + 1) * 128],
                        qkT[r0:r0 + 64, c, q0:q1], True, not need_bias),
                     [("qk", 4 + c, kt)] + [("qk", c, t) for t in qtiles], [sck])
                if need_bias:
                    S.pe(MM(sc[:, 0:nq], esel[r0:r0 + 64, h * 8 + n, :],
                            biasT[r0:r0 + 64, q0 - 1024:q1 - 1024], False, True),
                         [("biasT", t) for t in qtiles] + ["esel"], [sck])
                S.act(AV(pr[:, 0:nq], sc[:, 0:nq], AF.Exp, scale=0.125), [sck], [prk])
                if kt >= 4 * qc:
                    S.dve(TT(pr[:, 0:128], pr[:, 0:128], tri[:], ALU.mult), [prk, "tri"], [prk])

            def emit_pv(idx):
                c, qc, hh, kt = items[idx]
                h = 2 * c + hh
                q0 = qc * 512 if kt < 4 * qc else kt * 128
                q1 = (qc + 1) * 512
                qt0 = q0 // 128
                pr = probs[idx % 3]
                prk = ("probs", idx % 3)
                ap_sl = (c * 4 + qc) % 2
                gpar = ((c * 4 + qc) * 2 + hh) % 2
                accb = PF[2 + gpar]
                for qt in range(qt0, q1 // 128):
                    qq = qt - 4 * qc
                    acck = ("acc", gpar, qq)
                    S.pe(MM(accb[:, qq * 128:qq * 128 + 65], pr[:, (qt - qt0) * 128:(qt - qt0 + 1) * 128], vaug[:, kt, h, :],
                            kt == 0 and qt == qt0, kt == 4 * qc + 3), [prk, ("v", kt), "vones"], [acck])
                if kt == 4 * qc + 3:
                    for qq in range(4):
                        accks = [("acc", gpar, q_) for q_ in range(4)]
                        S.dve(RCP(rcp[:, qq:qq + 1], accb[:, qq * 128 + 64:qq * 128 + 65]), accks, [("rcp", qq)])
                        S.dve(TS(apair[ap_sl][:, qq, hh * 64:(hh + 1) * 64], accb[:, qq * 128:qq * 128 + 64], rcp[:, qq:qq + 1],
                                 None, ALU.mult), accks + [("rcp", qq)], [("apair", ap_sl, qq)])
                    if hh == 1:
                        for qq in range(4):
                            S.pe(TR(PB[:, qq * 128:(qq + 1) * 128], apair[ap_sl][:, qq, :]),
                                 [("apair", ap_sl, qq), "ident"], ["pb"])
                        S.act(ACP(qkT[:, c, qc * 512:(qc + 1) * 512], PB[:, 0:512]), ["pb"],
                              [("qk", c, 4 * qc + t) for t in range(4)])

            emit_qk(0)
            for idx in range(1, len(items)):
                emit_qk(idx)
                emit_pv(idx - 1)
            emit_pv(len(items) - 1)
            if dbg_is("a4"):
                S.barrier()
                S.dve(CP(lnA[0][:], qkT[:, dbg[2], 0:1024]), [], ["Xd"])
                S.dve(CP(lnA[1][:], qkT[:, dbg[2], 1024:2048]), [], ["Xd"])
                dump2(lnA[0][:], lnA[1][:])
                stop[0] = True
                break
            for g in range(4):
                for tc in range(4):
                    pf = PF[tc % 2]
                    pk = "pf%d" % (tc % 2)
                    S.pe(MM(pf[:, :], wpbf[:, g, :], dT[:, g, tc * 512:(tc + 1) * 512], True, True), ["wpbf", ("dT", g)], [pk])
                    S.act(AV(qkT[:, 4 + g, tc * 512:(tc + 1) * 512], pf[:, :], AF.Identity, scale=pscol[:, l, g:g + 1]),
                          [pk, ("pscol", l)], [("qk", 4 + g, 4 * tc + t) for t in range(4)])
            if dbg_is("amT"):
                S.barrier()
                S.dve(CP(lnA[0][:], qkT[:, dbg[2], 0:1024]), [], ["Xd"])
                S.dve(CP(lnA[1][:], qkT[:, dbg[2], 1024:2048]), [], ["Xd"])
                dump2(lnA[0][:], lnA[1][:])
                stop[0] = True
                break
            S.barrier()
            for i in range(NT):
                S.dma("sp", DM(X[:, i, :], xin[s, i * 128:(i + 1) * 128, :]),
                      [("xs", s, i)] if l > 0 else [], [("X", i)])
            for i in range(NT):
                for half in range(2):
                    pf = PF[4 + half]
                    pk = "pf%d" % (4 + half)
                    for k in range(8):
                        S.pe(MM(pf[:, :], qkT[:, k, i * 128:(i + 1) * 128], wo[:, k, half * 512:(half + 1) * 512], k == 0, k == 7),
                             wokeys, [pk])
                    S.dve(STT(X[:, i, half * 512:(half + 1) * 512], X[:, i, half * 512:(half + 1) * 512], ALPHA,
                              pf[:, :], ALU.mult, ALU.add), [pk, ("X", i)], [("X", i)])
                ln_stats(i, X[:, i, :], [("X", i)])
            ln_finish()
            for i in range(NT):
                sl = i % 2
                ln_apply(i, X[:, i, :], [("X", i)], lnA[sl][:], ["lnA%d" % sl], bc[1][:], [("bc", 1)],
                         bc[2][:], [("bc", 2)], X[:, i, :], [("X", i)])
                ln_stats(i, X[:, i, :], [("X", i)])
            ln_finish()
            for i in range(NT):
                sl = i % 2
                ln_apply(i, X[:, i, :], [("X", i)], lnA[sl][:], ["lnA%d" % sl], bc[0][:], [("bc", 0)],
                         bc[3][:], [("bc", 3)], htok[sl][:], [("htok", sl)])
                transpose_to_hT(i, sl)
            if dbg_is("X1"):
                S.barrier()
                dump2(X[:, dbg[2], :])
                stop[0] = True
                break
            for tg in range(4):
                lg = PF[6]
                for tt in range(4):
                    i = 4 * tg + tt
                    for k in range(8):
                        S.pe(MM(lg[:, tt * 16:(tt + 1) * 16], hT[:, k, i * 128:(i + 1) * 128], wrb[:, k, :], k == 0, k == 7),
                             [("hT", i), "wrb"], ["pf6"])

                def v3(t, a):
                    return t[:].rearrange("p (a b) -> p a b", a=a)

                def bcl(t, a):
                    return t[:, 0:a].rearrange("p (a o) -> p a o", o=1).broadcast_to([128, a, 64 // a])

                R_ = lambda k: ("rt", k)
                rb_keys = [("rb4", i_) for i_ in range(4)]
                lg3 = lg[:, 0:64].rearrange("p (a b) -> p a b", a=4)
                S.dve(TRD(rt[0][:, 0:4], lg3, ALU.max), ["pf6"], [R_(0)])
                S.dve(TT(v3(rt[1], 4), lg3, bcl(rt[0], 4), ALU.subtract), ["pf6", R_(0)], [R_(1)])
                S.act(AV(rt[1][:], rt[1][:], AF.Exp), [R_(1)], [R_(1)])
                S.dve(TRD(rt[0][:, 0:4], v3(rt[1], 4), ALU.add), [R_(1)], [R_(0)])
                S.dve(RCP(rt[0][:, 0:4], rt[0][:, 0:4]), [R_(0)], [R_(0)])
                S.dve(TT(v3(rt[2], 4), v3(rt[1], 4), bcl(rt[0], 4), ALU.mult), [R_(1), R_(0)], [R_(2)])
                S.dve(TT(rt[3][:], rt[2][:], rb4[:].rearrange("p a b -> p (a b)"), ALU.add), [R_(2)] + rb_keys, [R_(3)])
                S.dve(TRD(rt[4][:, 0:16], v3(rt[3], 16), ALU.max), [R_(3)], [R_(4)])
                S.dve(TT(v3(rt[5], 16), v3(rt[3], 16), bcl(rt[4], 16), ALU.is_equal), [R_(3), R_(4)], [R_(5)])
                S.dve(STT(rt[5][:], rt[5][:], NEG, rt[3][:], ALU.mult, ALU.add), [R_(5), R_(3)], [R_(5)])
                S.dve(TRD(rt[6][:, 0:16], v3(rt[5], 16), ALU.max), [R_(5)], [R_(6)])
                S.dve(TT(rt[4][:, 0:16], rt[4][:, 0:16], rt[6][:, 0:16], ALU.add), [R_(4), R_(6)], [R_(4)])
                gs3 = rt[4][:, 0:16].rearrange("p (a b) -> p a b", a=4)
                S.dve(TRD(rt[6][:, 0:4], gs3, ALU.max), [R_(4)], [R_(6)])
                S.dve(TT(rt[7][:, 0:16].rearrange("p (a b) -> p a b", a=4), gs3,
                         rt[6][:, 0:4].rearrange("p (a o) -> p a o", o=1).broadcast_to([128, 4, 4]), ALU.is_equal),
                      [R_(4), R_(6)], [R_(7)])
                S.dve(TS(rt[7][:, 0:16], rt[7][:, 0:16], -1.0, -NEG, ALU.add, ALU.mult), [R_(7)], [R_(7)])
                S.dve(TT(v3(rt[8], 16), v3(rt[3], 16), bcl(rt[7], 16), ALU.add), [R_(3), R_(7)], [R_(8)])
                S.dve(TRD(rt[6][:, 0:4], v3(rt[8], 4), ALU.max), [R_(8)], [R_(6)])
                S.dve(TT(v3(rt[9], 4), v3(rt[8], 4), bcl(rt[6], 4), ALU.is_equal), [R_(8), R_(6)], [R_(9)])
                S.dve(STT(rt[8][:], rt[9][:], NEG, rt[8][:], ALU.mult, ALU.add), [R_(9), R_(8)], [R_(8)])
                S.dve(TRD(rt[6][:, 0:4], v3(rt[8], 4), ALU.max), [R_(8)], [R_(6)])
                S.dve(TT(v3(rt[10], 4), v3(rt[8], 4), bcl(rt[6], 4), ALU.is_equal), [R_(8), R_(6)], [R_(10)])
                S.dve(TT(rt[9][:], rt[9][:], rt[10][:], ALU.add), [R_(9), R_(10)], [R_(9)])
                S.dve(TT(rt[9][:], rt[9][:], rt[2][:], ALU.mult), [R_(9), R_(2)], [R_(9)])
                S.dve(TRD(rt[6][:, 0:4], v3(rt[9], 4), ALU.add), [R_(9)], [R_(6)])
                S.dve(RCP(rt[6][:, 0:4], rt[6][:, 0:4]), [R_(6)], [R_(6)])
                S.dve(TS(rt[6][:, 0:4], rt[6][:, 0:4], float(1.0 / ALPHA), None, ALU.mult), [R_(6)], [R_(6)])
                S.dve(TT(gates[:, 4 * tg:4 * tg + 4, :], v3(rt[9], 4), bcl(rt[6], 4), ALU.mult), [R_(9), R_(6)], [("gates", tg)])
            if dbg_is("gates"):
                S.barrier()
                S.dve(CP(lnA[0][:, 0:256], gates[:].rearrange("p a b -> p (a b)")), [], ["Xd"])
                dump2(lnA[0][:])
                stop[0] = True
                break
            S.barrier()
            load_bc(0, mod_d[l, s:s + 1, 5 * D:6 * D], bmod_d[l:l + 1, 5 * D:6 * D])
            load_bc(1, ln2g_d[l:l + 1, :])
            load_bc(2, ln2b_d[l:l + 1, :])
            nexp = 1 if dbg_is("moe1") else NE

            def load_expert_dma(e_):
                sl = e_ % 2
                for gu, wsrc in ((0, wg_d), (1, wu_d)):
                    S.dma("pool", DM(wgu[sl][:, :, gu, :], wsrc[l, e_].rearrange("(k p) n -> p k n", p=128)),
                          [], [("wgu", sl, gu, 0), ("wgu", sl, gu, 1)])
                for h in range(2):
                    S.dma("sp", DM(stg[h][:].rearrange("p (k n) -> p k n", n=D),
                                   wd_d[l, e_, 256 * h:256 * h + 256, :].rearrange("(k p) n -> p k n", p=128)),
                          [], [("stg", h)])

            def load_expert_cast(e_):
                sl = e_ % 2
                for h in range(2):
                    for kk in range(2):
                        S.dve(TT(wdb[sl][:, 2 * h + kk, :], stg[h][:, kk * D:(kk + 1) * D], bc[0][:], ALU.mult),
                              [("stg", h), ("bc", 0)], [("wd", sl, 2 * h + kk)])

            stages = [(e_, tc) for e_ in range(nexp) for tc in range(4)]

            def emit_gu(si):
                e_, tc = stages[si]
                sl = e_ % 2
                asl = si % 2
                for f in range(4):
                    Gp, Gk = PF[f % 2], "pf%d" % (f % 2)
                    Up, Uk = PF[2 + f % 2], "pf%d" % (2 + f % 2)
                    for gu, pp, pk in ((0, Gp, Gk), (1, Up, Uk)):
                        for k in range(8):
                            S.pe(MM(pp[:, :], wgu[sl][:, k, gu, f * 128:(f + 1) * 128], hT[:, k, tc * 512:(tc + 1) * 512],
                                    k == 0, k == 7),
                                 [("wgu", sl, gu, 0), ("wgu", sl, gu, 1)] + [("hT", 4 * tc + j) for j in range(4)], [pk])
                    S.act(AV(sg[f % 2][:], Gp[:, :], AF.Silu), [Gk], [("sg", f % 2)])
                    S.dve(TT(aT[asl][:, f, :], Up[:, :], sg[f % 2][:], ALU.mult), [Uk, ("sg", f % 2)], [("aT", asl, f)])

            def emit_y(si):
                e_, tc = stages[si]
                sl = e_ % 2
                asl = si % 2
                for tt in range(4):
                    i = 4 * tc + tt
                    for half in range(2):
                        Yp, Yk = PF[4 + half], "pf%d" % (4 + half)
                        for f in range(4):
                            S.pe(MM(Yp[:, :], aT[asl][:, f, tt * 128:(tt + 1) * 128], wdb[sl][:, f, half * 512:(half + 1) * 512],
                                    f == 0, f == 3), [("aT", asl, f), ("wd", sl, f)], [Yk])
                        S.dve(STT(X[:, i, half * 512:(half + 1) * 512], Yp[:, :], gates[:, i, e_:e_ + 1],
                                  X[:, i, half * 512:(half + 1) * 512], ALU.mult, ALU.add),
                              [Yk, ("gates", i // 4), ("X", i)], [("X", i)])

            load_expert_dma(0)
            load_expert_cast(0)
            for si in range(len(stages)):
                e_, tc = stages[si]
                emit_gu(si)
                if si > 0:
                    emit_y(si - 1)
                if tc == 0 and e_ + 1 < nexp:
                    load_expert_dma(e_ + 1)
                if tc == 2 and e_ + 1 < nexp:
                    load_expert_cast(e_ + 1)
            emit_y(len(stages) - 1)
            if dbg_is("moe1") or dbg_is("X2"):
                S.barrier()
                dump2(X[:, dbg[2], :])
                stop[0] = True
                break
            for i in range(NT):
                ln_stats(i, X[:, i, :], [("X", i)])
            ln_finish(float(EPS / (ALPHA * ALPHA)))
            for i in range(NT):
                sl = i % 2
                ln_apply(i, X[:, i, :], [("X", i)], lnA[sl][:], ["lnA%d" % sl], bc[1][:], [("bc", 1)],
                         bc[2][:], [("bc", 2)], lnA[sl][:], ["lnA%d" % sl])
                S.dma("pool", DM(xout[s, i * 128:(i + 1) * 128, :], lnA[sl][:]),
                      ["lnA%d" % sl], [("xs", s, i)] if xout is xs_d else [("out", s, i)])
        if stop[0]:
            break
    S.barrier()
    S.emit(nc)
    return nc


_NC_CACHE = {}


def kernel(**inputs):
    if "nc" not in _NC_CACHE:
        _NC_CACHE["nc"] = build()
    nc = _NC_CACHE["nc"]
    f = lambda a: np.ascontiguousarray(np.asarray(a, dtype=np.float32))
    x = f(inputs["x"])
    c = f(inputs["c"])
    shared = {k: f(inputs[k]) for k in ("w_mod", "b_mod", "w_in", "w_pool", "pool_scale", "w_out", "ln1_g", "ln1_b",
                                        "w_router", "w_gate", "w_up", "w_down", "ln2_g", "ln2_b")}
    shared["router_bias"] = f(inputs["router_bias"]).reshape(1, NE)
    in_maps = []
    for core in range(8):
        m = dict(shared)
        m["x"] = x[2 * core:2 * core + 2]
        m["c"] = c[2 * core:2 * core + 2]
        in_maps.append(m)
    res = run_bass_kernel_spmd(nc, in_maps, core_ids=list(range(8)))
    return np.concatenate([r["out"] for r in res.results], axis=0)
```

```python
import contextlib
import math
import numpy as np
import concourse.bass as bass
import concourse.mybir as mybir
from concourse.bass_utils import run_bass_kernel_spmd

F32 = mybir.dt.float32
BF16 = mybir.dt.bfloat16
I32 = mybir.dt.int32
AF = mybir.ActivationFunctionType
ALU = mybir.AluOpType
AX = mybir.AxisListType

ENGS = ["pe", "act", "dve", "pool", "sp"]
_EMBED_WAIT = True
DMA_RING = {"sp": 8, "act": 4, "pool": 6}


class Op:
    __slots__ = ("idx", "eng", "fn", "reads", "writes", "dma", "deps", "signal",
                 "sig", "ring_wait", "extra", "is_bar")

    def __init__(self, idx, eng, fn, reads, writes, dma):
        self.idx = idx
        self.eng = eng
        self.fn = fn
        self.reads = tuple(reads)
        self.writes = tuple(writes)
        self.dma = dma
        self.deps = ()
        self.signal = False
        self.sig = None
        self.ring_wait = None
        self.extra = ()
        self.is_bar = False


class Sched:
    def __init__(self):
        self.ops = []

    def add(self, eng, fn, reads=(), writes=(), dma=False):
        op = Op(len(self.ops), eng, fn, reads, writes, dma)
        self.ops.append(op)
        return op

    def pe(self, fn, reads=(), writes=()):
        return self.add("pe", fn, reads, writes)

    def act(self, fn, reads=(), writes=()):
        return self.add("act", fn, reads, writes)

    def dve(self, fn, reads=(), writes=()):
        return self.add("dve", fn, reads, writes)

    def pool(self, fn, reads=(), writes=()):
        return self.add("pool", fn, reads, writes)

    def dma(self, q, fn, reads=(), writes=()):
        return self.add(q, fn, reads, writes, dma=True)

    def barrier(self):
        last = {}
        dmas = {q: [] for q in DMA_RING}
        for op in self.ops:
            if op.is_bar:
                continue
            if op.dma:
                dmas[op.eng].append(op.idx)
            elif op.fn is not None:
                last[op.eng] = op.idx
        extra = list(last.values())
        for q, lst in dmas.items():
            extra.extend(lst[-DMA_RING[q]:])
        for e in ENGS:
            op = self.add(e, None)
            op.extra = tuple(extra)
            op.is_bar = True

    def analyze(self):
        last_w = {}
        readers = {}
        for op in self.ops:
            if op.is_bar:
                op.deps = tuple(sorted(d for d in op.extra
                                       if not (self.ops[d].eng == "pe" and op.eng == "pe")))
                continue
            deps = set()
            for k in op.reads:
                w = last_w.get(k)
                if w is not None:
                    deps.add(w)
            for k in op.writes:
                w = last_w.get(k)
                if w is not None:
                    deps.add(w)
                deps.update(readers.get(k, {}).values())
            deps.discard(op.idx)
            pruned = []
            for d in deps:
                p = self.ops[d]
                if p.eng == "pe" and op.eng == "pe" and not p.dma and not op.dma:
                    continue
                pruned.append(d)
            op.deps = tuple(sorted(pruned))
            for k in op.reads:
                rk = (op.eng, op.idx) if op.dma else op.eng
                readers.setdefault(k, {})[rk] = op.idx
            for k in op.writes:
                last_w[k] = op.idx
                readers[k] = {}
        for op in self.ops:
            for d in op.deps:
                self.ops[d].signal = True
        cnt = {e: 0 for e in ENGS}
        dcnt = {}
        dnum = {q: 0 for q in DMA_RING}
        for op in self.ops:
            if op.dma:
                q = op.eng
                i = dnum[q]
                dnum[q] += 1
                key = ("d", q, i % DMA_RING[q])
                prev = dcnt.get(key, 0)
                if prev > 0:
                    op.ring_wait = (key, prev)
                dcnt[key] = prev + 16
                op.sig = (key, prev + 16)
                op.signal = True
            elif op.signal:
                cnt[op.eng] += 1
                op.sig = (("c", op.eng), cnt[op.eng])

    def emit(self, nc):
        self.analyze()
        streams = {e: [o for o in self.ops if o.eng == e] for e in ENGS}
        with contextlib.ExitStack() as st:
            sems = {}
            for e in ENGS:
                sems[("c", e)] = st.enter_context(nc.semaphore("c_" + e))
            for q, n in DMA_RING.items():
                for i in range(n):
                    sems[("d", q, i)] = st.enter_context(nc.semaphore("d_%s%d" % (q, i)))
            block = st.enter_context(nc.Block())
            ops = self.ops

            def run(ename, eng):
                known = {}
                for op in streams[ename]:
                    waits = []
                    if op.ring_wait is not None:
                        waits.append(op.ring_wait)
                    for d in op.deps:
                        waits.append(ops[d].sig)
                    best = {}
                    for k, v in waits:
                        if v > best.get(k, 0):
                            best[k] = v
                    pend = []
                    for k, v in best.items():
                        if known.get(k, 0) >= v:
                            continue
                        pend.append((k, v))
                        known[k] = v
                    attach = None
                    if _EMBED_WAIT and pend and op.fn is not None and not op.dma and ename == 'pe':
                        attach = pend.pop()
                    for k, v in pend:
                        eng.wait_ge(sems[k], v)
                    if op.fn is None:
                        continue
                    ins = op.fn(eng)
                    if attach is not None:
                        ins._wait_ge(sems[attach[0]], attach[1])
                    if op.signal:
                        assert ins is not None
                        ins.then_inc(sems[op.sig[0]], 16 if op.dma else 1)

            @block.tensor
            def _(e):
                run("pe", e)

            @block.scalar
            def _(e):
                run("act", e)

            @block.vector
            def _(e):
                run("dve", e)

            @block.gpsimd
            def _(e):
                run("pool", e)

            @block.sync
            def _(e):
                run("sp", e)


D = 1024
SEQ = 2048
NT = 16
NL = 2
NE = 16
ALPHA = float(4.0 ** 0.25)
EPS = 1e-5
NEG = -1.0e30
BIGB = 30000.0
INVF = [float(np.float32(500000.0) ** np.float32(-(i * 2.0 / 16.0))) for i in range(8)]
TWO_PI = float(2 * np.pi)
PI = float(np.pi)


def build(nseq=2, nlayers=NL, dbg=None):
    nc = bass.Bass("TRN2", target_bir_lowering=False)

    def din(name, shape):
        return nc.dram_tensor(name, shape, F32, kind="ExternalInput").ap()

    x_d = din("x", [2, SEQ, D])
    c_d = din("c", [2, D])
    wmod_d = din("w_mod", [NL, D, 6 * D])
    bmod_d = din("b_mod", [NL, 6 * D])
    win_d = din("w_in", [NL, D, 2048])
    wpool_d = din("w_pool", [NL, 4, 128, 128])
    pscale_d = din("pool_scale", [NL, 512])
    wout_d = din("w_out", [NL, D, D])
    ln1g_d = din("ln1_g", [NL, D])
    ln1b_d = din("ln1_b", [NL, D])
    wr_d = din("w_router", [D, NE])
    rb_d = din("router_bias", [1, NE])
    wg_d = din("w_gate", [NL, NE, D, 512])
    wu_d = din("w_up", [NL, NE, D, 512])
    wd_d = din("w_down", [NL, NE, 512, D])
    ln2g_d = din("ln2_g", [NL, D])
    ln2b_d = din("ln2_b", [NL, D])
    out_d = nc.dram_tensor("out", [2, SEQ, D], F32, kind="ExternalOutput").ap()
    xs_d = nc.dram_tensor("xs_scr", [2, SEQ, D], F32, kind="Internal").ap()
    mod_d = nc.dram_tensor("mod_scr", [NL, 2, 6 * D], F32, kind="Internal").ap()
    dbg_d = None
    if dbg is not None:
        dbg_d = nc.dram_tensor("dbg", [128, dbg[1]], F32, kind="ExternalOutput").ap()

    S = Sched()

    B0 = 16640
    LIMIT = 229376

    def at(name, shape, dt, off):
        assert off % 32 == 0, (name, off)
        nb = int(np.prod(shape[1:])) * (4 if dt in (F32, I32) else 2)
        assert off + nb <= LIMIT, (name, off, nb)
        return nc.alloc_sbuf_tensor_at(name, shape, dt, offset=off)

    hT = at("hT", [128, 8, SEQ], BF16, B0)
    R = B0 + 32768
    qkT = at("qkT", [128, 8, SEQ], BF16, R)
    wbf = [at("wbf%d" % i, [128, 8, 512], BF16, R + 32768 + 8192 * i) for i in range(2)]
    stg = [at("stg%d" % i, [128, 2048], F32, R + 49152 + 8192 * i) for i in range(2)]
    wo = at("wo", [128, 8, D], BF16, R + 32768)
    vaug = at("vaug", [128, NT, 8, 65], BF16, R + 65536)
    dT = at("dT", [128, 4, SEQ], BF16, R + 82176)
    pbuf = at("pbuf", [128, 16 + SEQ], F32, R + 98560)
    tmpA = at("tmpA", [128, 16 + SEQ], F32, R + 106816)
    tmpB = at("tmpB", [128, 16 + SEQ], F32, R + 115072)
    probs = [at("probs%d" % i, [128, 512], BF16, R + 98560 + 1024 * i) for i in range(3)]
    probs.append(at("probs3", [128, 512], BF16, R + 105728))
    biasT = at("biasT", [128, 1024], BF16, R + 101632)
    apair = [at("apair%d" % i, [128, 4, 128], BF16, R + 103680 + 1024 * i) for i in range(2)]
    qktok = [at("qktok%d" % i, [128, 512], BF16, R + 123328 + 1024 * i) for i in range(2)]
    esel = at("esel", [128, 64, 128], BF16, R + 106816)
    wpst = at("wpst", [128, 4, 128], F32, R + 125376)
    wpbf = at("wpbf", [128, 4, 128], BF16, R + 127424)
    wgu = [at("wgu%d" % i, [128, 8, 2, 512], BF16, R + 16384 * i) for i in range(2)]
    wdb = [at("wdb%d" % i, [128, 4, D], BF16, R + 32768 + 8192 * i) for i in range(2)]
    X = at("X", [128, NT, D], F32, R + 65536)
    Pp = R + 131072
    lnA = [at("lnA%d" % i, [128, D], F32, Pp + 4096 * i) for i in range(2)]
    htok = [at("htok%d" % i, [128, D], BF16, Pp + 8192 + 2048 * i) for i in range(2)]
    bc = [at("bc%d" % i, [128, D], F32, Pp + 12288 + 4096 * i) for i in range(4)]
    aT = [at("aT%d" % i, [128, 4, 512], BF16, Pp + 28672 + 4096 * i) for i in range(2)]
    sg = [at("sg%d" % i, [128, 512], F32, Pp + 36864 + 2048 * i) for i in range(2)]
    cpos = [Pp + 40960]

    def small(name, shape, dt):
        nb = int(np.prod(shape[1:])) * (4 if dt in (F32, I32) else 2)
        nb = (nb + 31) // 32 * 32
        t = at(name, shape, dt, cpos[0])
        cpos[0] += nb
        return t

    ident = small("ident", [128, 128], BF16)
    tri = small("tri", [128, 128], BF16)
    cos_t = small("cos_t", [128, NT, 8], F32)
    sin_t = small("sin_t", [128, NT, 8], F32)
    pastm = small("pastm", [128, 4, 64], F32)
    corr = small("corr", [128, 4, 16], F32)
    rb4 = small("rb4", [128, 4, NE], F32)
    wrb = small("wrb", [128, 8, NE], BF16)
    wrst = small("wrst", [128, 8, NE], F32)
    pscol = small("pscol", [128, NL, 4], F32)
    condT = small("condT", [128, 8, 2], F32)
    st6 = [small("st6_%d" % i, [128, 12], F32) for i in range(2)]
    mv = [small("mv%d" % i, [128, 2], F32) for i in range(2)]
    rstd = [small("rstd%d" % i, [128, 1], F32) for i in range(2)]
    nmr = [small("nmr%d" % i, [128, 1], F32) for i in range(2)]
    rcp = small("rcp", [128, 4], F32)
    gates = small("gates", [128, NT, NE], F32)
    rtb = Pp + 28672
    rt = [at("rt%d" % i, [128, 64], F32, rtb + 256 * i) for i in range(14)]
    rope_t = [at("rope_t%d" % i, [128, 8, 8], F32, rtb + 4096 + 256 * i) for i in range(4)]
    KB32 = at("KB32", [128, 4, 64], F32, rtb + 5120)
    KB = at("KB", [128, 4, 64], BF16, rtb + 6144)
    gm = at("gm", [128, 64], F32, rtb + 6656)
    srt = at("srt", [128, 64], F32, rtb + 6912)
    selt = at("selt", [128, 64], F32, rtb + 7168)
    btok = at("btok", [128, 128], BF16, rtb + 7424)
    modrow = at("modrow", [2, 512], F32, Pp + 36864)

    stA = small("stA", [128, NT, 12], F32)
    mvA = small("mvA", [128, NT, 2], F32)
    rsA = small("rsA", [128, NT], F32)
    nmA = small("nmA", [128, NT], F32)
    PF = [nc.alloc_psum_tensor("pf%d" % i, [128, 512], F32) for i in range(7)]
    PB = nc.alloc_psum_tensor("pb", [128, 1024], BF16)

    def MM(out, lhsT, rhs, st, sp):
        return lambda e: e.matmul(out, lhsT=lhsT, rhs=rhs, start=st, stop=sp)

    def TR(out, in_):
        idn = ident[:]
        return lambda e: e.transpose(out=out, in_=in_, identity=idn)

    def TT(out, in0, in1, op):
        return lambda e: e.tensor_tensor(out=out, in0=in0, in1=in1, op=op)

    def TS(out, in0, s1, s2, op0, op1=None):
        if op1 is None:
            return lambda e: e.tensor_scalar(out=out, in0=in0, scalar1=s1, scalar2=None, op0=op0)
        return lambda e: e.tensor_scalar(out=out, in0=in0, scalar1=s1, scalar2=s2, op0=op0, op1=op1)

    def STT(out, in0, scalar, in1, op0, op1):
        return lambda e: e.scalar_tensor_tensor(out=out, in0=in0, scalar=scalar, in1=in1, op0=op0, op1=op1)

    def CP(out, in_):
        return lambda e: e.tensor_copy(out=out, in_=in_)

    def ACP(out, in_):
        return lambda e: e.copy(out=out, in_=in_)

    def AV(out, in_, func, bias=None, scale=None):
        kw = {}
        if bias is not None:
            kw["bias"] = bias
        if scale is not None:
            kw["scale"] = scale
        return lambda e: e.activation(out=out, in_=in_, func=func, **kw)

    def DM(out, in_, **kw):
        return lambda e: e.dma_start(out=out, in_=in_, **kw)

    def MS(ap, val):
        return lambda e: e.memset(ap, val)

    def TRD(out, in_, op):
        return lambda e: e.tensor_reduce(out=out, in_=in_, axis=AX.X, op=op)

    def RCP(out, in_):
        return lambda e: e.reciprocal(out=out, in_=in_)

    def MAX8(out, in_):
        return lambda e: e.max(out=out, in_=in_)

    def range_reduce(a, t_i, t_f, t_m):
        S.dve(TS(t_f, a, float(1.0 / TWO_PI), None, ALU.mult), ["ang"], ["rr_f"])
        S.dve(CP(t_i, t_f), ["rr_f"], ["rr_i"])
        S.dve(CP(t_f, t_i), ["rr_i"], ["rr_f"])
        S.dve(STT(a, t_f, -TWO_PI, a, ALU.mult, ALU.add), ["rr_f", "ang"], ["ang"])
        S.dve(TS(t_m, a, PI, None, ALU.is_gt), ["ang"], ["rr_m"])
        S.dve(STT(a, t_m, -TWO_PI, a, ALU.mult, ALU.add), ["rr_m", "ang"], ["ang"])
        S.dve(TS(t_m, a, -PI, None, ALU.is_lt), ["ang"], ["rr_m"])
        S.dve(STT(a, t_m, TWO_PI, a, ALU.mult, ALU.add), ["rr_m", "ang"], ["ang"])

    S.pool(MS(ident[:], 1.0), [], ["ident"])
    S.pool(lambda e: e.affine_select(out=ident[:], in_=ident[:], pattern=[[-1, 128]], compare_op=ALU.is_equal,
                                     fill=0.0, base=0, channel_multiplier=1), ["ident"], ["ident"])
    S.pool(MS(tri[:], 1.0), [], ["tri"])
    S.pool(lambda e: e.affine_select(out=tri[:], in_=tri[:], pattern=[[1, 128]], compare_op=ALU.is_ge,
                                     fill=0.0, base=0, channel_multiplier=-1), ["tri"], ["tri"])
    posi = at("posi", [128, NT], I32, Pp)
    posf = at("posf", [128, NT], F32, Pp + 64)
    angc = at("angc", [128, NT * 8], F32, Pp + 128)
    angs = at("angs", [128, NT * 8], F32, Pp + 128 + 512)
    rr_i = at("rr_i", [128, NT * 8], I32, Pp + 128 + 1024)
    rr_f = at("rr_f", [128, NT * 8], F32, Pp + 128 + 1536)
    rr_m = at("rr_m", [128, NT * 8], F32, Pp + 128 + 2048)
    S.pool(lambda e: e.iota(posi[:], pattern=[[128, NT]], base=0, channel_multiplier=1), [], ["posi"])
    S.dve(CP(posf[:], posi[:]), ["posi"], ["posf"])
    angs3 = angs[:].rearrange("p (i f) -> p i f", f=8)
    for f in range(8):
        S.dve(TS(angs3[:, :, f], posf[:], INVF[f], None, ALU.mult), ["posf"], ["ang"])
    S.dve(TS(angc[:], angs[:], float(PI / 2), None, ALU.add), ["ang"], ["angc"])
    range_reduce(angs[:], rr_i[:], rr_f[:], rr_m[:])
    S.act(AV(sin_t[:].rearrange("p i f -> p (i f)"), angs[:], AF.Sin), ["ang"], ["sin_t"])
    S.dve(CP(angs[:], angc[:]), ["angc", "sin_t"], ["ang"])
    range_reduce(angs[:], rr_i[:], rr_f[:], rr_m[:])
    S.act(AV(cos_t[:].rearrange("p i f -> p (i f)"), angs[:], AF.Sin), ["ang"], ["cos_t"])
    S.pool(MS(pastm[:], 0.0), [], ["pastm"])
    for jj in range(4):
        pv = pastm[:, jj, :].rearrange("p (h n) -> p h n", n=8)
        S.pool(MS(pv[:, :, 4 + jj:8], NEG), ["pastm"], ["pastm"])
    S.pool(MS(corr[:], 1.0), [], ["corr"])
    for g in range(4):
        w = 2 ** (g + 1)
        for t in range(w - 1):
            S.pool(MS(corr[:, g, t:t + 1], float(w) / float(t + 1)), ["corr"], ["corr"])
    S.dma("sp", DM(wrst[:], wr_d.rearrange("(k p) n -> p k n", p=128)), [], ["wrst"])
    S.dve(CP(wrb[:], wrst[:]), ["wrst"], ["wrb"])
    for i in range(4):
        S.dma("sp", DM(rb4[:, i, :], rb_d.partition_broadcast(128)), [], [("rb4", i)])
    for l in range(NL):
        S.dma("sp", DM(pscol[:, l, :], pscale_d[l].rearrange("(g p) -> p g", p=128), allow_slow_non_contiguous=True),
              [], [("pscol", l)])
    for k in range(8):
        S.dma("sp", DM(condT[:, k, :], c_d[:, k * 128:(k + 1) * 128].rearrange("b p -> p b"),
                       allow_slow_non_contiguous=True), [], [("condT", k)])
    S.act(AV(condT[:], condT[:], AF.Silu), [("condT", k) for k in range(8)], ["condT"])
    NWM = 6
    wm = [at("wm%d" % i, [128, 8, 512], BF16, R + 8192 * i) for i in range(NWM)]
    condTb = small("condTb", [128, 8, 2], BF16)
    S.dve(CP(condTb[:], condT[:]), ["condT"], ["condTb"])
    modbig = at("modbig", [2, 6 * D], F32, R + 65536)
    gi = 0
    for l in range(nlayers):
        for cg in range(12):
            sl = gi % NWM
            pfm = PF[gi % 2]
            pfk = "pf%d" % (gi % 2)
            S.dma("pool", DM(wm[sl][:], wmod_d[l, :, cg * 512:(cg + 1) * 512].rearrange("(k p) n -> p k n", p=128)),
                  [], [("wm", sl)])
            for k in range(8):
                S.pe(MM(pfm[0:2, :], condTb[:, k, :], wm[sl][:, k, :], k == 0, k == 7),
                     [("wm", sl), "condTb"], [pfk])
            S.act(ACP(modbig[:, cg * 512:(cg + 1) * 512], pfm[0:2, :]), [pfk], [("modbig", cg)])
            gi += 1
        S.dma("sp", DM(mod_d[l, :, :], modbig[:]), [("modbig", cg_) for cg_ in range(12)],
              [("mod_d", l, cg_) for cg_ in range(12)])
    mod_keys = [("mod_d", l, cg) for l in range(nlayers) for cg in range(12)]

    def load_bc(slot, src_row, add_row=None, plus1=False):
        key = ("bc", slot)
        S.dma("sp", DM(bc[slot][:], src_row.partition_broadcast(128)), [], [key])
        if add_row is not None:
            S.dma("sp", DM(lnA[1][:], add_row.partition_broadcast(128)), [], ["lnA1"])
            if plus1:
                S.dve(STT(bc[slot][:], bc[slot][:], 1.0, lnA[1][:], ALU.add, ALU.add), [key, "lnA1"], [key])
            else:
                S.dve(TT(bc[slot][:], bc[slot][:], lnA[1][:], ALU.add), [key, "lnA1"], [key])

    def ln_tile(src, src_keys, work, work_keys, gain, gain_keys, bias, bias_keys, out, out_keys, sl, add_eng="pool"):
        kst, kmv, krs, knm = "st6_%d" % sl, "mv%d" % sl, "rstd%d" % sl, "nmr%d" % sl
        st_, mv_, rs_, nm_ = st6[sl], mv[sl], rstd[sl], nmr[sl]
        S.dve(lambda e: e.bn_stats(out=st_[:, 0:6], in_=src[:, 0:512]), src_keys, [kst + "a"])
        S.dve(lambda e: e.bn_stats(out=st_[:, 6:12], in_=src[:, 512:1024]), src_keys, [kst + "b"])
        S.dve(lambda e: e.bn_aggr(out=mv_[:], in_=st_[:]), [kst + "a", kst + "b"], [kmv])
        S.dve(TS(rs_[:], mv_[:, 1:2], EPS, None, ALU.add), [kmv], [krs])
        S.act(lambda e: e.sqrt(out=rs_[:], in_=rs_[:]), [krs], [krs])
        S.dve(RCP(rs_[:], rs_[:]), [krs], [krs])
        S.dve(STT(nm_[:], mv_[:, 0:1], -1.0, rs_[:], ALU.mult, ALU.mult), [kmv, krs], [knm])
        S.act(AV(work, src, AF.Identity, bias=nm_[:], scale=rs_[:]), list(src_keys) + [krs, knm], work_keys)
        S.dve(TT(work, work, gain, ALU.mult), list(work_keys) + list(gain_keys), work_keys)
        S.add(add_eng, TT(out, work, bias, ALU.add), list(work_keys) + list(bias_keys), out_keys)

    def ln_stats(i, src, src_keys):
        S.dve(lambda e: e.bn_stats(out=stA[:, i, 0:6], in_=src[:, 0:512]), src_keys, [("stA", i, 0)])
        S.dve(lambda e: e.bn_stats(out=stA[:, i, 6:12], in_=src[:, 512:1024]), src_keys, [("stA", i, 1)])
        S.dve(lambda e: e.bn_aggr(out=mvA[:, i, :], in_=stA[:, i, :]), [("stA", i, 0), ("stA", i, 1)], [("mvA", i)])

    def ln_finish(eps=EPS):
        mvk = [("mvA", i_) for i_ in range(NT)]
        S.dve(TS(rsA[:], mvA[:, :, 1], eps, None, ALU.add), mvk, ["rsA"])
        S.act(lambda e: e.sqrt(out=rsA[:], in_=rsA[:]), ["rsA"], ["rsA"])
        S.dve(RCP(rsA[:], rsA[:]), ["rsA"], ["rsA"])
        S.dve(STT(nmA[:], mvA[:, :, 0], -1.0, rsA[:], ALU.mult, ALU.mult), mvk + ["rsA"], ["nmA"])

    def ln_apply(i, src, src_keys, work, work_keys, gain, gain_keys, bias, bias_keys, out, out_keys):
        S.act(AV(work, src, AF.Identity, bias=nmA[:, i:i + 1], scale=rsA[:, i:i + 1]),
              list(src_keys) + ["rsA", "nmA"], work_keys)
        S.dve(TT(work, work, gain, ALU.mult), list(work_keys) + list(gain_keys), work_keys)
        S.dve(TT(out, work, bias, ALU.add), list(work_keys) + list(bias_keys), out_keys)

    def transpose_to_hT(i, sl):
        for k in range(8):
            S.pe(TR(PB[:, k * 128:(k + 1) * 128], htok[sl][:, k * 128:(k + 1) * 128]), [("htok", sl), "ident"], ["pb"])
        S.act(ACP(hT[:, :, i * 128:(i + 1) * 128], PB[:, :].rearrange("p (k t) -> p k t", t=128)), ["pb"], [("hT", i)])

    def dump2(ap0, ap1=None):
        S.barrier()
        S.dma("sp", DM(dbg_d[:, 0:1024], ap0), [], ["dbg0"])
        if ap1 is not None:
            S.dma("sp", DM(dbg_d[:, 1024:2048], ap1), [], ["dbg1"])

    def dbg_is(name):
        return dbg is not None and dbg[0] == name

    stop = [False]

    for s in range(nseq):
        for l in range(nlayers):
            if stop[0]:
                break
            xin = x_d if l == 0 else xs_d
            xout = out_d if l == nlayers - 1 else xs_d
            S.barrier()
            load_bc(0, mod_d[l, s:s + 1, 1 * D:2 * D], bmod_d[l:l + 1, 1 * D:2 * D], plus1=True)
            load_bc(1, mod_d[l, s:s + 1, 0:D], bmod_d[l:l + 1, 0:D])
            for gpre in range(2):
                S.dma("pool", DM(wbf[gpre][:], win_d[l, :, gpre * 512:(gpre + 1) * 512].rearrange("(k p) n -> p k n", p=128)),
                      [], [("wbf", gpre, 0), ("wbf", gpre, 1)])
            for i in range(NT):
                S.dma("sp", DM(X[:, i, :], xin[s, i * 128:(i + 1) * 128, :]),
                      [("xs", s, i)] if l > 0 else [], [("X", i)])
                ln_stats(i, X[:, i, :], [("X", i)])
            ln_finish()
            for i in range(NT):
                sl = i % 2
                ln_apply(i, X[:, i, :], [("X", i)], lnA[sl][:], ["lnA%d" % sl], bc[0][:], [("bc", 0)],
                         bc[1][:], [("bc", 1)], htok[sl][:], [("htok", sl)])
                transpose_to_hT(i, sl)
            S.barrier()
            if dbg_is("hT"):
                S.barrier()
                S.dve(CP(lnA[0][:], hT[:, 0, 0:1024]), [], ["Xd"])
                dump2(lnA[0][:])
                stop[0] = True
                break
            S.pool(MS(vaug[:, :, :, 64:65], 1.0), [], ["vones"])

            def load_win(gidx, sl):
                S.dma("pool", DM(wbf[sl][:], win_d[l, :, gidx * 512:(gidx + 1) * 512].rearrange("(k p) n -> p k n", p=128)),
                      [], [("wbf", sl, 0), ("wbf", sl, 1)])

            for gidx in range(4):
                sl = gidx % 2
                if gidx >= 2:
                    load_win(gidx, sl)
                wkeys = [("wbf", sl, 0), ("wbf", sl, 1)]
                if gidx < 3:
                    def proj_mm(i):
                        pf = PF[i % 2]
                        pk = "pf%d" % (i % 2)
                        for k in range(8):
                            S.pe(MM(pf[:, :], hT[:, k, i * 128:(i + 1) * 128], wbf[sl][:, k, :], k == 0, k == 7),
                                 [("hT", i)] + wkeys, [pk])

                    def proj_post(i):
                        pf = PF[i % 2]
                        pk = "pf%d" % (i % 2)
                        ps3 = pf[:, :].rearrange("p (h d) -> p h d", d=64)
                        if gidx == 2:
                            S.act(ACP(vaug[:, i, :, 0:64], ps3), [pk], [("v", i)])
                            return
                        qs = i % 2
                        o3 = qktok[qs][:].rearrange("p (h d) -> p h d", d=64)
                        cb = cos_t[:, i:i + 1, :].broadcast_to([128, 8, 8])
                        sb_ = sin_t[:, i:i + 1, :].broadcast_to([128, 8, 8])
                        x1 = ps3[:, :, 0:8]
                        x2 = ps3[:, :, 8:16]
                        qk_key = ("qktok", qs)
                        S.dve(TT(rope_t[0][:], x1, cb, ALU.mult), [pk, "cos_t"], ["rt0"])
                        S.dve(TT(rope_t[1][:], x2, sb_, ALU.mult), [pk, "sin_t"], ["rt1"])
                        S.dve(TT(rope_t[2][:], x2, cb, ALU.mult), [pk, "cos_t"], ["rt2"])
                        S.dve(TT(rope_t[3][:], x1, sb_, ALU.mult), [pk, "sin_t"], ["rt3"])
                        S.pool(TT(o3[:, :, 0:8], rope_t[0][:], rope_t[1][:], ALU.subtract), ["rt0", "rt1"], [qk_key])
                        S.pool(TT(o3[:, :, 8:16], rope_t[2][:], rope_t[3][:], ALU.add), ["rt2", "rt3"], [qk_key])
                        S.act(ACP(o3[:, :, 16:64], ps3[:, :, 16:64]), [pk], [qk_key])
                        for cc in range(4):
                            S.pe(TR(PB[:, cc * 128:(cc + 1) * 128], qktok[qs][:, cc * 128:(cc + 1) * 128]),
                                 [qk_key, "ident"], ["pb"])
                        base = 4 * gidx
                        S.dve(CP(qkT[:, base:base + 4, i * 128:(i + 1) * 128],
                                 PB[:, 0:512].rearrange("p (k t) -> p k t", t=128)),
                              ["pb"], [("qk", base + cc_, i) for cc_ in range(4)])

                    proj_mm(0)
                    for i in range(1, NT):
                        proj_mm(i)
                        proj_post(i - 1)
                    proj_post(NT - 1)
                else:
                    for g in range(4):
                        w = 2 ** (g + 1)
                        S.pool(MS(pbuf[:, 0:16], 0.0), [], ["pbuf_h"])
                        S.pool(MS(tmpA[:, 0:16], 0.0), [], ["tmpA_h"])
                        S.pool(MS(tmpB[:, 0:16], 0.0), [], ["tmpB_h"])
                        for tc in range(4):
                            pf = PF[tc % 2]
                            pk = "pf%d" % (tc % 2)
                            for k in range(8):
                                S.pe(MM(pf[:, :], wbf[sl][:, k, g * 128:(g + 1) * 128], hT[:, k, tc * 512:(tc + 1) * 512],
                                        k == 0, k == 7), [("hT", 4 * tc + j) for j in range(4)] + wkeys, [pk])
                            S.act(ACP(pbuf[:, 16 + tc * 512:16 + (tc + 1) * 512], pf[:, :]), [pk], [("pbuf", tc)])
                        pkeys = ["pbuf_h"] + [("pbuf", tc) for tc in range(4)]
                        cur, curk = pbuf, pkeys
                        nxts = [(tmpA, ["tmpA_h", "tmpA"]), (tmpB, ["tmpB_h", "tmpB"])]
                        for step in range(g + 1):
                            sh = 2 ** step
                            nxt, nk = nxts[step % 2]
                            eng = "dve"
                            S.add(eng, TT(nxt[:, 16:16 + SEQ], cur[:, 16:16 + SEQ], cur[:, 16 - sh:16 - sh + SEQ], ALU.add),
                                  curk, [nk[1]])
                            cur, curk = nxt, nk
                        S.dve(TT(cur[:, 16:16 + w - 1], cur[:, 16:16 + w - 1], corr[:, g, 0:w - 1], ALU.mult),
                              curk + ["corr"], [curk[1]])
                        S.dve(STT(dT[:, g, :], cur[:, 16:16 + SEQ], float(1.0 / w), pbuf[:, 16:16 + SEQ], ALU.mult, ALU.subtract),
                              curk + pkeys, [("dT", g)])
                if dbg_is("qearly") and gidx == dbg[3]:
                    break
            S.dma("sp", DM(wpst[:], wpool_d[l].rearrange("g c e -> c g e")), [], ["wpst"])
            S.dve(CP(wpbf[:], wpst[:]), ["wpst"], ["wpbf"])
            if dbg_is("qkT") or dbg_is("qearly"):
                S.barrier()
                cidx = dbg[2]
                S.dve(CP(lnA[0][:], qkT[:, cidx, 0:1024]), [], ["Xd"])
                S.dve(CP(lnA[1][:], qkT[:, cidx, 1024:2048]), [], ["Xd"])
                dump2(lnA[0][:], lnA[1][:])
                stop[0] = True
                break
            S.barrier()
            S.pool(MS(esel[:, :, :], 1.0), [], ["esel"])
            for hb in range(2):
                ev = esel[hb * 64:(hb + 1) * 64, :, :]
                S.pool((lambda ev=ev: lambda e: e.affine_select(out=ev, in_=ev, pattern=[[-1, 64], [0, 128]],
                                                                compare_op=ALU.is_equal, fill=0.0, base=0,
                                                                channel_multiplier=1))(), ["esel"], ["esel"])
            S.dve(MS(KB32[:], 0.0), [], ["KB32"])
            for c in range(4):
                for hh in range(2):
                    h = 2 * c + hh
                    S.dve(TRD(KB32[hh * 64:(hh + 1) * 64, c, h * 8:(h + 1) * 8],
                              qkT[hh * 64:(hh + 1) * 64, 4 + c, :].rearrange("p (n k) -> p n k", k=256), ALU.add),
                          [], ["KB32"])
            S.dve(CP(KB[:], KB32[:]), ["KB32"], ["KB"])
            for i in range(8, NT):
                j = i // 2
                jj = j - 4
                for c in range(4):
                    S.pe(MM(PF[6][:, 0:64], qkT[:, c, i * 128:(i + 1) * 128], KB[:, c, :], c == 0, c == 3), ["KB"], ["pf6"])
                S.dve(TT(gm[:], PF[6][:, 0:64], pastm[:, jj, :], ALU.add), ["pf6", "pastm"], ["gm"])
                for h in range(8):
                    S.dve(MAX8(srt[:, h * 8:(h + 1) * 8], gm[:, h * 8:(h + 1) * 8]), ["gm"], ["srt"])
                gm3 = gm[:].rearrange("p (h n) -> p h n", n=8)
                srt3 = srt[:].rearrange("p (h n) -> p h n", n=8)
                sel3 = selt[:].rearrange("p (h n) -> p h n", n=8)
                bt3 = btok[:, 0:64].rearrange("p (h n) -> p h n", n=8)
                S.dve(TT(sel3, gm3, srt3[:, :, 2:3].broadcast_to([128, 8, 8]), ALU.is_ge), ["gm", "srt"], ["selt"])
                S.dve(TS(btok[:, 0:64], selt[:], -1.0, BIGB, ALU.add, ALU.mult), ["selt"], ["btok"])
                S.dve(MS(bt3[:, :, j:j + 1], 0.0), ["btok"], ["btok"])
                S.dve(CP(btok[:, 64:128], btok[:, 0:64]), ["btok"], ["btok"])
                S.pe(TR(PB[:, 0:128], btok[:, :]), ["btok", "ident"], ["pb"])
                S.act(ACP(biasT[:, (i - 8) * 128:(i - 7) * 128], PB[:, 0:128]), ["pb"], [("biasT", i)])
            load_bc(3, mod_d[l, s:s + 1, 2 * D:3 * D], bmod_d[l:l + 1, 2 * D:3 * D])
            for pc in range(4):
                S.dma("sp", DM(stg[pc % 2][:].rearrange("p (k n) -> p k n", n=D),
                               wout_d[l, 256 * pc:256 * pc + 256, :].rearrange("(k p) n -> p k n", p=128)),
                      [], [("stg", pc % 2)])
                for kk in range(2):
                    S.dve(TT(wo[:, 2 * pc + kk, :], stg[pc % 2][:, kk * D:(kk + 1) * D], bc[3][:], ALU.mult),
                          [("stg", pc % 2), ("bc", 3)], [("wo", 2 * pc + kk)])
            wokeys = [("wo", k) for k in range(8)]
            load_bc(1, ln1g_d[l:l + 1, :])
            load_bc(2, ln1b_d[l:l + 1, :])
            load_bc(0, mod_d[l, s:s + 1, 4 * D:5 * D], bmod_d[l:l + 1, 4 * D:5 * D], plus1=True)
            load_bc(3, mod_d[l, s:s + 1, 3 * D:4 * D], bmod_d[l:l + 1, 3 * D:4 * D])
            if dbg_is("a3"):
                S.barrier()
                S.dve(CP(lnA[0][0:64, :], biasT[0:64, :]), [], ["Xd"])
                dump2(lnA[0][:])
                stop[0] = True
                break
            items = []
            for c in range(4):
                for qc in range(4):
                    for hh in range(2):
                        for kt in range(4 * qc + 4):
                            items.append((c, qc, hh, kt))

            def emit_qk(idx):
                c, qc, hh, kt = items[idx]
                h = 2 * c + hh
                r0 = hh * 64
                n = kt // 2
                q0 = qc * 512 if kt < 4 * qc else kt * 128
                q1 = (qc + 1) * 512
                nq = q1 - q0
                qtiles = list(range(q0 // 128, q1 // 128))
                sc = (PF[0], PF[1], PF[4])[idx % 3]
                sck = ("pf0", "pf1", "pf4")[idx % 3]
                pr = probs[idx % 4]
                prk = ("probs", idx % 4)
                need_bias = qc >= 2
                S.pe(MM(sc[:, 0:nq], qkT[r0:r0 + 64, 4 + c, kt * 128:(kt + 1) * 128],
                        qkT[r0:r0 + 64, c, q0:q1], True, not need_bias),
                     [("qk", 4 + c, kt)] + [("qk", c, t) for t in qtiles], [sck])
                if need_bias:
                    S.pe(MM(sc[:, 0:nq], esel[r0:r0 + 64, h * 8 + n, :],
                            biasT[r0:r0 + 64, q0 - 1024:q1 - 1024], False, True),
                         [("biasT", t) for t in qtiles] + ["esel"], [sck])
                S.act(AV(pr[:, 0:nq], sc[:, 0:nq], AF.Exp, scale=0.125), [sck], [prk])
                if kt >= 4 * qc:
                    S.dve(TT(pr[:, 0:128], pr[:, 0:128], tri[:], ALU.mult), [prk, "tri"], [prk])

            def emit_pv(idx):
                c, qc, hh, kt = items[idx]
                h = 2 * c + hh
                q0 = qc * 512 if kt < 4 * qc else kt * 128
                q1 = (qc + 1) * 512
                qt0 = q0 // 128
                pr = probs[idx % 4]
                prk = ("probs", idx % 4)
                ap_sl = (c * 4 + qc) % 2
                gpar = ((c * 4 + qc) * 2 + hh) % 2
                accb = PF[2 + gpar]
                for qt in range(qt0, q1 // 128):
                    qq = qt - 4 * qc
                    acck = ("acc", gpar, qq)
                    S.pe(MM(accb[:, qq * 128:qq * 128 + 65], pr[:, (qt - qt0) * 128:(qt - qt0 + 1) * 128], vaug[:, kt, h, :],
                            kt == 0 and qt == qt0, kt == 4 * qc + 3), [prk, ("v", kt), "vones"], [acck])
                if kt == 4 * qc + 3:
                    for qq in range(4):
                        accks = [("acc", gpar, q_) for q_ in range(4)]
                        S.dve(RCP(rcp[:, qq:qq + 1], accb[:, qq * 128 + 64:qq * 128 + 65]), accks, [("rcp", qq)])
                        S.dve(TS(apair[ap_sl][:, qq, hh * 64:(hh + 1) * 64], accb[:, qq * 128:qq * 128 + 64], rcp[:, qq:qq + 1],
                                 None, ALU.mult), accks + [("rcp", qq)], [("apair", ap_sl, qq)])
                    if hh == 1:
                        for qq in range(4):
                            S.pe(TR(PB[:, qq * 128:(qq + 1) * 128], apair[ap_sl][:, qq, :]),
                                 [("apair", ap_sl, qq), "ident"], ["pb"])
                        S.act(ACP(qkT[:, c, qc * 512:(qc + 1) * 512], PB[:, 0:512]), ["pb"],
                              [("qk", c, 4 * qc + t) for t in range(4)])

            emit_qk(0)
            emit_qk(1)
            for idx in range(2, len(items)):
                emit_qk(idx)
                emit_pv(idx - 2)
            emit_pv(len(items) - 2)
            emit_pv(len(items) - 1)
            if dbg_is("a4"):
                S.barrier()
                S.dve(CP(lnA[0][:], qkT[:, dbg[2], 0:1024]), [], ["Xd"])
                S.dve(CP(lnA[1][:], qkT[:, dbg[2], 1024:2048]), [], ["Xd"])
                dump2(lnA[0][:], lnA[1][:])
                stop[0] = True
                break
            for g in range(4):
                for tc in range(4):
                    pf = PF[tc % 2]
                    pk = "pf%d" % (tc % 2)
                    S.pe(MM(pf[:, :], wpbf[:, g, :], dT[:, g, tc * 512:(tc + 1) * 512], True, True), ["wpbf", ("dT", g)], [pk])
                    S.act(AV(qkT[:, 4 + g, tc * 512:(tc + 1) * 512], pf[:, :], AF.Identity, scale=pscol[:, l, g:g + 1]),
                          [pk, ("pscol", l)], [("qk", 4 + g, 4 * tc + t) for t in range(4)])
            if dbg_is("amT"):
                S.barrier()
                S.dve(CP(lnA[0][:], qkT[:, dbg[2], 0:1024]), [], ["Xd"])
                S.dve(CP(lnA[1][:], qkT[:, dbg[2], 1024:2048]), [], ["Xd"])
                dump2(lnA[0][:], lnA[1][:])
                stop[0] = True
                break
            S.barrier()
            for i in range(NT):
                S.dma("sp", DM(X[:, i, :], xin[s, i * 128:(i + 1) * 128, :]),
                      [("xs", s, i)] if l > 0 else [], [("X", i)])
            for i in range(NT):
                for half in range(2):
                    pf = PF[4 + half]
                    pk = "pf%d" % (4 + half)
                    for k in range(8):
                        S.pe(MM(pf[:, :], qkT[:, k, i * 128:(i + 1) * 128], wo[:, k, half * 512:(half + 1) * 512], k == 0, k == 7),
                             wokeys, [pk])
                    S.dve(STT(X[:, i, half * 512:(half + 1) * 512], X[:, i, half * 512:(half + 1) * 512], ALPHA,
                              pf[:, :], ALU.mult, ALU.add), [pk, ("X", i)], [("X", i)])
                ln_stats(i, X[:, i, :], [("X", i)])
            ln_finish()
            for i in range(NT):
                sl = i % 2
                ln_apply(i, X[:, i, :], [("X", i)], lnA[sl][:], ["lnA%d" % sl], bc[1][:], [("bc", 1)],
                         bc[2][:], [("bc", 2)], X[:, i, :], [("X", i)])
                ln_stats(i, X[:, i, :], [("X", i)])
            ln_finish()
            for i in range(NT):
                sl = i % 2
                ln_apply(i, X[:, i, :], [("X", i)], lnA[sl][:], ["lnA%d" % sl], bc[0][:], [("bc", 0)],
                         bc[3][:], [("bc", 3)], htok[sl][:], [("htok", sl)])
                transpose_to_hT(i, sl)
            if dbg_is("X1"):
                S.barrier()
                dump2(X[:, dbg[2], :])
                stop[0] = True
                break
            for tg in range(4):
                lg = PF[6]
                for tt in range(4):
                    i = 4 * tg + tt
                    for k in range(8):
                        S.pe(MM(lg[:, tt * 16:(tt + 1) * 16], hT[:, k, i * 128:(i + 1) * 128], wrb[:, k, :], k == 0, k == 7),
                             [("hT", i), "wrb"], ["pf6"])

                def v3(t, a):
                    return t[:].rearrange("p (a b) -> p a b", a=a)

                def bcl(t, a):
                    return t[:, 0:a].rearrange("p (a o) -> p a o", o=1).broadcast_to([128, a, 64 // a])

                R_ = lambda k: ("rt", k)
                rb_keys = [("rb4", i_) for i_ in range(4)]
                lg3 = lg[:, 0:64].rearrange("p (a b) -> p a b", a=4)
                S.dve(TRD(rt[0][:, 0:4], lg3, ALU.max), ["pf6"], [R_(0)])
                S.dve(TT(v3(rt[1], 4), lg3, bcl(rt[0], 4), ALU.subtract), ["pf6", R_(0)], [R_(1)])
                S.act(AV(rt[1][:], rt[1][:], AF.Exp), [R_(1)], [R_(1)])
                S.dve(TRD(rt[0][:, 0:4], v3(rt[1], 4), ALU.add), [R_(1)], [R_(0)])
                S.dve(RCP(rt[0][:, 0:4], rt[0][:, 0:4]), [R_(0)], [R_(0)])
                S.dve(TT(v3(rt[2], 4), v3(rt[1], 4), bcl(rt[0], 4), ALU.mult), [R_(1), R_(0)], [R_(2)])
                S.dve(TT(rt[3][:], rt[2][:], rb4[:].rearrange("p a b -> p (a b)"), ALU.add), [R_(2)] + rb_keys, [R_(3)])
                S.dve(TRD(rt[4][:, 0:16], v3(rt[3], 16), ALU.max), [R_(3)], [R_(4)])
                S.dve(TT(v3(rt[5], 16), v3(rt[3], 16), bcl(rt[4], 16), ALU.is_equal), [R_(3), R_(4)], [R_(5)])
                S.dve(STT(rt[5][:], rt[5][:], NEG, rt[3][:], ALU.mult, ALU.add), [R_(5), R_(3)], [R_(5)])
                S.dve(TRD(rt[6][:, 0:16], v3(rt[5], 16), ALU.max), [R_(5)], [R_(6)])
                S.dve(TT(rt[4][:, 0:16], rt[4][:, 0:16], rt[6][:, 0:16], ALU.add), [R_(4), R_(6)], [R_(4)])
                gs3 = rt[4][:, 0:16].rearrange("p (a b) -> p a b", a=4)
                S.dve(TRD(rt[6][:, 0:4], gs3, ALU.max), [R_(4)], [R_(6)])
                S.dve(TT(rt[7][:, 0:16].rearrange("p (a b) -> p a b", a=4), gs3,
                         rt[6][:, 0:4].rearrange("p (a o) -> p a o", o=1).broadcast_to([128, 4, 4]), ALU.is_equal),
                      [R_(4), R_(6)], [R_(7)])
                S.dve(TS(rt[7][:, 0:16], rt[7][:, 0:16], -1.0, -NEG, ALU.add, ALU.mult), [R_(7)], [R_(7)])
                S.dve(TT(v3(rt[8], 16), v3(rt[3], 16), bcl(rt[7], 16), ALU.add), [R_(3), R_(7)], [R_(8)])
                S.dve(TRD(rt[6][:, 0:4], v3(rt[8], 4), ALU.max), [R_(8)], [R_(6)])
                S.dve(TT(v3(rt[9], 4), v3(rt[8], 4), bcl(rt[6], 4), ALU.is_equal), [R_(8), R_(6)], [R_(9)])
                S.dve(STT(rt[8][:], rt[9][:], NEG, rt[8][:], ALU.mult, ALU.add), [R_(9), R_(8)], [R_(8)])
                S.dve(TRD(rt[6][:, 0:4], v3(rt[8], 4), ALU.max), [R_(8)], [R_(6)])
                S.dve(TT(v3(rt[10], 4), v3(rt[8], 4), bcl(rt[6], 4), ALU.is_equal), [R_(8), R_(6)], [R_(10)])
                S.dve(TT(rt[9][:], rt[9][:], rt[10][:], ALU.add), [R_(9), R_(10)], [R_(9)])
                S.dve(TT(rt[9][:], rt[9][:], rt[2][:], ALU.mult), [R_(9), R_(2)], [R_(9)])
                S.dve(TRD(rt[6][:, 0:4], v3(rt[9], 4), ALU.add), [R_(9)], [R_(6)])
                S.dve(RCP(rt[6][:, 0:4], rt[6][:, 0:4]), [R_(6)], [R_(6)])
                S.dve(TS(rt[6][:, 0:4], rt[6][:, 0:4], float(1.0 / ALPHA), None, ALU.mult), [R_(6)], [R_(6)])
                S.dve(TT(gates[:, 4 * tg:4 * tg + 4, :], v3(rt[9], 4), bcl(rt[6], 4), ALU.mult), [R_(9), R_(6)], [("gates", tg)])
            if dbg_is("gates"):
                S.barrier()
                S.dve(CP(lnA[0][:, 0:256], gates[:].rearrange("p a b -> p (a b)")), [], ["Xd"])
                dump2(lnA[0][:])
                stop[0] = True
                break
            S.barrier()
            load_bc(0, mod_d[l, s:s + 1, 5 * D:6 * D], bmod_d[l:l + 1, 5 * D:6 * D])
            load_bc(1, ln2g_d[l:l + 1, :])
            load_bc(2, ln2b_d[l:l + 1, :])
            nexp = 1 if dbg_is("moe1") else NE

            def load_expert_dma(e_):
                sl = e_ % 2
                for gu, wsrc in ((0, wg_d), (1, wu_d)):
                    S.dma("pool", DM(wgu[sl][:, :, gu, :], wsrc[l, e_].rearrange("(k p) n -> p k n", p=128)),
                          [], [("wgu", sl, gu, 0), ("wgu", sl, gu, 1)])
                for h in range(2):
                    S.dma("sp", DM(stg[h][:].rearrange("p (k n) -> p k n", n=D),
                                   wd_d[l, e_, 256 * h:256 * h + 256, :].rearrange("(k p) n -> p k n", p=128)),
                          [], [("stg", h)])

            def load_expert_cast(e_):
                sl = e_ % 2
                for h in range(2):
                    for kk in range(2):
                        S.dve(TT(wdb[sl][:, 2 * h + kk, :], stg[h][:, kk * D:(kk + 1) * D], bc[0][:], ALU.mult),
                              [("stg", h), ("bc", 0)], [("wd", sl, 2 * h + kk)])

            stages = [(e_, tc) for e_ in range(nexp) for tc in range(4)]

            def emit_gu(si):
                e_, tc = stages[si]
                sl = e_ % 2
                asl = si % 2
                for f in range(4):
                    Gp, Gk = PF[f % 2], "pf%d" % (f % 2)
                    Up, Uk = PF[2 + f % 2], "pf%d" % (2 + f % 2)
                    for gu, pp, pk in ((0, Gp, Gk), (1, Up, Uk)):
                        for k in range(8):
                            S.pe(MM(pp[:, :], wgu[sl][:, k, gu, f * 128:(f + 1) * 128], hT[:, k, tc * 512:(tc + 1) * 512],
                                    k == 0, k == 7),
                                 [("wgu", sl, gu, 0), ("wgu", sl, gu, 1)] + [("hT", 4 * tc + j) for j in range(4)], [pk])
                    S.act(AV(sg[f % 2][:], Gp[:, :], AF.Silu), [Gk], [("sg", f % 2)])
                    S.dve(TT(aT[asl][:, f, :], Up[:, :], sg[f % 2][:], ALU.mult), [Uk, ("sg", f % 2)], [("aT", asl, f)])

            def emit_y(si):
                e_, tc = stages[si]
                sl = e_ % 2
                asl = si % 2
                for tt in range(4):
                    i = 4 * tc + tt
                    for half in range(2):
                        Yp, Yk = PF[4 + half], "pf%d" % (4 + half)
                        for f in range(4):
                            S.pe(MM(Yp[:, :], aT[asl][:, f, tt * 128:(tt + 1) * 128], wdb[sl][:, f, half * 512:(half + 1) * 512],
                                    f == 0, f == 3), [("aT", asl, f), ("wd", sl, f)], [Yk])
                        S.dve(STT(X[:, i, half * 512:(half + 1) * 512], Yp[:, :], gates[:, i, e_:e_ + 1],
                                  X[:, i, half * 512:(half + 1) * 512], ALU.mult, ALU.add),
                              [Yk, ("gates", i // 4), ("X", i)], [("X", i)])

            load_expert_dma(0)
            load_expert_cast(0)
            for si in range(len(stages)):
                e_, tc = stages[si]
                emit_gu(si)
                if si > 0:
                    emit_y(si - 1)
                if tc == 0 and e_ + 1 < nexp:
                    load_expert_dma(e_ + 1)
                if tc == 2 and e_ + 1 < nexp:
                    load_expert_cast(e_ + 1)
            emit_y(len(stages) - 1)
            if dbg_is("moe1") or dbg_is("X2"):
                S.barrier()
                dump2(X[:, dbg[2], :])
                stop[0] = True
                break
            for i in range(NT):
                ln_stats(i, X[:, i, :], [("X", i)])
            ln_finish(float(EPS / (ALPHA * ALPHA)))
            for i in range(NT):
                sl = i % 2
                ln_apply(i, X[:, i, :], [("X", i)], lnA[sl][:], ["lnA%d" % sl], bc[1][:], [("bc", 1)],
                         bc[2][:], [("bc", 2)], lnA[sl][:], ["lnA%d" % sl])
                S.dma("pool", DM(xout[s, i * 128:(i + 1) * 128, :], lnA[sl][:]),
                      ["lnA%d" % sl], [("xs", s, i)] if xout is xs_d else [("out", s, i)])
        if stop[0]:
            break
    S.barrier()
    S.emit(nc)
    return nc


_NC_CACHE = {}


def kernel(**inputs):
    if "nc" not in _NC_CACHE:
        _NC_CACHE["nc"] = build()
    nc = _NC_CACHE["nc"]
    f = lambda a: np.ascontiguousarray(np.asarray(a, dtype=np.float32))
    x = f(inputs["x"])
    c = f(inputs["c"])
    shared = {k: f(inputs[k]) for k in ("w_mod", "b_mod", "w_in", "w_pool", "pool_scale", "w_out", "ln1_g", "ln1_b",
                                        "w_router", "w_gate", "w_up", "w_down", "ln2_g", "ln2_b")}
    shared["router_bias"] = f(inputs["router_bias"]).reshape(1, NE)
    in_maps = []
    for core in range(8):
        m = dict(shared)
        m["x"] = x[2 * core:2 * core + 2]
        m["c"] = c[2 * core:2 * core + 2]
        in_maps.append(m)
    res = run_bass_kernel_spmd(nc, in_maps, core_ids=list(range(8)))
    return np.concatenate([r["out"] for r in res.results], axis=0)
```
